# Optimizing a Trainium2 kernel written in Bass

```python
import math
import jax, jax.numpy as jnp
from jax import lax
import numpy as np

D_MODEL = 1024
BATCH = 16
SEQ = 2048
DEPTH = 4

GRID_W = 64
CTX_LEN = 256
N_MIXERS = 3
N_LAYERS_A = (DEPTH + 2) // 3
N_LAYERS_B = (DEPTH + 1) // 3
N_LAYERS_C = DEPTH // 3
EPS = 1e-6

DA_HEADS = 8
DA_QK_DIM = 64
DA_V_DIM = 2 * DA_QK_DIM
Q_BLOCK = 128
ROPE_BASE = 10000.0

LRU_WIDTH = 1280
LRU_BLOCKS = 10
LRU_BLOCK_DIM = LRU_WIDTH // LRU_BLOCKS
CONV_W = 4
LRU_C = 8.0

POOL_GROUPS = 4
POOL_WINDOWS = (2, 4, 8, 16)
POOL_GROUP_DIM = D_MODEL // POOL_GROUPS

N_GROUPS = 4
EXPERTS_PER_GROUP = 4
N_EXPERTS = N_GROUPS * EXPERTS_PER_GROUP
TOP_K = 2
D_EXPERT = 512

kernel_name = "hybrid_diffattn_rglru_pool_hmoe_prefix_dit"


def rms_norm(x, g):
    xf = x.astype(jnp.float32)
    y = xf * lax.rsqrt(jnp.mean(xf * xf, axis=-1, keepdims=True) + EPS)
    return (y * g.astype(jnp.float32)).astype(x.dtype)


def modulate(h, shift, scale):
    return h * (1 + scale) + shift


def axial_rope_tables(n_tokens):
    rows = n_tokens // GRID_W
    row = jnp.repeat(jnp.arange(rows), GRID_W).astype(jnp.float32)
    col = jnp.tile(jnp.arange(GRID_W), rows).astype(jnp.float32)
    n_freq = DA_QK_DIM // 4
    inv = ROPE_BASE ** (-jnp.arange(n_freq, dtype=jnp.float32) / n_freq)
    ang = jnp.concatenate([row[:, None] * inv, col[:, None] * inv], axis=-1)
    return jnp.cos(ang), jnp.sin(ang)


def apply_rope(x, cos, sin):
    x1, x2 = jnp.split(x, 2, axis=-1)
    cos = cos[None, :, None, None, :].astype(x.dtype)
    sin = sin[None, :, None, None, :].astype(x.dtype)
    return jnp.concatenate([x1 * cos - x2 * sin, x1 * sin + x2 * cos], axis=-1)


def diff_attend(q, k, v, lam):
    s = jnp.einsum('bqhmd,bkhmd->bhmqk', q, k,
                   preferred_element_type=jnp.float32) * (DA_QK_DIM ** -0.5)
    p = jax.nn.softmax(s, axis=-1)
    a = p[:, :, 0] - lam * p[:, :, 1]
    return jnp.einsum('bhqk,bkhd->bqhd', a.astype(v.dtype), v)


def diff_attn_mixer(hx, hc, w_in, q_gain, k_gain, lam_qk, sub_gain, w_out, layer_idx, need_ctx):
    B, S, _ = hx.shape
    lam_init = 0.8 - 0.6 * math.exp(-0.3 * layer_idx)
    lq = lam_qk.astype(jnp.float32)
    lam = jnp.exp(jnp.sum(lq[0] * lq[1])) - jnp.exp(jnp.sum(lq[2] * lq[3])) + lam_init

    def project(h):
        n = h.shape[1]
        q, k, v = jnp.split(h @ w_in, 3, axis=-1)
        q = rms_norm(q.reshape(B, n, DA_HEADS, 2, DA_QK_DIM), q_gain)
        k = rms_norm(k.reshape(B, n, DA_HEADS, 2, DA_QK_DIM), k_gain)
        return q, k, v.reshape(B, n, DA_HEADS, DA_V_DIM)

    qx, kx, vx = project(hx)
    qc, kc, vc = project(hc)
    cos, sin = axial_rope_tables(S)
    qx = apply_rope(qx, cos, sin)
    kx = apply_rope(kx, cos, sin)
    k_all = jnp.concatenate([kc, kx], axis=1)
    v_all = jnp.concatenate([vc, vx], axis=1)
    nb = S // Q_BLOCK
    q_blocks = qx.reshape(B, nb, Q_BLOCK, DA_HEADS, 2, DA_QK_DIM).transpose(1, 0, 2, 3, 4, 5)
    o = lax.map(lambda qb: diff_attend(qb, k_all, v_all, lam), q_blocks)
    ox = o.transpose(1, 0, 2, 3, 4).reshape(B, S, DA_HEADS, DA_V_DIM)

    def finish(o):
        n = o.shape[1]
        o = rms_norm(o, sub_gain) * (1.0 - lam_init)
        return o.reshape(B, n, DA_HEADS * DA_V_DIM) @ w_out

    yx = finish(ox)
    yc = finish(diff_attend(qc, kc, vc, lam)) if need_ctx else None
    return yx, yc


def dw_conv_centred(x, w, b):
    n = x.shape[1]
    left = CONV_W // 2
    xp = jnp.pad(x, ((0, 0), (left, CONV_W - 1 - left), (0, 0)))
    return b + sum(w[k] * xp[:, k:k + n] for k in range(CONV_W))


def block_diag(x, w, b):
    B, n, _ = x.shape
    xb = x.reshape(B, n, LRU_BLOCKS, LRU_BLOCK_DIM)
    return jnp.einsum('bngi,gij->bngj', xb, w).reshape(B, n, LRU_WIDTH) + b


def lru_coeffs(xc, w_a, b_a, w_x, b_x, lam, reset_idx):
    r = jax.nn.sigmoid(block_diag(xc, w_a, b_a)).astype(jnp.float32)
    i = jax.nn.sigmoid(block_diag(xc, w_x, b_x)).astype(jnp.float32)
    log_a = -LRU_C * r * jax.nn.softplus(-lam.astype(jnp.float32))
    a = jnp.exp(log_a)
    mult = jnp.sqrt(-jnp.expm1(2.0 * log_a))
    if reset_idx is not None:
        mult = mult.at[:, reset_idx].set(1.0)
    return a, mult * i * xc.astype(jnp.float32)


def linear_scan(a, b, h0, reverse):
    if h0 is not None:
        edge = -1 if reverse else 0
        b = b.at[:, edge].add(a[:, edge] * h0)

    def comb(e1, e2):
        a1, b1 = e1
        a2, b2 = e2
        return a1 * a2, a2 * b1 + b2

    _, h = lax.associative_scan(comb, (a, b), axis=1, reverse=reverse)
    return h


def rglru_mixer(hx, hc, w_in, conv_w, conv_b, w_a, b_a, w_x, b_x, lam, w_out, need_ctx):
    def branches(h):
        gate, xr = jnp.split(h @ w_in, 2, axis=-1)
        return gate, dw_conv_centred(xr, conv_w, conv_b)

    gx, xx = branches(hx)
    gc, xcx = branches(hc)
    hsum_x = 0.0
    hsum_c = 0.0
    for d, reverse in enumerate((False, True)):
        start = -1 if reverse else 0
        end = 0 if reverse else -1
        ac, bc = lru_coeffs(xcx, w_a[d], b_a[d], w_x[d], b_x[d], lam[d], start)
        hc_seq = linear_scan(ac, bc, None, reverse)
        h_final = hc_seq[:, end]
        ax, bx = lru_coeffs(xx, w_a[d], b_a[d], w_x[d], b_x[d], lam[d], None)
        hx_seq = linear_scan(ax, bx, h_final, reverse)
        hsum_x = hsum_x + hx_seq
        hsum_c = hsum_c + hc_seq
    yx = (hsum_x.astype(gx.dtype) * jax.nn.gelu(gx)) @ w_out
    yc = (hsum_c.astype(gc.dtype) * jax.nn.gelu(gc)) @ w_out if need_ctx else None
    return yx, yc


def centred_mean_minus_self(x, window):
    B, n, C = x.shape
    cs = jnp.concatenate([jnp.zeros((B, 1, C), x.dtype), jnp.cumsum(x, axis=1)], axis=1)
    t = jnp.arange(n)
    lo = jnp.clip(t - window // 2, 0, n)
    hi = jnp.clip(t + window // 2, 0, n)
    cnt = (hi - lo).astype(jnp.float32)[None, :, None]
    return (cs[:, hi] - cs[:, lo]) / cnt - x


def pool_mixer(hx, hc, w_in, w_grp, scale, need_ctx):
    def mix(h):
        B, n, _ = h.shape
        u = (h @ w_in).astype(jnp.float32).reshape(B, n, POOL_GROUPS, POOL_GROUP_DIM)
        pooled = jnp.stack([centred_mean_minus_self(u[:, :, g], w)
                            for g, w in enumerate(POOL_WINDOWS)], axis=2)
        y = jnp.einsum('bngc,gcd->bngd', pooled.astype(h.dtype), w_grp).reshape(B, n, D_MODEL)
        return y * scale

    return mix(hx), (mix(hc) if need_ctx else None)


def hier_moe(h, router_g, router_e, w1, w3, w2):
    B, n, D = h.shape
    t = h.reshape(B * n, D)
    lg = (t @ router_g).astype(jnp.float32)
    pg = jax.nn.softmax(lg, axis=-1)
    g_sel = jnp.argmax(lg, axis=-1)
    p_sel = jnp.take_along_axis(pg, g_sel[:, None], axis=-1)
    le = (t @ router_e).astype(jnp.float32).reshape(-1, N_GROUPS, EXPERTS_PER_GROUP)
    le_sel = jnp.take_along_axis(le, g_sel[:, None, None], axis=1)[:, 0]
    top_p, top_i = lax.top_k(jax.nn.softmax(le_sel, axis=-1), TOP_K)
    top_p = top_p / jnp.sum(top_p, axis=-1, keepdims=True) * p_sel
    expert_id = g_sel[:, None] * EXPERTS_PER_GROUP + top_i
    combine = jnp.sum(jax.nn.one_hot(expert_id, N_EXPERTS, dtype=jnp.float32)
                      * top_p[..., None], axis=1)
    y = jnp.zeros((B * n, D), jnp.float32)
    for e in range(N_EXPERTS):
        he = jax.nn.silu(t @ w1[e]) * (t @ w3[e])
        y = y + combine[:, e:e + 1] * (he @ w2[e]).astype(jnp.float32)
    return y.astype(h.dtype).reshape(B, n, D)


def setup_inputs(seed: int = 0) -> dict:
    key = jax.random.key(seed)
    ks = iter(jax.random.split(key, 40))
    f32 = jnp.float32
    D = D_MODEL

    def nrm(shape, fan_in):
        return jax.random.normal(next(ks), shape, f32) * fan_in ** -0.5

    def gain(shape):
        return 1.0 + 0.1 * jax.random.normal(next(ks), shape, f32)

    def small(shape):
        return 0.02 * jax.random.normal(next(ks), shape, f32)

    u = jax.random.uniform(next(ks), (N_LAYERS_B, 2, LRU_WIDTH), f32, 0.9, 0.999)
    s = u ** (1.0 / LRU_C)
    lru_lam = jnp.log(s) - jnp.log1p(-s)
    return {
        "x": jax.random.normal(next(ks), (BATCH, SEQ, D), f32),
        "c": jax.random.normal(next(ks), (BATCH, D), f32),
        "ctx": jax.random.normal(next(ks), (BATCH, CTX_LEN, D), f32),
        "c_ctx": jax.random.normal(next(ks), (D,), f32),
        "w_ada": nrm((DEPTH, D, 6 * D), D),
        "b_ada": small((DEPTH, 6 * D)),
        "norm_g": gain((DEPTH, 2, D)),
        "attn_w_in": nrm((N_LAYERS_A, D, 3 * D), D),
        "attn_q_gain": gain((N_LAYERS_A, DA_QK_DIM)),
        "attn_k_gain": gain((N_LAYERS_A, DA_QK_DIM)),
        "attn_lam": 0.1 * jax.random.normal(next(ks), (N_LAYERS_A, 4, DA_QK_DIM), f32),
        "attn_sub_gain": gain((N_LAYERS_A, DA_V_DIM)),
        "attn_w_out": nrm((N_LAYERS_A, DA_HEADS * DA_V_DIM, D), DA_HEADS * DA_V_DIM),
        "lru_w_in": nrm((N_LAYERS_B, D, 2 * LRU_WIDTH), D),
        "lru_conv_w": nrm((N_LAYERS_B, CONV_W, LRU_WIDTH), CONV_W),
        "lru_conv_b": small((N_LAYERS_B, LRU_WIDTH)),
        "lru_w_a": nrm((N_LAYERS_B, 2, LRU_BLOCKS, LRU_BLOCK_DIM, LRU_BLOCK_DIM), LRU_BLOCK_DIM),
        "lru_b_a": small((N_LAYERS_B, 2, LRU_WIDTH)),
        "lru_w_x": nrm((N_LAYERS_B, 2, LRU_BLOCKS, LRU_BLOCK_DIM, LRU_BLOCK_DIM), LRU_BLOCK_DIM),
        "lru_b_x": small((N_LAYERS_B, 2, LRU_WIDTH)),
        "lru_lam": lru_lam,
        "lru_w_out": nrm((N_LAYERS_B, LRU_WIDTH, D), LRU_WIDTH),
        "pool_w_in": nrm((N_LAYERS_C, D, D), D),
        "pool_w_grp": nrm((N_LAYERS_C, POOL_GROUPS, POOL_GROUP_DIM, POOL_GROUP_DIM), POOL_GROUP_DIM),
        "pool_scale": gain((N_LAYERS_C, D)),
        "moe_router_g": nrm((DEPTH, D, N_GROUPS), D),
        "moe_router_e": nrm((DEPTH, D, N_EXPERTS), D),
        "moe_w1": nrm((DEPTH, N_EXPERTS, D, D_EXPERT), D),
        "moe_w3": nrm((DEPTH, N_EXPERTS, D, D_EXPERT), D),
        "moe_w2": nrm((DEPTH, N_EXPERTS, D_EXPERT, D), D_EXPERT),
    }


def reference(x, c, ctx, c_ctx, w_ada, b_ada, norm_g,
              attn_w_in, attn_q_gain, attn_k_gain, attn_lam, attn_sub_gain, attn_w_out,
              lru_w_in, lru_conv_w, lru_conv_b, lru_w_a, lru_b_a, lru_w_x, lru_b_x, lru_lam, lru_w_out,
              pool_w_in, pool_w_grp, pool_scale,
              moe_router_g, moe_router_e, moe_w1, moe_w3, moe_w2):
    xs = x
    cs = ctx
    sc = jax.nn.silu(c)
    scc = jax.nn.silu(c_ctx)
    for i in range(DEPTH):
        need_ctx = i < DEPTH - 1
        mx = (sc @ w_ada[i] + b_ada[i])[:, None, :]
        mc = (scc @ w_ada[i] + b_ada[i])[None, None, :]
        sh1x, sc1x, g1x, sh2x, sc2x, g2x = jnp.split(mx, 6, axis=-1)
        sh1c, sc1c, g1c, sh2c, sc2c, g2c = jnp.split(mc, 6, axis=-1)

        ux = modulate(rms_norm(xs, norm_g[i, 0]), sh1x, sc1x)
        uc = modulate(rms_norm(cs, norm_g[i, 0]), sh1c, sc1c)
        kind, j = i % N_MIXERS, i // N_MIXERS
        if kind == 0:
            yx, yc = diff_attn_mixer(ux, uc, attn_w_in[j], attn_q_gain[j], attn_k_gain[j],
                                     attn_lam[j], attn_sub_gain[j], attn_w_out[j], i, need_ctx)
        elif kind == 1:
            yx, yc = rglru_mixer(ux, uc, lru_w_in[j], lru_conv_w[j], lru_conv_b[j], lru_w_a[j],
                                 lru_b_a[j], lru_w_x[j], lru_b_x[j], lru_lam[j], lru_w_out[j], need_ctx)
        else:
            yx, yc = pool_mixer(ux, uc, pool_w_in[j], pool_w_grp[j], pool_scale[j], need_ctx)
        xs = xs + g1x * yx
        ux = modulate(rms_norm(xs, norm_g[i, 1]), sh2x, sc2x)
        xs = xs + g2x * hier_moe(ux, moe_router_g[i], moe_router_e[i], moe_w1[i], moe_w3[i], moe_w2[i])
        if need_ctx:
            cs = cs + g1c * yc
            uc = modulate(rms_norm(cs, norm_g[i, 1]), sh2c, sc2c)
            cs = cs + g2c * hier_moe(uc, moe_router_g[i], moe_router_e[i], moe_w1[i], moe_w3[i], moe_w2[i])
    return xs
```

```python
import contextlib
import math
import numpy as np
import concourse.bass as bass
import concourse.mybir as mybir
from concourse.ap import AP
from concourse.bass_utils import run_bass_kernel_spmd

F32 = mybir.dt.float32
BF16 = mybir.dt.bfloat16
AF = mybir.ActivationFunctionType
ALU = mybir.AluOpType
AX = mybir.AxisListType

N_DMA_SEMS = 12
SAME_ENGINE_SYNC = True


class Tok:
    __slots__ = ("name", "last_w", "readers")

    def __init__(self, name=""):
        self.name = name
        self.last_w = None
        self.readers = []


class Op:
    __slots__ = ("eng", "fn", "deps", "signal", "val", "is_dma", "slot")

    def __init__(self, eng, fn, is_dma):
        self.eng = eng
        self.fn = fn
        self.deps = []
        self.signal = False
        self.val = None
        self.is_dma = is_dma
        self.slot = None


class Prog:
    ENGS = ("pe", "act", "dve", "pool", "sp")

    def __init__(self, nc):
        self.nc = nc
        self.ops = []
        self.stack = contextlib.ExitStack()
        self.n_alloc = 0
        self.bar_idx = 0

    def sbuf(self, shape, dtype, name=None):
        self.n_alloc += 1
        return self.stack.enter_context(self.nc.sbuf_tensor(name or f"sb{self.n_alloc}", list(shape), dtype))

    def psum(self, shape, dtype, name=None):
        self.n_alloc += 1
        return self.stack.enter_context(self.nc.psum_tensor(name or f"ps{self.n_alloc}", list(shape), dtype))

    def op(self, eng, fn, reads=(), writes=(), dma=False):
        o = Op(eng, fn, dma)
        deps = []
        for t in reads:
            if t.last_w is not None:
                deps.append(t.last_w)
        for t in writes:
            if t.last_w is not None:
                deps.append(t.last_w)
            deps.extend(t.readers)
        seen = set()
        for d in deps:
            if id(d) in seen or d is o:
                continue
            seen.add(id(d))
            if (not d.is_dma) and (not dma) and d.eng == eng:
                if eng == "pe" or not SAME_ENGINE_SYNC:
                    continue
            d.signal = True
            o.deps.append(d)
        for t in writes:
            t.last_w = o
            t.readers = []
        for t in reads:
            if t in writes:
                continue
            if not dma:
                t.readers = [r for r in t.readers if r.is_dma or r.eng != eng]
            t.readers.append(o)
        self.ops.append(o)
        return o

    def dma(self, eng, out, in_, reads=(), writes=(), **kw):
        return self.op(eng, lambda e: e.dma_start(out=out, in_=in_, **kw), reads, writes, dma=True)

    def barrier(self):
        last = {}
        dmas = []
        for o in self.ops[self.bar_idx:]:
            if o.is_dma:
                dmas.append(o)
            else:
                last[o.eng] = o
        deps = list(last.values()) + dmas
        first = len(self.ops)
        for e in self.ENGS:
            o = Op(e, lambda eng: eng.nop(), False)
            o.deps = [d for d in deps if d.is_dma or d.eng != e]
            for d in o.deps:
                d.signal = True
            self.ops.append(o)
        self.bar_idx = first

    def emit(self):
        nc = self.nc
        cnt = {e: 0 for e in self.ENGS}
        dcnt = {e: 0 for e in self.ENGS}
        slotcnt = {e: [0] * N_DMA_SEMS for e in self.ENGS}
        for o in self.ops:
            if o.is_dma:
                k = dcnt[o.eng] % N_DMA_SEMS
                dcnt[o.eng] += 1
                o.slot = k
                slotcnt[o.eng][k] += 16
                o.val = slotcnt[o.eng][k]
            elif o.signal:
                cnt[o.eng] += 1
                o.val = cnt[o.eng]
        sems = {}
        for e in self.ENGS:
            if cnt[e] > 0:
                sems[e] = self.stack.enter_context(nc.semaphore(f"s_{e}"))
        dsems = {}
        for e in self.ENGS:
            if dcnt[e] > 0:
                dsems[e] = [self.stack.enter_context(nc.semaphore(f"d_{e}{k}"))
                            for k in range(min(N_DMA_SEMS, dcnt[e]))]
        by_eng = {e: [o for o in self.ops if o.eng == e] for e in self.ENGS}
        self.stats = {e: len(by_eng[e]) for e in self.ENGS}
        self.stats["maxsem"] = dict(cnt)
        nwaits = [0]

        def run(e_name, eng):
            known = {}
            for o in by_eng[e_name]:
                waits = {}
                for d in o.deps:
                    key = ("d", d.eng, d.slot) if d.is_dma else ("c", d.eng)
                    if known.get(key, 0) >= d.val:
                        continue
                    waits[key] = max(waits.get(key, 0), d.val)
                if o.is_dma and o.val > 16:
                    key = ("d", o.eng, o.slot)
                    if known.get(key, 0) < o.val - 16:
                        waits[key] = max(waits.get(key, 0), o.val - 16)
                for key, v in waits.items():
                    s = dsems[key[1]][key[2]] if key[0] == "d" else sems[key[1]]
                    eng.wait_ge(s, v)
                    known[key] = v
                    nwaits[0] += 1
                ins = o.fn(eng)
                if o.is_dma:
                    ins.then_inc(dsems[o.eng][o.slot], 16)
                elif o.signal:
                    ins.then_inc(sems[o.eng], 1)

        with nc.Block() as block:
            if by_eng["sp"]:
                @block.sync
                def _(eng):
                    run("sp", eng)
            if by_eng["pe"]:
                @block.tensor
                def _(eng):
                    run("pe", eng)
            if by_eng["act"]:
                @block.scalar
                def _(eng):
                    run("act", eng)
            if by_eng["dve"]:
                @block.vector
                def _(eng):
                    run("dve", eng)
            if by_eng["pool"]:
                @block.gpsimd
                def _(eng):
                    run("pool", eng)
        self.stats["waits"] = nwaits[0]
        self.stack.close()


class Arena:
    def __init__(self, P, nbytes):
        self.t = P.sbuf([128, nbytes // 4], F32, "arena")
        self.cap = nbytes
        self.off = 0

    def alloc(self, free_shape, dtype):
        n = 1
        for s in free_shape:
            n *= s
        nb = n * (2 if dtype == BF16 else 4)
        nb = (nb + 31) // 32 * 32
        assert self.off + nb <= self.cap, f"arena overflow {self.off}+{nb}>{self.cap}"
        a = self.t[:, self.off // 4:(self.off + nb) // 4]
        self.off += nb
        if dtype != F32:
            a = a.bitcast(dtype)
        a = a[:, 0:n]
        if len(free_shape) == 2:
            a = a.rearrange("p (a b) -> p a b", b=free_shape[1])
        elif len(free_shape) == 3:
            a = a.rearrange("p (a b c) -> p a b c", b=free_shape[1], c=free_shape[2])
        return a


def bc(ap, shape):
    return ap.broadcast_to(list(shape))


def rev2d(ap2d):
    a = ap2d.ap
    n = a[1][1]
    st = a[1][0]
    return AP(ap2d.tensor, ap2d.offset + (n - 1) * st, [list(a[0]), [-st, n]])


D = 1024
KD = 8
S = 2048
CT = 256
NTILE = 18
TB = NTILE * 128
EPS = 1e-6
NL = 4
N_EXP = 16
DE = 512
LRU_W = 1280
LW = 2310
LAT0 = 2
CTX0 = 2053


def build(layers=(0, 1, 2, 3), dbg=False, do_mixer=True, do_moe=True):
    nc = bass.Bass("TRN2", target_bir_lowering=False)
    P = Prog(nc)

    def din(name, shape):
        return nc.dram_tensor(name, list(shape), F32, kind="ExternalInput").ap()

    resin = din("resin", [4608, D])
    cT_d = din("cT", [D, 3])
    w_ada = din("w_ada", [NL, D, 6 * D])
    badaT_d = din("badaT", [128, NL, 48])
    normgT_d = din("normgT", [128, NL, 2, 8])
    ident_d = din("ident", [128, 128])
    attn_w_in = din("attn_w_in", [2, D, 3 * D])
    attn_qg = din("attn_q_gain", [2, 64])
    attn_kg = din("attn_k_gain", [2, 64])
    attn_lam = din("attn_lam", [2, 256])
    attn_sgT = din("attn_sgT", [128, 2])
    attn_w_out = din("attn_w_out", [2, D, D])
    rope_cos = din("rope_cos", [128, NTILE, 32])
    rope_sin = din("rope_sin", [128, NTILE, 32])
    lru_w_in = din("lru_w_in", [1, D, 2 * LRU_W])
    lru_cwT = din("lru_cwT", [128, 10, 4])
    lru_cbT = din("lru_cbT", [128, 10])
    lru_w_a = din("lru_w_a", [1, 2, 10, 128, 128])
    lru_w_x = din("lru_w_x", [1, 2, 10, 128, 128])
    lru_baT = din("lru_baT", [128, 2, 10])
    lru_bxT = din("lru_bxT", [128, 2, 10])
    lru_lamT = din("lru_lamT", [128, 2, 10])
    lru_w_out = din("lru_w_out", [1, LRU_W, D])
    pool_w_in = din("pool_w_in", [1, D, D])
    pool_w_grp = din("pool_w_grp", [1, 4, 256, 256])
    pool_scale = din("pool_scale", [1, D])
    pool_edge = din("pool_edge", [128, 4, 2, 8])
    moe_router = din("moe_router", [NL, D, 20])
    moe_w1r = din("moe_w1r", [NL, N_EXP * 128 * 2, 2048])
    moe_w3r = din("moe_w3r", [NL, N_EXP * 128 * 2, 2048])
    moe_w2r = din("moe_w2r", [NL, N_EXP * 128 * 2, 2048])
    normg_raw = din("normg_raw", [NL, 2, D])
    utri_d = din("utri", [128, 128])
    tvals_d = din("tvals", [128, 40])
    pidx_d = din("pidx", [128, 1])
    NSLOT = 34 * 512
    xg = nc.dram_tensor("xg", [NSLOT, D], BF16, kind="Internal").ap()
    yg = nc.dram_tensor("yg", [NSLOT, D], F32, kind="Internal").ap()
    out = nc.dram_tensor("out", [4096, D], F32, kind="ExternalOutput").ap()
    if dbg:
        outc = nc.dram_tensor("outc", [512, D], F32, kind="ExternalOutput").ap()
    res = nc.dram_tensor("res", [4608, D], F32, kind="Internal").ap()
    mods_d = nc.dram_tensor("mods_d", [NL, 144, 128], F32, kind="Internal").ap()

    ident32 = P.sbuf([128, 128], F32, "sb_ident32")
    identb = P.sbuf([128, 128], BF16, "sb_identb")
    modsT = P.sbuf([128, NL, 3, 48], F32, "sb_modsT")
    normgT = P.sbuf([128, NL, 2, 8], F32, "sb_normgT")
    Amod = P.sbuf([128, 2, 3, 8], F32, "sb_Amod")
    cm05 = P.sbuf([128, 16], F32, "sb_cm05")
    t_const = Tok("const")
    t_mods = Tok("modsT")
    t_amod = Tok("amod")
    t_modsd = [Tok(f"modsd{l}") for l in range(NL)]
    psum = P.psum([128, 8, 512], F32, "ps_all")
    t_ps = [Tok(f"psb{i}") for i in range(8)]
    ar = Arena(P, 196 * 1024)
    t_res = [[Tok(f"res{b}_{i}") for i in range(NTILE)] for b in range(2)]

    dumps = {}

    def dump(name, ap, reads):
        if not dbg or name in dumps:
            return
        shp = list(ap.shape)
        d = nc.dram_tensor("dump_" + name, shp, F32, kind="ExternalOutput").ap()
        dumps[name] = d
        P.dma("pool", d, ap, reads=reads, allow_slow_non_contiguous=True)

    def bank(i):
        return psum[:, i, :]

    def bankb(i):
        return psum[:, i, :].bitcast(BF16)

    def rows(b, i):
        if i < 16:
            return b * S + i * 128
        return 4096 + b * CT + (i - 16) * 128

    def mset(b, i):
        return b if i < 16 else 2

    def vec_op(eng):
        return "dve" if eng == 0 else "pool"

    P.dma("sp", ident32[:], ident_d, writes=[t_const])
    P.op("dve", lambda e: e.tensor_copy(out=identb[:], in_=ident32[:]), reads=[t_const], writes=[t_const])
    P.op("dve", lambda e: e.memset(cm05[:], -0.5), writes=[t_const])
    P.dma("sp", normgT[:], normgT_d, writes=[t_const])
    for b in range(2):
        for i in range(NTILE):
            r0 = rows(b, i)
            P.dma("sp", res[r0:r0 + 128, :], resin[r0:r0 + 128, :], writes=[t_res[b][i]])

    def phase_ada():
        ar.off = 0
        scT = ar.alloc([8, 3], F32)
        t_sc = Tok("scT")
        P.dma("sp", scT, cT_d.rearrange("(k p) s -> p k s", p=128), writes=[t_sc])
        P.op("act", lambda e: e.activation(out=scT, in_=scT, func=AF.Silu), reads=[t_sc], writes=[t_sc])
        badaT = ar.alloc([NL, 48], F32)
        t_b = Tok("bada")
        P.dma("sp", badaT, badaT_d, writes=[t_b])
        NW = 3
        Wt = [ar.alloc([8, 512], F32) for _ in range(NW)]
        t_W = [Tok(f"W{i}") for i in range(NW)]
        tmpT = ar.alloc([128], F32)
        t_tmp = Tok("tmpT")
        wi = 0
        for l in range(NL):
            if l not in layers:
                continue
            pb = l % 2
            wsrc = w_ada[l].rearrange("(k p) n -> p k n", p=128)
            for nch in range(12):
                s = wi % NW
                wi += 1
                P.dma("sp", Wt[s], wsrc[:, :, nch * 512:(nch + 1) * 512], writes=[t_W[s]])
                for cc in range(4):
                    c = nch * 4 + cc
                    for k in range(KD):
                        P.op("pe", lambda e, s=s, k=k, cc=cc, c=c, pb=pb: e.matmul(
                            psum[:, pb, c:c + 97:48], lhsT=Wt[s][:, k, cc * 128:(cc + 1) * 128],
                            rhs=scT[:, k, :], start=(k == 0), stop=(k == KD - 1)),
                            reads=[t_W[s], t_sc], writes=[t_ps[pb]])
            P.op("dve", lambda e, l=l, pb=pb: e.tensor_tensor(
                out=modsT[:, l], in0=psum[:, pb, 0:144].rearrange("p (s c) -> p s c", c=48),
                in1=bc(badaT[:, l:l + 1, :], [128, 3, 48]), op=ALU.add),
                reads=[t_ps[pb], t_b], writes=[t_mods])
            src = modsT[:, l].rearrange("p s c -> p (s c)")
            for (r0, r1) in ((0, 128), (128, 144)):
                n = r1 - r0
                P.op("pe", lambda e, r0=r0, r1=r1, n=n, src=src: e.transpose(
                    out=psum[0:n, 2, 0:128], in_=src[:, r0:r1], identity=ident32[:]),
                    reads=[t_mods, t_const], writes=[t_ps[2]])
                P.op("act", lambda e, n=n: e.activation(out=tmpT[0:n, :], in_=psum[0:n, 2, 0:128], func=AF.Copy),
                     reads=[t_ps[2]], writes=[t_tmp])
                P.dma("sp", mods_d[l, r0:r1, :], tmpT[0:n, :], reads=[t_tmp], writes=[t_modsd[l]])
            dump("modsT", modsT[:, l], [t_mods])
        P.barrier()

    def layer_setup(l):
        for n in range(2):
            for s in range(3):
                j = 1 + 3 * n
                P.op("dve", lambda e, n=n, s=s, j=j: e.scalar_tensor_tensor(
                    out=Amod[:, n, s, :], in0=modsT[:, l, s, j * 8:(j + 1) * 8], scalar=1.0,
                    in1=normgT[:, l, n, :], op0=ALU.add, op1=ALU.mult),
                    reads=[t_mods, t_const], writes=[t_amod])

    def Bmod(l, n, s):
        j = 3 * n
        return modsT[:, l, s, j * 8:(j + 1) * 8]

    def load_gate(l, which, s, dst, tok):
        r0 = s * 48 + which * 8
        src = mods_d[l, r0:r0 + 8, :]
        a = AP(src.tensor, src.offset, [[0, 128], [1, 1024]])
        P.dma("sp", dst, a, reads=[t_modsd[l]], writes=[tok])

    class NormBufs:
        def __init__(self, want32):
            self.NX = 3
            self.xt = [ar.alloc([D], F32) for _ in range(self.NX)]
            self.t_xt = [Tok() for _ in range(self.NX)]
            self.junk = ar.alloc([D], BF16)
            self.t_junk = Tok()
            self.st = [ar.alloc([4], F32) for _ in range(self.NX)]
            self.t_st = [Tok() for _ in range(self.NX)]
            self.xh = [ar.alloc([D], F32) for _ in range(2)]
            self.t_xh = [Tok() for _ in range(2)]
            self.cnt = 0
            if want32:
                self.u32 = [ar.alloc([KD, 128], F32) for _ in range(2)]
                self.t_u32 = [Tok() for _ in range(2)]

    def norm_tile(nb, l, n, b, i, uT, t_uT, col, want32=False):
        c = nb.cnt
        nb.cnt += 1
        sx = c % nb.NX
        s2 = c % 2
        xt, st, xh = nb.xt[sx], nb.st[sx], nb.xh[s2]
        r0 = rows(b, i)
        s = mset(b, i)
        P.dma("sp", xt, res[r0:r0 + 128, :], reads=[t_res[b][i]], writes=[nb.t_xt[sx]])
        P.op("act", lambda e: e.activation(out=nb.junk, in_=xt, func=AF.Square, accum_out=st[:, 0:1]),
             reads=[nb.t_xt[sx]], writes=[nb.t_junk, nb.t_st[sx]])
        P.op("dve", lambda e: e.tensor_scalar(out=st[:, 1:2], in0=st[:, 0:1], scalar1=1.0 / D, scalar2=EPS,
                                              op0=ALU.mult, op1=ALU.add),
             reads=[nb.t_st[sx]], writes=[nb.t_st[sx]])
        P.op("pool", lambda e: e.tensor_tensor(out=st[:, 2:3], in0=st[:, 1:2], in1=cm05[:, 0:1], op=ALU.pow),
             reads=[nb.t_st[sx], t_const], writes=[nb.t_st[sx]])
        P.op("act", lambda e: e.activation(out=xh, in_=xt, func=AF.Copy, scale=st[:, 2:3]),
             reads=[nb.t_xt[sx], nb.t_st[sx]], writes=[nb.t_xh[s2]])
        pb0 = 2 * (c % 2)
        for k in range(KD):
            pb = pb0 + k // 4
            P.op("pe", lambda e, k=k, pb=pb: e.transpose(
                out=psum[:, pb, (k % 4) * 128:(k % 4 + 1) * 128], in_=xh[:, k * 128:(k + 1) * 128],
                identity=ident32[:]), reads=[nb.t_xh[s2], t_const], writes=[t_ps[pb]])
        A = Amod[:, n, s, :]
        B = Bmod(l, n, s)
        if want32:
            u32 = nb.u32[s2]
            for k in range(KD):
                pb = pb0 + k // 4
                P.op("dve", lambda e, k=k, pb=pb: e.tensor_scalar(
                    out=u32[:, k, :], in0=psum[:, pb, (k % 4) * 128:(k % 4 + 1) * 128],
                    scalar1=A[:, k:k + 1], scalar2=B[:, k:k + 1], op0=ALU.mult, op1=ALU.add),
                    reads=[t_ps[pb], t_amod, t_mods], writes=[nb.t_u32[s2]])
            P.op("act", lambda e: e.activation(out=uT[:, :, col:col + 128], in_=u32, func=AF.Copy),
                 reads=[nb.t_u32[s2]], writes=[t_uT])
            return u32, nb.t_u32[s2]
        for k in range(KD):
            pb = pb0 + k // 4
            src = psum[:, pb, (k % 4) * 128:(k % 4 + 1) * 128]
            dst = uT[:, k, col:col + 128]
            if k % 2 == 0:
                P.op("act", lambda e, k=k, src=src, dst=dst: e.activation(
                    out=dst, in_=src, func=AF.Identity, scale=A[:, k:k + 1], bias=B[:, k:k + 1]),
                    reads=[t_ps[pb], t_amod, t_mods], writes=[t_uT])
            else:
                P.op("dve", lambda e, k=k, src=src, dst=dst: e.tensor_scalar(
                    out=dst, in0=src, scalar1=A[:, k:k + 1], scalar2=B[:, k:k + 1], op0=ALU.mult, op1=ALU.add),
                    reads=[t_ps[pb], t_amod, t_mods], writes=[t_uT])
        return None, None

    class ResUpd:
        def __init__(self):
            self.N = 3
            self.xt = [ar.alloc([D], F32) for _ in range(self.N)]
            self.t_xt = [Tok() for _ in range(self.N)]
            self.tmp = [ar.alloc([D], F32) for _ in range(2)]
            self.t_tmp = [Tok() for _ in range(2)]
            self.cnt = 0

        def prefetch(self, b, i):
            sx = self.cnt % self.N
            r0 = rows(b, i)
            P.dma("sp", self.xt[sx], res[r0:r0 + 128, :], reads=[t_res[b][i]], writes=[self.t_xt[sx]])

        def apply(self, b, i, ysrc, yreads, G, t_G, dst=None):
            sx = self.cnt % self.N
            s2 = self.cnt % 2
            self.cnt += 1
            xt, tmp = self.xt[sx], self.tmp[s2]
            for (ap_in, c0, n) in ysrc:
                P.op("dve", lambda e, ap_in=ap_in, c0=c0, n=n: e.tensor_tensor(
                    out=tmp[:, c0:c0 + n], in0=ap_in, in1=G[:, c0:c0 + n], op=ALU.mult),
                    reads=list(yreads) + [t_G], writes=[self.t_tmp[s2]])
            P.op("pool", lambda e: e.tensor_tensor(out=xt, in0=xt, in1=tmp, op=ALU.add),
                 reads=[self.t_tmp[s2], self.t_xt[sx]], writes=[self.t_xt[sx]])
            r0 = rows(b, i)
            if dst is None:
                P.dma("sp", res[r0:r0 + 128, :], xt, reads=[self.t_xt[sx]], writes=[t_res[b][i]])
            else:
                P.dma("sp", dst, xt, reads=[self.t_xt[sx]], writes=[t_res[b][i]])

    def attn_layer(l, b, need_ctx):
        j = l // 3
        lam_init = 0.8 - 0.6 * math.exp(-0.3 * l)
        ar.off = 0
        qT = ar.alloc([KD, TB], BF16)
        kT = ar.alloc([KD, TB], BF16)
        vx = ar.alloc([NTILE, 8, 130], BF16)
        t_qT, t_kT, t_vx = Tok("qT"), Tok("kT"), Tok("vx")
        small = ar.alloc([16], F32)
        t_small = Tok("small")
        mark = ar.off
        uT = ar.alloc([KD, TB], BF16)
        t_uT = Tok("uT")
        mark1 = ar.off
        nb = NormBufs(False)
        layer_setup(l)
        for i in range(NTILE):
            norm_tile(nb, l, 0, b, i, uT, t_uT, i * 128)
        dump("uT", uT, [t_uT])
        P.barrier()
        ar.off = mark1
        P.op("pool", lambda e: e.memset(vx[:, :, :, 128:130], 1.0), writes=[t_vx])
        cs_t = ar.alloc([2, NTILE, 32], F32)
        t_cs = Tok("cs")
        P.dma("sp", cs_t[:, 0], rope_cos, writes=[t_cs])
        P.dma("sp", cs_t[:, 1], rope_sin, writes=[t_cs])
        gt = ar.alloc([2, 512], F32)
        t_gt = Tok("gt")
        for qk, src in enumerate((attn_qg, attn_kg)):
            a = src[j:j + 1, :]
            P.dma("sp", gt[:, qk, :].rearrange("p (a b) -> p a b", b=64),
                  AP(a.tensor, a.offset, [[0, 128], [0, 8], [1, 64]]), writes=[t_gt])
        lq = ar.alloc([256], F32)
        t_lq = Tok("lq")
        a = attn_lam[j:j + 1, :]
        P.dma("sp", lq, AP(a.tensor, a.offset, [[0, 128], [1, 256]]), writes=[t_lq])
        lqj = ar.alloc([128], F32)
        P.op("dve", lambda e: e.tensor_tensor(out=lqj[:, 0:64], in0=lq[:, 0:64], in1=lq[:, 64:128], op=ALU.mult),
             reads=[t_lq], writes=[t_lq])
        P.op("dve", lambda e: e.tensor_tensor(out=lqj[:, 64:128], in0=lq[:, 128:192], in1=lq[:, 192:256], op=ALU.mult),
             reads=[t_lq], writes=[t_lq])
        P.op("dve", lambda e: e.tensor_reduce(out=small[:, 0:2], in_=lqj.rearrange("p (a b) -> p a b", b=64),
                                              axis=AX.X, op=ALU.add), reads=[t_lq], writes=[t_small])
        P.op("act", lambda e: e.activation(out=small[:, 2:4], in_=small[:, 0:2], func=AF.Exp),
             reads=[t_small], writes=[t_small])
        P.op("dve", lambda e: e.tensor_tensor(out=small[:, 4:5], in0=small[:, 3:4], in1=small[:, 2:3], op=ALU.subtract),
             reads=[t_small], writes=[t_small])
        P.op("dve", lambda e: e.tensor_scalar(out=small[:, 5:6], in0=small[:, 4:5], scalar1=-lam_init, scalar2=None,
                                              op0=ALU.add), reads=[t_small], writes=[t_small])
        neglam = small[:, 5:6]
        P.dma("sp", small[:, 8:10], attn_sgT, writes=[t_small])
        P.op("dve", lambda e: e.tensor_scalar(out=small[:, 6:7], in0=small[:, 8 + j:9 + j], scalar1=1.0 - lam_init,
                                              scalar2=None, op0=ALU.mult), reads=[t_small], writes=[t_small])
        sgs = small[:, 6:7]

        NWB = 2
        wch = [ar.alloc([KD, 512], BF16) for _ in range(NWB)]
        t_wch = [Tok() for _ in range(NWB)]
        qs = [ar.alloc([512], F32) for _ in range(2)]
        ta = [ar.alloc([512], F32) for _ in range(2)]
        sq = ta
        qn = qs
        qr = [ar.alloc([512], BF16) for _ in range(2)]
        stt = [ar.alloc([32], F32) for _ in range(2)]
        t_q = [Tok() for _ in range(2)]
        wsrc = attn_w_in[j].rearrange("(k p) n -> p k n", p=128)
        cnt = 0
        for nch in range(6):
            ws = nch % NWB
            P.dma("pool", wch[ws], wsrc[:, :, nch * 512:(nch + 1) * 512], writes=[t_wch[ws]])
            for i in range(NTILE):
                pb = 4 + (cnt % 2)
                s2 = cnt % 2
                cnt += 1
                for k in range(KD):
                    P.op("pe", lambda e, k=k, pb=pb, ws=ws, i=i: e.matmul(
                        bank(pb), lhsT=uT[:, k, i * 128:(i + 1) * 128], rhs=wch[ws][:, k, :],
                        start=(k == 0), stop=(k == KD - 1)), reads=[t_uT, t_wch[ws]], writes=[t_ps[pb]])
                if nch >= 4:
                    h0 = (nch - 4) * 4
                    P.op("act", lambda e, pb=pb, i=i, h0=h0: e.activation(
                        out=vx[:, i, h0:h0 + 4, 0:128], in_=bank(pb).rearrange("p (h d) -> p h d", d=128),
                        func=AF.Copy), reads=[t_ps[pb]], writes=[t_vx])
                    continue
                qk = nch // 2
                h0 = (nch % 2) * 4
                tq = t_q[s2]
                P.op("act", lambda e, pb=pb, s2=s2: e.activation(out=qs[s2], in_=bank(pb), func=AF.Copy),
                     reads=[t_ps[pb]], writes=[tq])
                P.op("act", lambda e, pb=pb, s2=s2: e.activation(out=sq[s2], in_=bank(pb), func=AF.Square),
                     reads=[t_ps[pb]], writes=[tq])
                P.op("dve", lambda e, s2=s2: e.tensor_reduce(
                    out=stt[s2][:, 0:8], in_=sq[s2].rearrange("p (a b) -> p a b", b=64), axis=AX.X, op=ALU.add),
                    reads=[tq], writes=[tq])
                P.op("dve", lambda e, s2=s2: e.tensor_scalar(
                    out=stt[s2][:, 8:16], in0=stt[s2][:, 0:8], scalar1=1.0 / 64, scalar2=EPS, op0=ALU.mult, op1=ALU.add),
                    reads=[tq], writes=[tq])
                P.op("pool", lambda e, s2=s2: e.tensor_tensor(
                    out=stt[s2][:, 16:24], in0=stt[s2][:, 8:16], in1=cm05[:, 0:8], op=ALU.pow),
                    reads=[tq, t_const], writes=[tq])
                P.op("dve", lambda e, s2=s2: e.tensor_tensor(
                    out=qn[s2].rearrange("p (a b) -> p a b", b=64), in0=qs[s2].rearrange("p (a b) -> p a b", b=64),
                    in1=bc(stt[s2][:, 16:24].unsqueeze(2), [128, 8, 64]), op=ALU.mult),
                    reads=[tq], writes=[tq])
                P.op("pool", lambda e, s2=s2, qk=qk: e.tensor_tensor(out=qn[s2], in0=qn[s2], in1=gt[:, qk, :], op=ALU.mult),
                     reads=[tq, t_gt], writes=[tq])
                x4 = qn[s2].rearrange("p (a m d) -> p a m d", m=2, d=32)
                t4 = ta[s2].rearrange("p (a m d) -> p a m d", m=2, d=32)
                r4 = qr[s2].rearrange("p (a m d) -> p a m d", m=2, d=32)

                def tb(ti, i=i):
                    return bc(cs_t[:, (0, 1, 1, 0)[ti], i, :].unsqueeze(1), [128, 8, 32])
                P.op("pool", lambda e, x4=x4, t4=t4, tb=tb: e.tensor_tensor(out=t4[:, :, 0, :], in0=x4[:, :, 0, :], in1=tb(0), op=ALU.mult),
                     reads=[tq, t_cs], writes=[tq])
                P.op("pool", lambda e, x4=x4, t4=t4, tb=tb: e.tensor_tensor(out=t4[:, :, 1, :], in0=x4[:, :, 1, :], in1=tb(1), op=ALU.mult),
                     reads=[tq, t_cs], writes=[tq])
                P.op("dve", lambda e, t4=t4, r4=r4: e.tensor_tensor(out=r4[:, :, 0, :], in0=t4[:, :, 0, :], in1=t4[:, :, 1, :], op=ALU.subtract),
                     reads=[tq], writes=[tq])
                P.op("pool", lambda e, x4=x4, t4=t4, tb=tb: e.tensor_tensor(out=t4[:, :, 0, :], in0=x4[:, :, 0, :], in1=tb(2), op=ALU.mult),
                     reads=[tq, t_cs], writes=[tq])
                P.op("pool", lambda e, x4=x4, t4=t4, tb=tb: e.tensor_tensor(out=t4[:, :, 1, :], in0=x4[:, :, 1, :], in1=tb(3), op=ALU.mult),
                     reads=[tq, t_cs], writes=[tq])
                P.op("dve", lambda e, t4=t4, r4=r4: e.tensor_tensor(out=r4[:, :, 1, :], in0=t4[:, :, 0, :], in1=t4[:, :, 1, :], op=ALU.add),
                     reads=[tq], writes=[tq])
                pbt = 6 + (cnt % 2)
                for hh in range(4):
                    P.op("pe", lambda e, hh=hh, pbt=pbt, s2=s2: e.transpose(
                        out=bankb(pbt)[:, hh * 128:(hh + 1) * 128], in_=qr[s2][:, hh * 128:(hh + 1) * 128],
                        identity=identb[:]), reads=[tq, t_const], writes=[t_ps[pbt]])
                dstT = (qT if qk == 0 else kT)
                P.op("act", lambda e, pbt=pbt, dstT=dstT, h0=h0, i=i: e.activation(
                    out=dstT[:, h0:h0 + 4, i * 128:(i + 1) * 128],
                    in_=bankb(pbt)[:, 0:512].rearrange("p (h t) -> p h t", t=128), func=AF.Copy),
                    reads=[t_ps[pbt]], writes=[t_qT if qk == 0 else t_kT])
        dump("qT", qT, [t_qT])
        dump("kT", kT, [t_kT])
        dump("vx", vx, [t_vx])
        P.barrier()
        ar.off = mark
        wout = ar.alloc([KD, D], BF16)
        t_wout = Tok("wout")
        P.dma("pool", wout, attn_w_out[j].rearrange("(k p) n -> p k n", p=128), writes=[t_wout])
        G1 = [ar.alloc([D], F32) for _ in range(2)]
        t_G1 = [Tok(), Tok()]
        load_gate(l, 2, b, G1[0], t_G1[0])
        load_gate(l, 2, 2, G1[1], t_G1[1])
        ru = ResUpd()
        osb = ar.alloc([4, D], F32)
        t_osb = Tok("osb")
        es = [[ar.alloc([512], BF16) for _ in range(2)] for _ in range(2)]
        t_es = [[Tok(), Tok()], [Tok(), Tok()]]
        fin = ar.alloc([8], F32)
        t_fin = Tok("fin")
        tsb = ar.alloc([128], F32)
        sqt = ar.alloc([D], F32)
        onb = ar.alloc([D], BF16)
        oT = ar.alloc([KD, 128], BF16)
        t_on = Tok("on")
        t_oT = Tok("oT")
        s8 = ar.alloc([32], F32)

        qranges = [(q0, 512, list(range(NTILE))) for q0 in range(0, S, 512)]
        if need_ctx:
            qranges.append((S, CT, [16, 17]))
        def head(h, q0, nq, ktiles, nqi):
            if True:
                def s_mm(ki):
                    kt = ktiles[ki]
                    for m in range(2):
                        pb = m * 2 + (ki % 2)
                        P.op("pe", lambda e, m=m, pb=pb, kt=kt: e.matmul(
                            psum[:, pb, 0:nq], lhsT=kT[m * 64:(m + 1) * 64, h, kt * 128:(kt + 1) * 128],
                            rhs=qT[m * 64:(m + 1) * 64, h, q0:q0 + nq], start=True, stop=True),
                            reads=[t_kT, t_qT], writes=[t_ps[pb]])
                        P.op("act", lambda e, m=m, pb=pb, ki=ki: e.activation(
                            out=es[m][ki % 2][:, 0:nq], in_=psum[:, pb, 0:nq], func=AF.Exp, scale=0.125),
                            reads=[t_ps[pb]], writes=[t_es[m][ki % 2]])
                s_mm(0)
                for ki in range(len(ktiles)):
                    if ki + 1 < len(ktiles):
                        s_mm(ki + 1)
                    kt = ktiles[ki]
                    for m in range(2):
                        for qi in range(nqi):
                            gi = m * 4 + qi
                            pb = 4 + gi // 3
                            c0 = (gi % 3) * 129
                            P.op("pe", lambda e, m=m, qi=qi, pb=pb, c0=c0, kt=kt, ki=ki, gi=gi: e.matmul(
                                psum[:, pb, c0:c0 + 129], lhsT=es[m][ki % 2][:, qi * 128:(qi + 1) * 128],
                                rhs=vx[:, kt, h, 0:129], start=(ki == 0 and (gi % 3 == 0 or (nqi < 4 and qi == 0))),
                                stop=(ki == len(ktiles) - 1), skip_group_check=True),
                                reads=[t_es[m][ki % 2], t_vx], writes=[t_ps[pb]])
                for qi in range(nqi):
                    g0, g1 = qi, 4 + qi
                    pb0, c00 = 4 + g0 // 3, (g0 % 3) * 129
                    pb1, c01 = 4 + g1 // 3, (g1 % 3) * 129
                    P.op("dve", lambda e, pb0=pb0, c00=c00: e.reciprocal(out=fin[:, 0:1], in_=psum[:, pb0, c00 + 128:c00 + 129]),
                         reads=[t_ps[pb0]], writes=[t_fin])
                    P.op("dve", lambda e, pb1=pb1, c01=c01: e.reciprocal(out=fin[:, 1:2], in_=psum[:, pb1, c01 + 128:c01 + 129]),
                         reads=[t_ps[pb1]], writes=[t_fin])
                    P.op("dve", lambda e: e.tensor_tensor(out=fin[:, 2:3], in0=fin[:, 1:2], in1=neglam, op=ALU.mult),
                         reads=[t_fin, t_small], writes=[t_fin])
                    P.op("dve", lambda e, pb1=pb1, c01=c01: e.tensor_scalar(
                        out=tsb, in0=psum[:, pb1, c01:c01 + 128], scalar1=fin[:, 2:3], scalar2=None, op0=ALU.mult),
                        reads=[t_ps[pb1], t_fin], writes=[t_fin])
                    P.op("dve", lambda e, pb0=pb0, c00=c00, qi=qi: e.scalar_tensor_tensor(
                        out=osb[:, qi, h * 128:(h + 1) * 128], in0=psum[:, pb0, c00:c00 + 128], scalar=fin[:, 0:1],
                        in1=tsb, op0=ALU.mult, op1=ALU.add), reads=[t_ps[pb0], t_fin], writes=[t_osb])
        for (q0, nq, ktiles) in qranges:
            nqi = nq // 128
            for h in range(8):
                head(h, q0, nq, ktiles, nqi)
            dump("osb", osb, [t_osb])
            for qi in range(nqi):
                i = q0 // 128 + qi
                ru.prefetch(b, i)
                P.op("dve", lambda e, qi=qi: e.tensor_tensor(out=sqt, in0=osb[:, qi, :], in1=osb[:, qi, :], op=ALU.mult),
                     reads=[t_osb], writes=[t_on])
                P.op("dve", lambda e: e.tensor_reduce(out=s8[:, 0:8], in_=sqt.rearrange("p (a b) -> p a b", b=128),
                                                      axis=AX.X, op=ALU.add), reads=[t_on], writes=[t_on])
                P.op("dve", lambda e: e.tensor_scalar(out=s8[:, 8:16], in0=s8[:, 0:8], scalar1=1.0 / 128, scalar2=EPS,
                                                      op0=ALU.mult, op1=ALU.add), reads=[t_on], writes=[t_on])
                P.op("pool", lambda e: e.tensor_tensor(out=s8[:, 16:24], in0=s8[:, 8:16], in1=cm05[:, 0:8], op=ALU.pow),
                     reads=[t_on, t_const], writes=[t_on])
                P.op("dve", lambda e, qi=qi: e.tensor_tensor(
                    out=onb.rearrange("p (a b) -> p a b", b=128), in0=osb[:, qi, :].rearrange("p (a b) -> p a b", b=128),
                    in1=bc(s8[:, 16:24].unsqueeze(2), [128, 8, 128]), op=ALU.mult), reads=[t_on, t_osb], writes=[t_on])
                for hh in range(8):
                    P.op("pe", lambda e, hh=hh: e.transpose(
                        out=bankb(7)[:, hh * 128:(hh + 1) * 128], in_=onb[:, hh * 128:(hh + 1) * 128],
                        identity=identb[:]), reads=[t_on, t_const], writes=[t_ps[7]])
                P.op("act", lambda e: e.activation(out=oT, in_=bankb(7).rearrange("p (h t) -> p h t", t=128),
                                                   func=AF.Copy, scale=sgs), reads=[t_ps[7], t_small], writes=[t_oT])
                for nh in range(2):
                    for hh in range(8):
                        P.op("pe", lambda e, nh=nh, hh=hh: e.matmul(
                            bank(nh), lhsT=oT[:, hh, :], rhs=wout[:, hh, nh * 512:(nh + 1) * 512],
                            start=(hh == 0), stop=(hh == 7)), reads=[t_oT, t_wout], writes=[t_ps[nh]])
                gs = 0 if i < 16 else 1
                ru.apply(b, i, [(bank(0), 0, 512), (bank(1), 512, 512)], [t_ps[0], t_ps[1]], G1[gs], t_G1[gs])
        P.barrier()

    def lru_layer(l, b, need_ctx):
        ar.off = 0
        layer_setup(l)
        uT = ar.alloc([KD, TB], BF16)
        t_uT = Tok("uT")
        mark0 = ar.off
        nb = NormBufs(False)
        for i in range(NTILE):
            norm_tile(nb, l, 0, b, i, uT, t_uT, i * 128)
        P.barrier()
        ar.off = mark0
        mT = ar.alloc([10, TB], BF16)
        t_mT = Tok("mT")
        cw = ar.alloc([10, 4], F32)
        cb = ar.alloc([10], F32)
        bax = ar.alloc([2, 2, 10], F32)
        lam = ar.alloc([2, 10], F32)
        nsp = ar.alloc([2, 2, 10], F32)
        t_c = Tok("lruc")
        P.dma("sp", cw, lru_cwT, writes=[t_c])
        P.dma("sp", cb, lru_cbT, writes=[t_c])
        P.dma("sp", bax[:, 0], lru_baT, writes=[t_c])
        P.dma("sp", bax[:, 1], lru_bxT, writes=[t_c])
        P.dma("sp", lam, lru_lamT, writes=[t_c])
        P.op("act", lambda e: e.activation(out=lam, in_=lam, func=AF.Exp, scale=-1.0), reads=[t_c], writes=[t_c])
        P.op("act", lambda e: e.activation(out=lam, in_=lam, func=AF.Ln, bias=1.0), reads=[t_c], writes=[t_c])
        P.op("dve", lambda e: e.tensor_scalar(out=nsp[:, 0], in0=lam, scalar1=-8.0, scalar2=None, op0=ALU.mult),
             reads=[t_c], writes=[t_c])
        P.op("dve", lambda e: e.tensor_scalar(out=nsp[:, 1], in0=lam, scalar1=-16.0, scalar2=None, op0=ALU.mult),
             reads=[t_c], writes=[t_c])
        NWB = 2
        win = [ar.alloc([KD, 256], BF16) for _ in range(NWB)]
        t_win = [Tok() for _ in range(NWB)]
        wbd = [ar.alloc([4, 128], BF16) for _ in range(NWB)]
        t_wbd = [Tok() for _ in range(NWB)]
        xr = ar.alloc([LW], F32)
        xc = ar.alloc([LW], F32)
        xcb = ar.alloc([LW], BF16)
        Abuf = ar.alloc([LW], F32)
        Bbuf = ar.alloc([LW], F32)
        Tbuf = ar.alloc([LW], F32)
        H0 = ar.alloc([LW], F32)
        gate = ar.alloc([TB], BF16)
        t_x = Tok("lrux")
        t_A, t_B, t_T, t_H0, t_g = Tok(), Tok(), Tok(), Tok(), Tok()
        P.op("pool", lambda e: e.memset(xr, 0.0), writes=[t_x])
        wsrc = lru_w_in[0].rearrange("(k p) n -> p k n", p=128)
        segs = [(LAT0 + q, q, 512) for q in range(0, S, 512)] + [(CTX0, S, CT)]
        cnt = 0
        for g in range(10):
            ws = g % NWB
            P.dma("pool", win[ws][:, :, 0:128], wsrc[:, :, g * 128:(g + 1) * 128], writes=[t_win[ws]])
            P.dma("pool", win[ws][:, :, 128:256], wsrc[:, :, LRU_W + g * 128:LRU_W + (g + 1) * 128], writes=[t_win[ws]])
            for d in range(2):
                P.dma("pool", wbd[ws][:, 2 * d, :], lru_w_a[0, d, g], writes=[t_wbd[ws]])
                P.dma("pool", wbd[ws][:, 2 * d + 1, :], lru_w_x[0, d, g], writes=[t_wbd[ws]])
            for (pc, tc, n) in segs:
                for half in range(2):
                    pb = cnt % 4
                    cnt += 1
                    for k in range(KD):
                        P.op("pe", lambda e, k=k, pb=pb, half=half, tc=tc, n=n, ws=ws: e.matmul(
                            psum[:, pb, 0:n], lhsT=win[ws][:, k, half * 128:(half + 1) * 128],
                            rhs=uT[:, k, tc:tc + n], start=(k == 0), stop=(k == KD - 1)),
                            reads=[t_uT, t_win[ws]], writes=[t_ps[pb]])
                    if half == 0:
                        P.op("act", lambda e, pb=pb, tc=tc, n=n: e.activation(
                            out=gate[:, tc:tc + n], in_=psum[:, pb, 0:n], func=AF.Gelu_apprx_tanh),
                            reads=[t_ps[pb]], writes=[t_g])
                    else:
                        P.op("act", lambda e, pb=pb, pc=pc, n=n: e.activation(
                            out=xr[:, pc:pc + n], in_=psum[:, pb, 0:n], func=AF.Copy),
                            reads=[t_ps[pb]], writes=[t_x])
            W0, W1 = 2, 2309
            nW = W1 - W0
            P.op("dve", lambda e, g=g: e.tensor_scalar(
                out=xc[:, W0:W1], in0=xr[:, W0 - 2:W1 - 2], scalar1=cw[:, g, 0:1], scalar2=cb[:, g:g + 1],
                op0=ALU.mult, op1=ALU.add), reads=[t_x, t_c], writes=[t_x])
            for k in range(1, 4):
                P.op("dve", lambda e, g=g, k=k: e.scalar_tensor_tensor(
                    out=xc[:, W0:W1], in0=xr[:, W0 - 2 + k:W1 - 2 + k], scalar=cw[:, g, k:k + 1], in1=xc[:, W0:W1],
                    op0=ALU.mult, op1=ALU.add), reads=[t_x, t_c], writes=[t_x])
            P.op("pool", lambda e: e.tensor_copy(out=xcb[:, W0:W1], in_=xc[:, W0:W1]), reads=[t_x], writes=[t_x])
            for d in range(2):
                for (pc, tc, n) in segs:
                    for ax in range(2):
                        pb = 4 + cnt % 4
                        cnt += 1
                        P.op("pe", lambda e, pb=pb, pc=pc, n=n, ws=ws, d=d, ax=ax: e.matmul(
                            psum[:, pb, 0:n], lhsT=wbd[ws][:, 2 * d + ax, :], rhs=xcb[:, pc:pc + n],
                            start=True, stop=True), reads=[t_x, t_wbd[ws]], writes=[t_ps[pb]])
                        dstb = Abuf if ax == 0 else Bbuf
                        P.op("act", lambda e, pb=pb, pc=pc, n=n, dstb=dstb, ax=ax, d=d, g=g: e.activation(
                            out=dstb[:, pc:pc + n], in_=psum[:, pb, 0:n], func=AF.Sigmoid,
                            bias=bax[:, ax, d, g:g + 1]), reads=[t_ps[pb], t_c], writes=[t_A if ax == 0 else t_B])
                P.op("pool", lambda e: e.tensor_tensor(out=Bbuf[:, W0:W1], in0=Bbuf[:, W0:W1], in1=xc[:, W0:W1], op=ALU.mult),
                     reads=[t_B, t_x], writes=[t_B])
                P.op("act", lambda e, d=d, g=g: e.activation(out=Tbuf[:, W0:W1], in_=Abuf[:, W0:W1], func=AF.Exp,
                                                             scale=nsp[:, 1, d, g:g + 1]), reads=[t_A, t_c], writes=[t_T])
                P.op("act", lambda e: e.activation(out=Tbuf[:, W0:W1], in_=Tbuf[:, W0:W1], func=AF.Sqrt, scale=-1.0, bias=1.0),
                     reads=[t_T], writes=[t_T])
                rc = CTX0 if d == 0 else CTX0 + CT - 1
                P.op("dve", lambda e, rc=rc: e.memset(Tbuf[:, rc:rc + 1], 1.0), writes=[t_T])
                P.op("dve", lambda e: e.tensor_tensor(out=Bbuf[:, W0:W1], in0=Bbuf[:, W0:W1], in1=Tbuf[:, W0:W1], op=ALU.mult),
                     reads=[t_B, t_T], writes=[t_B])
                P.op("act", lambda e, d=d, g=g: e.activation(out=Abuf[:, W0:W1], in_=Abuf[:, W0:W1], func=AF.Exp,
                                                             scale=nsp[:, 0, d, g:g + 1]), reads=[t_A, t_c], writes=[t_A])
                Hd = H0 if d == 0 else Tbuf
                t_Hd = t_H0 if d == 0 else t_T
                csl = slice(CTX0, CTX0 + CT)
                lsl = slice(LAT0, LAT0 + S)
                if d == 0:
                    P.op("dve", lambda e, Hd=Hd: e.tensor_tensor_scan(
                        out=Hd[:, csl], data0=Abuf[:, csl], data1=Bbuf[:, csl], initial=0.0, op0=ALU.mult, op1=ALU.add),
                        reads=[t_A, t_B], writes=[t_Hd])
                    P.op("dve", lambda e, Hd=Hd: e.tensor_tensor_scan(
                        out=Hd[:, lsl], data0=Abuf[:, lsl], data1=Bbuf[:, lsl], initial=Hd[:, CTX0 + CT - 1:CTX0 + CT],
                        op0=ALU.mult, op1=ALU.add), reads=[t_A, t_B, t_Hd], writes=[t_Hd])
                else:
                    P.op("dve", lambda e, Hd=Hd: e.tensor_tensor_scan(
                        out=rev2d(Hd[:, csl]), data0=rev2d(Abuf[:, csl]), data1=rev2d(Bbuf[:, csl]), initial=0.0,
                        op0=ALU.mult, op1=ALU.add), reads=[t_A, t_B], writes=[t_Hd])
                    P.op("dve", lambda e, Hd=Hd: e.tensor_tensor_scan(
                        out=rev2d(Hd[:, lsl]), data0=rev2d(Abuf[:, lsl]), data1=rev2d(Bbuf[:, lsl]),
                        initial=Hd[:, CTX0:CTX0 + 1], op0=ALU.mult, op1=ALU.add), reads=[t_A, t_B, t_Hd], writes=[t_Hd])
            P.op("pool", lambda e: e.tensor_tensor(out=H0[:, W0:W1], in0=H0[:, W0:W1], in1=Tbuf[:, W0:W1], op=ALU.add),
                 reads=[t_H0, t_T], writes=[t_H0])
            P.op("dve", lambda e, g=g: e.tensor_tensor(out=mT[:, g, 0:S], in0=H0[:, LAT0:LAT0 + S], in1=gate[:, 0:S], op=ALU.mult),
                 reads=[t_H0, t_g], writes=[t_mT])
            P.op("dve", lambda e, g=g: e.tensor_tensor(out=mT[:, g, S:TB], in0=H0[:, CTX0:CTX0 + CT], in1=gate[:, S:TB], op=ALU.mult),
                 reads=[t_H0, t_g], writes=[t_mT])
        P.barrier()
        ar.off = mark0 + 10 * TB * 2
        wout = ar.alloc([10, D], BF16)
        t_wout = Tok("wout")
        P.dma("pool", wout, lru_w_out[0].rearrange("(k p) n -> p k n", p=128), writes=[t_wout])
        G1 = [ar.alloc([D], F32) for _ in range(2)]
        t_G1 = [Tok(), Tok()]
        load_gate(l, 2, b, G1[0], t_G1[0])
        load_gate(l, 2, 2, G1[1], t_G1[1])
        ru = ResUpd()
        ntl = NTILE if need_ctx else 16
        for i in range(ntl):
            ru.prefetch(b, i)
            pbs = (2 * (i % 2), 2 * (i % 2) + 1)
            for nh in range(2):
                for g in range(10):
                    P.op("pe", lambda e, nh=nh, g=g, i=i, pbs=pbs: e.matmul(
                        bank(pbs[nh]), lhsT=mT[:, g, i * 128:(i + 1) * 128], rhs=wout[:, g, nh * 512:(nh + 1) * 512],
                        start=(g == 0), stop=(g == 9)), reads=[t_mT, t_wout], writes=[t_ps[pbs[nh]]])
            gs = 0 if i < 16 else 1
            ru.apply(b, i, [(bank(pbs[0]), 0, 512), (bank(pbs[1]), 512, 512)], [t_ps[pbs[0]], t_ps[pbs[1]]],
                     G1[gs], t_G1[gs])
        P.barrier()

    def pool_layer(l, b, need_ctx):
        ar.off = 0
        layer_setup(l)
        uT = ar.alloc([KD, TB], BF16)
        t_uT = Tok("uT")
        mark0 = ar.off
        nb = NormBufs(False)
        for i in range(NTILE):
            norm_tile(nb, l, 0, b, i, uT, t_uT, i * 128)
        P.barrier()
        ar.off = mark0
        pT = ar.alloc([KD, TB], BF16)
        t_pT = Tok("pT")
        edge = ar.alloc([4, 2, 8], F32)
        t_e = Tok("edge")
        P.dma("sp", edge, pool_edge, writes=[t_e])
        NWB = 2
        win = [ar.alloc([KD, 128], BF16) for _ in range(NWB)]
        t_win = [Tok() for _ in range(NWB)]
        PADW = 8
        XL = [ar.alloc([S + 16], F32) for _ in range(3)]
        XC = [ar.alloc([CT + 16], F32) for _ in range(3)]
        t_X = Tok("poolx")
        for t in XL + XC:
            P.op("pool", lambda e, t=t: e.memset(t, 0.0), writes=[t_X])
        wsrc = pool_w_in[0].rearrange("(k p) n -> p k n", p=128)
        cnt = 0
        for c in range(8):
            gidx = c // 2
            w = (2, 4, 8, 16)[gidx]
            ws = c % NWB
            P.dma("pool", win[ws], wsrc[:, :, c * 128:(c + 1) * 128], writes=[t_win[ws]])
            for (tc, n, X, off) in [(q, 512, XL, q) for q in range(0, S, 512)] + [(S, CT, XC, 0)]:
                pb = cnt % 4
                cnt += 1
                for k in range(KD):
                    P.op("pe", lambda e, k=k, pb=pb, tc=tc, n=n, ws=ws: e.matmul(
                        psum[:, pb, 0:n], lhsT=win[ws][:, k, :], rhs=uT[:, k, tc:tc + n],
                        start=(k == 0), stop=(k == KD - 1)), reads=[t_uT, t_win[ws]], writes=[t_ps[pb]])
                P.op("act", lambda e, pb=pb, n=n, X=X, off=off: e.activation(
                    out=X[0][:, PADW + off:PADW + off + n], in_=psum[:, pb, 0:n], func=AF.Copy),
                    reads=[t_ps[pb]], writes=[t_X])
            for (X, n, tc) in ((XL, S, 0), (XC, CT, S)):
                x0 = X[0]
                cur, other = X[1], X[2]
                lo, hi = 1, PADW + n + 8
                P.op("dve", lambda e, x0=x0, cur=cur, lo=lo, hi=hi: e.tensor_tensor(
                    out=cur[:, lo:hi], in0=x0[:, lo - 1:hi - 1], in1=x0[:, lo:hi], op=ALU.add), reads=[t_X], writes=[t_X])
                step = 1
                ww = 2
                while ww < w:
                    sp_ = ww // 2
                    lo, hi = lo + sp_, hi - sp_
                    P.op("dve", lambda e, cur=cur, other=other, lo=lo, hi=hi, sp_=sp_: e.tensor_tensor(
                        out=other[:, lo:hi], in0=cur[:, lo - sp_:hi - sp_], in1=cur[:, lo + sp_:hi + sp_], op=ALU.add),
                        reads=[t_X], writes=[t_X])
                    cur, other = other, cur
                    ww *= 2
                P.op("dve", lambda e, cur=cur, x0=x0, n=n, tc=tc, c=c, w=w: e.scalar_tensor_tensor(
                    out=pT[:, c, tc:tc + n], in0=cur[:, PADW:PADW + n], scalar=1.0 / w, in1=x0[:, PADW:PADW + n],
                    op0=ALU.mult, op1=ALU.subtract), reads=[t_X], writes=[t_pT])
                hw = w // 2
                for side in range(2):
                    a0 = PADW if side == 0 else PADW + n - 8
                    o0 = tc if side == 0 else tc + n - 8
                    P.op("dve", lambda e, cur=cur, a0=a0, side=side, gidx=gidx, other=other: e.tensor_tensor(
                        out=other[:, 0:8], in0=cur[:, a0:a0 + 8], in1=edge[:, gidx, side, :], op=ALU.mult),
                        reads=[t_X, t_e], writes=[t_X])
                    P.op("dve", lambda e, x0=x0, a0=a0, o0=o0, c=c, other=other: e.tensor_tensor(
                        out=pT[:, c, o0:o0 + 8], in0=other[:, 0:8], in1=x0[:, a0:a0 + 8], op=ALU.subtract),
                        reads=[t_X], writes=[t_pT])
        P.barrier()
        ar.off = mark0 + KD * TB * 2
        wg = ar.alloc([4, 2, 256], BF16)
        t_wg = Tok("wg")
        for g in range(4):
            P.dma("pool", wg[:, g], pool_w_grp[0, g].rearrange("(k p) n -> p k n", p=128), writes=[t_wg])
        G1 = [ar.alloc([D], F32) for _ in range(2)]
        t_G1 = [Tok(), Tok()]
        load_gate(l, 2, b, G1[0], t_G1[0])
        load_gate(l, 2, 2, G1[1], t_G1[1])
        psc = ar.alloc([D], F32)
        t_psc = Tok("psc")
        a = pool_scale[0:1, :]
        P.dma("sp", psc, AP(a.tensor, a.offset, [[0, 128], [1, D]]), writes=[t_psc])
        for s in range(2):
            P.op("dve", lambda e, s=s: e.tensor_tensor(out=G1[s], in0=G1[s], in1=psc, op=ALU.mult),
                 reads=[t_psc, t_G1[s]], writes=[t_G1[s]])
        ru = ResUpd()
        ntl = NTILE if need_ctx else 16
        for i in range(ntl):
            ru.prefetch(b, i)
            pbs = (2 * (i % 2), 2 * (i % 2) + 1)
            for g in range(4):
                pb = pbs[g // 2]
                c0 = (g % 2) * 256
                for kc in range(2):
                    P.op("pe", lambda e, g=g, kc=kc, pb=pb, c0=c0, i=i: e.matmul(
                        psum[:, pb, c0:c0 + 256], lhsT=pT[:, 2 * g + kc, i * 128:(i + 1) * 128], rhs=wg[:, g, kc, :],
                        start=(kc == 0 and g % 2 == 0), stop=(kc == 1), skip_group_check=True),
                        reads=[t_pT, t_wg], writes=[t_ps[pb]])
            gs = 0 if i < 16 else 1
            ru.apply(b, i, [(bank(pbs[0]), 0, 512), (bank(pbs[1]), 512, 512)], [t_ps[pbs[0]], t_ps[pbs[1]]],
                     G1[gs], t_G1[gs])
        P.barrier()

    I32 = mybir.dt.int32

    def moe_sparse(l, last):
        tl = [(b, i) for b in range(2) for i in range(NTILE if not last else 16)]
        NTT = len(tl)
        NTE = NTT * 2 * 128 // 512 + 15
        ar.off = 0
        layer_setup(l)
        S1a = ar.alloc([NTT, 16], F32)
        S2a = ar.alloc([NTT, 16], F32)
        wts = ar.alloc([NTT, 2], F32)
        posa = ar.alloc([NTT, 16], F32)
        base = ar.alloc([16], F32)
        sidx = ar.alloc([NTT, 2], I32)
        widx = ar.alloc([NTE, 2], I32)
        t_S, t_base, t_idx = Tok("S"), Tok("base"), Tok("idx")
        G2 = [ar.alloc([D], F32) for _ in range(3)]
        t_G2 = [Tok() for _ in range(3)]
        for s_ in range(3):
            load_gate(l, 5, s_, G2[s_], t_G2[s_])
        mark_keep = ar.off
        ub = ar.alloc([NTT, D], BF16)
        t_ub = [Tok() for _ in range(NTT)]
        At = [ar.alloc([D], F32) for _ in range(3)]
        Bt = [ar.alloc([D], F32) for _ in range(3)]
        t_AB = Tok("ABtok")
        gn = ar.alloc([D], F32)
        a_ = normg_raw[l, 1:2, :]
        P.dma("sp", gn, AP(a_.tensor, a_.offset, [[0, 128], [1, D]]), writes=[t_AB])
        for s_ in range(3):
            load_gate(l, 4, s_, At[s_], t_AB)
            load_gate(l, 3, s_, Bt[s_], t_AB)
            P.op("dve", lambda e, s_=s_: e.scalar_tensor_tensor(out=At[s_], in0=At[s_], scalar=1.0, in1=gn,
                                                                op0=ALU.add, op1=ALU.mult), reads=[t_AB], writes=[t_AB])
        wr = ar.alloc([KD, 20], F32)
        t_wr = Tok("wr")
        P.dma("sp", wr, moe_router[l].rearrange("(k p) n -> p k n", p=128), writes=[t_wr])
        utri = ar.alloc([128], BF16)
        onesb = ar.alloc([128], BF16)
        cst = ar.alloc([128 + 40 + 1], F32)
        t_cst = Tok("cst")
        P.dma("sp", cst[:, 0:128], utri_d, writes=[t_cst])
        P.dma("sp", cst[:, 128:168], tvals_d, writes=[t_cst])
        P.dma("sp", cst[:, 168:169], pidx_d, writes=[t_cst])
        P.op("dve", lambda e: e.tensor_copy(out=utri, in_=cst[:, 0:128]), reads=[t_cst], writes=[t_cst])
        P.op("dve", lambda e: e.memset(onesb, 1.0), writes=[t_cst])
        P.op("dve", lambda e: e.memset(base, 0.0), writes=[t_base])
        nb = NormBufs(True)
        rt = [ar.alloc([96], F32) for _ in range(2)]
        t_rt = [Tok(), Tok()]
        mb = [ar.alloc([16], BF16) for _ in range(2)]
        utmp = [ar.alloc([D], F32) for _ in range(2)]
        t_ut = [Tok(), Tok()]

        def route_tile(ti, b, i):
            c = nb.cnt
            nb.cnt += 1
            sx, s2 = c % nb.NX, c % 2
            xt, st, xh, u32 = nb.xt[sx], nb.st[sx], nb.xh[s2], nb.u32[s2]
            r0 = rows(b, i)
            s_ = mset(b, i)
            P.dma("sp", xt, res[r0:r0 + 128, :], reads=[t_res[b][i]], writes=[nb.t_xt[sx]])
            P.op("act", lambda e: e.activation(out=nb.junk, in_=xt, func=AF.Square, accum_out=st[:, 0:1]),
                 reads=[nb.t_xt[sx]], writes=[nb.t_junk, nb.t_st[sx]])
            P.op("dve", lambda e: e.tensor_scalar(out=st[:, 1:2], in0=st[:, 0:1], scalar1=1.0 / D, scalar2=EPS,
                                                  op0=ALU.mult, op1=ALU.add), reads=[nb.t_st[sx]], writes=[nb.t_st[sx]])
            P.op("pool", lambda e: e.tensor_tensor(out=st[:, 2:3], in0=st[:, 1:2], in1=cm05[:, 0:1], op=ALU.pow),
                 reads=[nb.t_st[sx], t_const], writes=[nb.t_st[sx]])
            P.op("act", lambda e: e.activation(out=xh, in_=xt, func=AF.Copy, scale=st[:, 2:3]),
                 reads=[nb.t_xt[sx], nb.t_st[sx]], writes=[nb.t_xh[s2]])
            pb0 = 2 * (c % 2)
            for k in range(KD):
                pb = pb0 + k // 4
                P.op("pe", lambda e, k=k, pb=pb: e.transpose(
                    out=psum[:, pb, (k % 4) * 128:(k % 4 + 1) * 128], in_=xh[:, k * 128:(k + 1) * 128],
                    identity=ident32[:]), reads=[nb.t_xh[s2], t_const], writes=[t_ps[pb]])
            A = Amod[:, 1, s_, :]
            B = Bmod(l, 1, s_)
            for k in range(KD):
                pb = pb0 + k // 4
                P.op("dve", lambda e, k=k, pb=pb: e.tensor_scalar(
                    out=u32[:, k, :], in0=psum[:, pb, (k % 4) * 128:(k % 4 + 1) * 128],
                    scalar1=A[:, k:k + 1], scalar2=B[:, k:k + 1], op0=ALU.mult, op1=ALU.add),
                    reads=[t_ps[pb], t_amod, t_mods], writes=[nb.t_u32[s2]])
            P.op("pool", lambda e: e.tensor_tensor(out=utmp[s2], in0=xh, in1=At[s_], op=ALU.mult),
                 reads=[nb.t_xh[s2], t_AB], writes=[t_ut[s2]])
            P.op("pool", lambda e: e.tensor_tensor(out=ub[:, ti, :], in0=utmp[s2], in1=Bt[s_], op=ALU.add),
                 reads=[t_ut[s2], t_AB], writes=[t_ub[ti]])
            R = rt[s2]
            tr = t_rt[s2]
            pb = 4 + s2
            for k in range(KD):
                P.op("pe", lambda e, k=k: e.matmul(
                    psum[:, pb, 0:20], lhsT=u32[:, k, :], rhs=wr[:, k, :], start=(k == 0), stop=(k == KD - 1)),
                    reads=[nb.t_u32[s2], t_wr], writes=[t_ps[pb]])
            P.op("dve", lambda e: e.tensor_copy(out=R[:, 0:20], in_=psum[:, pb, 0:20]), reads=[t_ps[pb]], writes=[tr])
            P.op("dve", lambda e: e.tensor_reduce(out=R[:, 20:21], in_=R[:, 0:4], axis=AX.X, op=ALU.max), reads=[tr], writes=[tr])
            P.op("dve", lambda e: e.tensor_scalar(out=R[:, 21:22], in0=R[:, 20:21], scalar1=-1.0, scalar2=None, op0=ALU.mult), reads=[tr], writes=[tr])
            P.op("dve", lambda e: e.tensor_scalar(out=R[:, 24:28], in0=R[:, 0:4], scalar1=R[:, 20:21], scalar2=None, op0=ALU.is_equal), reads=[tr], writes=[tr])
            P.op("act", lambda e: e.activation(out=R[:, 28:32], in_=R[:, 0:4], func=AF.Exp, bias=R[:, 21:22], accum_out=R[:, 32:33]), reads=[tr], writes=[tr])
            P.op("dve", lambda e: e.reciprocal(out=R[:, 33:34], in_=R[:, 32:33]), reads=[tr], writes=[tr])
            P.op("dve", lambda e: e.tensor_tensor(
                out=R[:, 36:52].rearrange("p (g j) -> p g j", j=4), in0=R[:, 4:20].rearrange("p (g j) -> p g j", j=4),
                in1=bc(R[:, 24:28].unsqueeze(2), [128, 4, 4]), op=ALU.mult), reads=[tr], writes=[tr])
            P.op("dve", lambda e: e.tensor_reduce(out=R[:, 52:56], in_=R[:, 36:52].rearrange("p (g j) -> p j g", j=4),
                                                  axis=AX.X, op=ALU.add), reads=[tr], writes=[tr])
            P.op("dve", lambda e: e.tensor_reduce(out=R[:, 56:57], in_=R[:, 52:56], axis=AX.X, op=ALU.max), reads=[tr], writes=[tr])
            P.op("dve", lambda e: e.tensor_scalar(out=R[:, 57:58], in0=R[:, 56:57], scalar1=-1.0, scalar2=None, op0=ALU.mult), reads=[tr], writes=[tr])
            P.op("dve", lambda e: e.tensor_scalar(out=R[:, 60:64], in0=R[:, 52:56], scalar1=R[:, 56:57], scalar2=None, op0=ALU.is_equal), reads=[tr], writes=[tr])
            P.op("dve", lambda e: e.scalar_tensor_tensor(out=R[:, 64:68], in0=R[:, 60:64], scalar=-1e30, in1=R[:, 52:56],
                                                         op0=ALU.mult, op1=ALU.add), reads=[tr], writes=[tr])
            P.op("dve", lambda e: e.tensor_reduce(out=R[:, 68:69], in_=R[:, 64:68], axis=AX.X, op=ALU.max), reads=[tr], writes=[tr])
            P.op("dve", lambda e: e.tensor_scalar(out=R[:, 72:76], in0=R[:, 64:68], scalar1=R[:, 68:69], scalar2=None, op0=ALU.is_equal), reads=[tr], writes=[tr])
            P.op("act", lambda e: e.activation(out=R[:, 76:77], in_=R[:, 68:69], func=AF.Exp, bias=R[:, 57:58]), reads=[tr], writes=[tr])
            P.op("dve", lambda e: e.tensor_scalar(out=R[:, 77:78], in0=R[:, 76:77], scalar1=1.0, scalar2=None, op0=ALU.add), reads=[tr], writes=[tr])
            P.op("dve", lambda e: e.reciprocal(out=R[:, 78:79], in_=R[:, 77:78]), reads=[tr], writes=[tr])
            P.op("dve", lambda e: e.tensor_tensor(out=wts[:, ti, 0:1], in0=R[:, 78:79], in1=R[:, 33:34], op=ALU.mult), reads=[tr], writes=[tr, t_S])
            P.op("dve", lambda e: e.tensor_tensor(out=wts[:, ti, 1:2], in0=wts[:, ti, 0:1], in1=R[:, 76:77], op=ALU.mult), reads=[tr, t_S], writes=[t_S])
            P.op("dve", lambda e: e.tensor_tensor(
                out=S1a[:, ti, :].rearrange("p (g j) -> p g j", j=4), in0=bc(R[:, 24:28].unsqueeze(2), [128, 4, 4]),
                in1=bc(R[:, 60:64].unsqueeze(1), [128, 4, 4]), op=ALU.mult), reads=[tr], writes=[t_S])
            P.op("dve", lambda e: e.tensor_tensor(
                out=S2a[:, ti, :].rearrange("p (g j) -> p g j", j=4), in0=bc(R[:, 24:28].unsqueeze(2), [128, 4, 4]),
                in1=bc(R[:, 72:76].unsqueeze(1), [128, 4, 4]), op=ALU.mult), reads=[tr], writes=[t_S])
            P.op("dve", lambda e: e.tensor_tensor(out=mb[s2], in0=S1a[:, ti, :], in1=S2a[:, ti, :], op=ALU.add),
                 reads=[t_S], writes=[tr])
            pbp = 6 + s2
            P.op("pe", lambda e: e.matmul(psum[:, pbp, 0:16], lhsT=utri, rhs=mb[s2], start=True, stop=True),
                 reads=[tr, t_cst], writes=[t_ps[pbp]])
            P.op("pe", lambda e: e.matmul(psum[:, pbp, 16:32], lhsT=onesb, rhs=mb[s2], start=False, stop=True, skip_group_check=True),
                 reads=[tr, t_cst], writes=[t_ps[pbp]])
            P.op("dve", lambda e: e.tensor_tensor(out=posa[:, ti, :], in0=psum[:, pbp, 0:16], in1=base, op=ALU.add),
                 reads=[t_ps[pbp], t_base], writes=[t_S])
            P.op("dve", lambda e: e.tensor_tensor(out=base, in0=psum[:, pbp, 16:32], in1=base, op=ALU.add),
                 reads=[t_ps[pbp], t_base], writes=[t_base])

        for ti, (b, i) in enumerate(tl):
            route_tile(ti, b, i)
        sg = ar.alloc([96], F32)
        sgi = ar.alloc([16], I32)
        t_sg = Tok("sg")
        P.op("dve", lambda e: e.tensor_scalar(out=sg[:, 0:16], in0=base, scalar1=511.0, scalar2=None, op0=ALU.add),
             reads=[t_base], writes=[t_sg])
        P.op("dve", lambda e: e.tensor_copy(out=sgi, in_=sg[:, 0:16]), reads=[t_sg], writes=[t_sg])
        P.op("dve", lambda e: e.tensor_scalar(out=sgi, in0=sgi, scalar1=9, scalar2=9, op0=ALU.arith_shift_right,
                                              op1=ALU.logical_shift_left), reads=[t_sg], writes=[t_sg])
        P.op("dve", lambda e: e.tensor_copy(out=sg[:, 0:16], in_=sgi), reads=[t_sg], writes=[t_sg])
        P.op("dve", lambda e: e.memset(sg[:, 48:64], 1.0), writes=[t_sg])
        P.op("dve", lambda e: e.tensor_tensor_scan(out=sg[:, 16:32], data0=sg[:, 48:64], data1=sg[:, 0:16], initial=0.0,
                                                   op0=ALU.mult, op1=ALU.add), reads=[t_sg], writes=[t_sg])
        P.op("dve", lambda e: e.tensor_tensor(out=sg[:, 32:48], in0=sg[:, 16:32], in1=sg[:, 0:16], op=ALU.subtract),
             reads=[t_sg], writes=[t_sg])
        big = ar.alloc([NTT, 16], F32)
        slf = ar.alloc([NTT, 2], F32)
        P.op("dve", lambda e: e.tensor_tensor(out=posa, in0=posa, in1=bc(sg[:, 32:48].unsqueeze(1), [128, NTT, 16]), op=ALU.add),
             reads=[t_S, t_sg], writes=[t_S])
        for k_, Sk in enumerate((S1a, S2a)):
            P.op("dve", lambda e, Sk=Sk: e.tensor_tensor(out=big, in0=posa, in1=Sk, op=ALU.mult), reads=[t_S], writes=[t_sg])
            P.op("dve", lambda e, k_=k_: e.tensor_reduce(out=slf[:, :, k_], in_=big, axis=AX.X, op=ALU.add), reads=[t_sg], writes=[t_sg])
        P.op("dve", lambda e: e.tensor_copy(out=sidx, in_=slf), reads=[t_sg], writes=[t_idx])
        cmp = ar.alloc([NTE, 16], F32)
        etf = ar.alloc([NTE, 4], F32)
        P.op("dve", lambda e: e.tensor_tensor(out=cmp, in0=bc(sg[:, 16:32].unsqueeze(1), [128, NTE, 16]),
                                              in1=bc(cst[:, 128:128 + NTE].unsqueeze(2), [128, NTE, 16]), op=ALU.is_le),
             reads=[t_sg, t_cst], writes=[t_sg])
        P.op("dve", lambda e: e.tensor_reduce(out=etf[:, :, 0], in_=cmp, axis=AX.X, op=ALU.add), reads=[t_sg], writes=[t_sg])
        P.op("dve", lambda e: e.tensor_scalar(out=etf[:, :, 1], in0=etf[:, :, 0], scalar1=15.0, scalar2=256.0, op0=ALU.min, op1=ALU.mult),
             reads=[t_sg], writes=[t_sg])
        P.op("dve", lambda e: e.tensor_scalar(out=etf[:, :, 1], in0=etf[:, :, 1], scalar1=float(l * N_EXP * 256), scalar2=None, op0=ALU.add),
             reads=[t_sg], writes=[t_sg])
        P.op("dve", lambda e: e.scalar_tensor_tensor(out=etf[:, :, 2], in0=bc(cst[:, 168:169], [128, NTE]), scalar=2.0, in1=etf[:, :, 1],
                                                     op0=ALU.mult, op1=ALU.add), reads=[t_sg, t_cst], writes=[t_sg])
        P.op("dve", lambda e: e.tensor_scalar(out=etf[:, :, 3], in0=etf[:, :, 2], scalar1=1.0, scalar2=None, op0=ALU.add),
             reads=[t_sg], writes=[t_sg])
        P.op("dve", lambda e: e.tensor_copy(out=widx, in_=etf[:, :, 2:4]), reads=[t_sg], writes=[t_idx])
        t_xg = Tok("xg")
        for ti in range(NTT):
            for k_ in range(2):
                P.op("pool", lambda e, ti=ti, k_=k_: e.indirect_dma_start(
                    out=xg, out_offset=bass.IndirectOffsetOnAxis(ap=sidx[:, ti, k_:k_ + 1], axis=0),
                    in_=ub[:, ti, :], in_offset=None), reads=[t_ub[ti], t_idx], writes=[t_xg], dma=True)
        dump("sidx", slf, [t_sg])
        dump("etf", etf[:, :, 0], [t_sg])
        dump("wts", wts, [t_S])
        P.barrier()
        ar.off = mark_keep
        NWB = 2
        w1 = [ar.alloc([KD, DE], BF16) for _ in range(NWB)]
        w3 = [ar.alloc([KD, DE], BF16) for _ in range(NWB)]
        w2 = [ar.alloc([4, D], BF16) for _ in range(NWB)]
        t_w1 = [Tok() for _ in range(NWB)]
        t_w3 = [Tok() for _ in range(NWB)]
        t_w2 = [Tok() for _ in range(NWB)]
        xtok = [ar.alloc([D], BF16) for _ in range(4)]
        t_xtok = [Tok() for _ in range(4)]
        xT = [ar.alloc([KD, 512], BF16) for _ in range(2)]
        t_xT = [Tok(), Tok()]
        hT = [ar.alloc([4, 512], BF16) for _ in range(2)]
        t_hT = [Tok(), Tok()]
        sl = [ar.alloc([512], F32) for _ in range(2)]
        t_sl = [Tok(), Tok()]
        ysb = [ar.alloc([D], F32) for _ in range(3)]
        t_ysb = [Tok() for _ in range(3)]
        t_yg = Tok("yg")
        w1r = moe_w1r.rearrange("l r n -> (l r) n")
        w3r = moe_w3r.rearrange("l r n -> (l r) n")
        w2r = moe_w2r.rearrange("l r n -> (l r) n")
        cnt = 0
        ycnt = 0
        xcnt = 0
        for t in range(NTE):
            ws = t % NWB
            for (wt, wsrc, tw) in ((w1, w1r, t_w1), (w3, w3r, t_w3), (w2, w2r, t_w2)):
                for hf in range(2):
                    dst = wt[ws].rearrange("p a b -> p (a b)")[:, hf * 2048:(hf + 1) * 2048]
                    P.op("pool", lambda e, dst=dst, wsrc=wsrc, t=t, hf=hf: e.indirect_dma_start(
                        out=dst, out_offset=None, in_=wsrc,
                        in_offset=bass.IndirectOffsetOnAxis(ap=widx[:, t, hf:hf + 1], axis=0)),
                        reads=[t_idx], writes=[tw[ws]], dma=True)
            xs_ = t % 2
            for i4 in range(4):
                xi = xcnt % 4
                xcnt += 1
                r0 = t * 512 + i4 * 128
                P.dma("sp", xtok[xi], xg[r0:r0 + 128, :], reads=[t_xg], writes=[t_xtok[xi]])
                pbt = i4 % 2
                for k in range(KD):
                    P.op("pe", lambda e, k=k, pbt=pbt, xi=xi: e.transpose(
                        out=bankb(pbt)[:, k * 128:(k + 1) * 128], in_=xtok[xi][:, k * 128:(k + 1) * 128],
                        identity=identb[:]), reads=[t_xtok[xi], t_const], writes=[t_ps[pbt]])
                src_ = bankb(pbt).rearrange("p (k t) -> p k t", t=128)
                dst_ = xT[xs_][:, :, i4 * 128:(i4 + 1) * 128]
                if i4 % 2 == 0:
                    P.op("act", lambda e, src_=src_, dst_=dst_: e.activation(out=dst_, in_=src_, func=AF.Copy),
                         reads=[t_ps[pbt]], writes=[t_xT[xs_]])
                else:
                    P.op("dve", lambda e, src_=src_, dst_=dst_: e.tensor_copy(out=dst_, in_=src_),
                         reads=[t_ps[pbt]], writes=[t_xT[xs_]])
            hs = t % 2
            for hc in range(4):
                pb1 = 2 + (cnt % 2) * 2
                pb3 = pb1 + 1
                s2 = cnt % 2
                cnt += 1
                for (wt, pb, twt) in ((w1, pb1, t_w1), (w3, pb3, t_w3)):
                    for k in range(KD):
                        P.op("pe", lambda e, wt=wt, pb=pb, k=k, hc=hc, ws=ws, xs_=xs_: e.matmul(
                            bank(pb), lhsT=wt[ws][:, k, hc * 128:(hc + 1) * 128], rhs=xT[xs_][:, k, :],
                            start=(k == 0), stop=(k == KD - 1)), reads=[t_xT[xs_], twt[ws]], writes=[t_ps[pb]])
                P.op("act", lambda e, pb1=pb1, s2=s2: e.activation(out=sl[s2], in_=bank(pb1), func=AF.Silu),
                     reads=[t_ps[pb1]], writes=[t_sl[s2]])
                P.op("dve", lambda e, pb3=pb3, s2=s2, hs=hs, hc=hc: e.tensor_tensor(
                    out=hT[hs][:, hc, :], in0=bank(pb3), in1=sl[s2], op=ALU.mult),
                    reads=[t_ps[pb3], t_sl[s2]], writes=[t_hT[hs]])
            for tq in range(4):
                yi = ycnt % 3
                ycnt += 1
                for nh in range(2):
                    pb = 6 + nh
                    for hc in range(4):
                        P.op("pe", lambda e, pb=pb, hc=hc, tq=tq, nh=nh, hs=hs, ws=ws: e.matmul(
                            bank(pb), lhsT=hT[hs][:, hc, tq * 128:(tq + 1) * 128], rhs=w2[ws][:, hc, nh * 512:(nh + 1) * 512],
                            start=(hc == 0), stop=(hc == 3)), reads=[t_hT[hs], t_w2[ws]], writes=[t_ps[pb]])
                    if nh == 0:
                        P.op("act", lambda e, pb=pb, yi=yi: e.activation(out=ysb[yi][:, 0:512], in_=bank(pb), func=AF.Copy),
                             reads=[t_ps[pb]], writes=[t_ysb[yi]])
                    else:
                        P.op("dve", lambda e, pb=pb, yi=yi: e.tensor_copy(out=ysb[yi][:, 512:1024], in_=bank(pb)),
                             reads=[t_ps[pb]], writes=[t_ysb[yi]])
                r0 = t * 512 + tq * 128
                P.dma("sp", yg[r0:r0 + 128, :], ysb[yi], reads=[t_ysb[yi]], writes=[t_yg])
        P.barrier()
        ar.off = mark_keep
        ru = ResUpd()
        yk = [[ar.alloc([D], F32) for _ in range(2)] for _ in range(2)]
        t_yk = [[Tok(), Tok()], [Tok(), Tok()]]
        yc = [ar.alloc([D], F32) for _ in range(2)]
        t_yc = [Tok(), Tok()]
        for ti, (b, i) in enumerate(tl):
            s2 = ti % 2
            ru.prefetch(b, i)
            for k_ in range(2):
                P.op("pool", lambda e, ti=ti, k_=k_, s2=s2: e.indirect_dma_start(
                    out=yk[s2][k_], out_offset=None, in_=yg,
                    in_offset=bass.IndirectOffsetOnAxis(ap=sidx[:, ti, k_:k_ + 1], axis=0)),
                    reads=[t_idx, t_yg], writes=[t_yk[s2][k_]], dma=True)
            P.op("dve", lambda e, ti=ti, s2=s2: e.tensor_scalar(out=yc[s2], in0=yk[s2][0], scalar1=wts[:, ti, 0:1], scalar2=None,
                                                                  op0=ALU.mult), reads=[t_yk[s2][0], t_S], writes=[t_yc[s2]])
            P.op("dve", lambda e, ti=ti, s2=s2: e.scalar_tensor_tensor(out=yc[s2], in0=yk[s2][1], scalar=wts[:, ti, 1:2], in1=yc[s2],
                                                                         op0=ALU.mult, op1=ALU.add),
                 reads=[t_yk[s2][1], t_S, t_yc[s2]], writes=[t_yc[s2]])
            gs = mset(b, i)
            dst = None
            if last and not dbg:
                r0 = rows(b, i)
                dst = out[r0:r0 + 128, :]
            ru.apply(b, i, [(yc[s2], 0, D)], [t_yc[s2]], G2[gs], t_G2[gs], dst=dst)
        P.barrier()

    phase_ada()
    last_layer = max(layers)
    for l in layers:
        need_ctx = l < NL - 1
        kind = l % 3
        for b in range(2):
            if do_mixer:
                if kind == 0:
                    attn_layer(l, b, need_ctx)
                elif kind == 1:
                    lru_layer(l, b, need_ctx)
                else:
                    pool_layer(l, b, need_ctx)
        if do_moe:
            moe_sparse(l, not need_ctx)
    if dbg:
        for b in range(2):
            for i in range(NTILE):
                r0 = rows(b, i)
                dst = out[r0:r0 + 128, :] if i < 16 else outc[r0 - 4096:r0 - 4096 + 128, :]
                P.dma("sp", dst, res[r0:r0 + 128, :], reads=[t_res[b][i]], writes=[t_res[b][i]])
    fin_reads = [t_res[b][i] for b in range(2) for i in range(NTILE)]
    P.op("sp", lambda e: e.nop(), reads=fin_reads)
    P.barrier()
    P.emit()
    return nc, P


def _fm(v, inner=None):
    v = np.asarray(v, np.float32)
    lead = v.shape[:-1]
    K = v.shape[-1] // 128
    v = v.reshape(*lead, K, 128)
    return np.ascontiguousarray(np.moveaxis(v, -1, 0))


def _wr(w, K):
    L, E, KP, N = w.shape
    w = w.reshape(L, E, K, 128, N).transpose(0, 1, 3, 2, 4)
    return np.ascontiguousarray(w).reshape(L, E * 128 * 2, K * N // 2)


def _rope_tables():
    t = np.arange(S)
    row = (t // 64).astype(np.float32)
    col = (t % 64).astype(np.float32)
    inv = (10000.0 ** (-np.arange(16, dtype=np.float32) / 16)).astype(np.float32)
    ang = np.concatenate([row[:, None] * inv, col[:, None] * inv], axis=-1)
    cos = np.ones((TB, 32), np.float32)
    sin = np.zeros((TB, 32), np.float32)
    cos[:S] = np.cos(ang)
    sin[:S] = np.sin(ang)
    cos = np.ascontiguousarray(cos.reshape(NTILE, 128, 32).transpose(1, 0, 2))
    sin = np.ascontiguousarray(sin.reshape(NTILE, 128, 32).transpose(1, 0, 2))
    return cos, sin


def _pool_edges():
    e = np.zeros((128, 4, 2, 8), np.float32)
    for g, w in enumerate((2, 4, 8, 16)):
        for n in (S,):
            pass
        hw = w // 2
        for jx in range(8):
            t = jx
            cnt = min(t + hw, 10 ** 9) - max(t - hw, 0)
            e[:, g, 0, jx] = 1.0 / cnt
            d = 8 - jx
            cnt = min(hw, d) + hw
            e[:, g, 1, jx] = 1.0 / cnt
    return e


def prep_inputs(inputs, core, resin_override=None):
    f = lambda k: np.ascontiguousarray(np.asarray(inputs[k], np.float32))
    b0, b1 = 2 * core, 2 * core + 1
    x, ctx, c = f("x"), f("ctx"), f("c")
    if resin_override is None:
        resin = np.concatenate([x[b0], x[b1], ctx[b0], ctx[b1]], axis=0)
    else:
        resin = resin_override
    cT = np.ascontiguousarray(np.stack([c[b0], c[b1], f("c_ctx")], axis=1))
    cos, sin = _rope_tables()
    m = {
        "resin": np.ascontiguousarray(resin),
        "cT": cT,
        "w_ada": f("w_ada"),
        "badaT": np.ascontiguousarray(_fm(f("b_ada"))),
        "normgT": np.ascontiguousarray(_fm(f("norm_g"))),
        "ident": np.eye(128, dtype=np.float32),
        "attn_w_in": f("attn_w_in"),
        "attn_q_gain": f("attn_q_gain"),
        "attn_k_gain": f("attn_k_gain"),
        "attn_lam": f("attn_lam").reshape(2, 256),
        "attn_sgT": np.ascontiguousarray(f("attn_sub_gain").T),
        "attn_w_out": f("attn_w_out"),
        "rope_cos": cos,
        "rope_sin": sin,
        "lru_w_in": f("lru_w_in"),
        "lru_cwT": np.ascontiguousarray(_fm(f("lru_conv_w")[0]).transpose(0, 2, 1)),
        "lru_cbT": np.ascontiguousarray(_fm(f("lru_conv_b")[0])),
        "lru_w_a": f("lru_w_a"),
        "lru_w_x": f("lru_w_x"),
        "lru_baT": np.ascontiguousarray(_fm(f("lru_b_a")[0])),
        "lru_bxT": np.ascontiguousarray(_fm(f("lru_b_x")[0])),
        "lru_lamT": np.ascontiguousarray(_fm(f("lru_lam")[0])),
        "lru_w_out": f("lru_w_out"),
        "pool_w_in": f("pool_w_in"),
        "pool_w_grp": f("pool_w_grp"),
        "pool_scale": f("pool_scale"),
        "pool_edge": _pool_edges(),
        "moe_router": np.ascontiguousarray(np.concatenate([f("moe_router_g"), f("moe_router_e")], axis=-1)),
        "moe_w1r": _wr(f("moe_w1"), 8),
        "moe_w3r": _wr(f("moe_w3"), 8),
        "moe_w2r": _wr(f("moe_w2"), 4),
        "normg_raw": f("norm_g"),
        "utri": np.triu(np.ones((128, 128), np.float32), 1),
        "tvals": np.broadcast_to((np.arange(40, dtype=np.float32) * 512.0)[None, :], (128, 40)).copy(),
        "pidx": np.arange(128, dtype=np.float32).reshape(128, 1),
    }
    return m


_CACHE = {}


def kernel(**inputs):
    if "nc" not in _CACHE:
        _CACHE["nc"] = build()[0]
    nc = _CACHE["nc"]
    shared = prep_inputs(inputs, 0)
    x, ctx, c = (np.asarray(inputs[k], np.float32) for k in ("x", "ctx", "c"))
    cc = np.asarray(inputs["c_ctx"], np.float32)
    in_maps = []
    for core in range(8):
        m = dict(shared)
        b0, b1 = 2 * core, 2 * core + 1
        m["resin"] = np.ascontiguousarray(np.concatenate([x[b0], x[b1], ctx[b0], ctx[b1]], axis=0))
        m["cT"] = np.ascontiguousarray(np.stack([c[b0], c[b1], cc], axis=1))
        in_maps.append(m)
    res = run_bass_kernel_spmd(nc, in_maps, core_ids=list(range(8)))
    outs = [r["out"].reshape(2, S, D) for r in res.results]
    return np.concatenate(outs, axis=0).astype(np.float32)
```

```python
import contextlib
import math
import numpy as np
import concourse.bass as bass
import concourse.mybir as mybir
from concourse.ap import AP
from concourse.bass_utils import run_bass_kernel_spmd

F32 = mybir.dt.float32
BF16 = mybir.dt.bfloat16
AF = mybir.ActivationFunctionType
ALU = mybir.AluOpType
AX = mybir.AxisListType

N_DMA_SEMS = 12
SAME_ENGINE_SYNC = True


class Tok:
    __slots__ = ("name", "last_w", "readers")

    def __init__(self, name=""):
        self.name = name
        self.last_w = None
        self.readers = []


class Op:
    __slots__ = ("eng", "fn", "deps", "signal", "val", "is_dma", "slot")

    def __init__(self, eng, fn, is_dma):
        self.eng = eng
        self.fn = fn
        self.deps = []
        self.signal = False
        self.val = None
        self.is_dma = is_dma
        self.slot = None


class Prog:
    ENGS = ("pe", "act", "dve", "pool", "sp")

    def __init__(self, nc):
        self.nc = nc
        self.ops = []
        self.stack = contextlib.ExitStack()
        self.n_alloc = 0
        self.bar_idx = 0

    def sbuf(self, shape, dtype, name=None):
        self.n_alloc += 1
        return self.stack.enter_context(self.nc.sbuf_tensor(name or f"sb{self.n_alloc}", list(shape), dtype))

    def psum(self, shape, dtype, name=None):
        self.n_alloc += 1
        return self.stack.enter_context(self.nc.psum_tensor(name or f"ps{self.n_alloc}", list(shape), dtype))

    def op(self, eng, fn, reads=(), writes=(), dma=False):
        o = Op(eng, fn, dma)
        deps = []
        for t in reads:
            if t.last_w is not None:
                deps.append(t.last_w)
        for t in writes:
            if t.last_w is not None:
                deps.append(t.last_w)
            deps.extend(t.readers)
        seen = set()
        for d in deps:
            if id(d) in seen or d is o:
                continue
            seen.add(id(d))
            if (not d.is_dma) and (not dma) and d.eng == eng:
                if eng == "pe" or not SAME_ENGINE_SYNC:
                    continue
            d.signal = True
            o.deps.append(d)
        for t in writes:
            t.last_w = o
            t.readers = []
        for t in reads:
            if t in writes:
                continue
            if not dma:
                t.readers = [r for r in t.readers if r.is_dma or r.eng != eng]
            t.readers.append(o)
        self.ops.append(o)
        return o

    def dma(self, eng, out, in_, reads=(), writes=(), **kw):
        return self.op(eng, lambda e: e.dma_start(out=out, in_=in_, **kw), reads, writes, dma=True)

    def barrier(self):
        last = {}
        dmas = []
        for o in self.ops[self.bar_idx:]:
            if o.is_dma:
                dmas.append(o)
            else:
                last[o.eng] = o
        deps = list(last.values()) + dmas
        first = len(self.ops)
        for e in self.ENGS:
            o = Op(e, lambda eng: eng.nop(), False)
            o.deps = [d for d in deps if d.is_dma or d.eng != e]
            for d in o.deps:
                d.signal = True
            self.ops.append(o)
        self.bar_idx = first

    def emit(self):
        nc = self.nc
        cnt = {e: 0 for e in self.ENGS}
        dcnt = {e: 0 for e in self.ENGS}
        slotcnt = {e: [0] * N_DMA_SEMS for e in self.ENGS}
        for o in self.ops:
            if o.is_dma:
                k = dcnt[o.eng] % N_DMA_SEMS
                dcnt[o.eng] += 1
                o.slot = k
                slotcnt[o.eng][k] += 16
                o.val = slotcnt[o.eng][k]
            elif o.signal:
                cnt[o.eng] += 1
                o.val = cnt[o.eng]
        sems = {}
        for e in self.ENGS:
            if cnt[e] > 0:
                sems[e] = self.stack.enter_context(nc.semaphore(f"s_{e}"))
        dsems = {}
        for e in self.ENGS:
            if dcnt[e] > 0:
                dsems[e] = [self.stack.enter_context(nc.semaphore(f"d_{e}{k}"))
                            for k in range(min(N_DMA_SEMS, dcnt[e]))]
        by_eng = {e: [o for o in self.ops if o.eng == e] for e in self.ENGS}
        self.stats = {e: len(by_eng[e]) for e in self.ENGS}
        self.stats["maxsem"] = dict(cnt)
        nwaits = [0]

        def run(e_name, eng):
            known = {}
            for o in by_eng[e_name]:
                waits = {}
                for d in o.deps:
                    key = ("d", d.eng, d.slot) if d.is_dma else ("c", d.eng)
                    if known.get(key, 0) >= d.val:
                        continue
                    waits[key] = max(waits.get(key, 0), d.val)
                if o.is_dma and o.val > 16:
                    key = ("d", o.eng, o.slot)
                    if known.get(key, 0) < o.val - 16:
                        waits[key] = max(waits.get(key, 0), o.val - 16)
                for key, v in waits.items():
                    s = dsems[key[1]][key[2]] if key[0] == "d" else sems[key[1]]
                    eng.wait_ge(s, v)
                    known[key] = v
                    nwaits[0] += 1
                ins = o.fn(eng)
                if o.is_dma:
                    ins.then_inc(dsems[o.eng][o.slot], 16)
                elif o.signal:
                    ins.then_inc(sems[o.eng], 1)

        with nc.Block() as block:
            if by_eng["sp"]:
                @block.sync
                def _(eng):
                    run("sp", eng)
            if by_eng["pe"]:
                @block.tensor
                def _(eng):
                    run("pe", eng)
            if by_eng["act"]:
                @block.scalar
                def _(eng):
                    run("act", eng)
            if by_eng["dve"]:
                @block.vector
                def _(eng):
                    run("dve", eng)
            if by_eng["pool"]:
                @block.gpsimd
                def _(eng):
                    run("pool", eng)
        self.stats["waits"] = nwaits[0]
        self.stack.close()


class Arena:
    def __init__(self, P, nbytes):
        self.t = P.sbuf([128, nbytes // 4], F32, "arena")
        self.cap = nbytes
        self.off = 0

    def alloc(self, free_shape, dtype):
        n = 1
        for s in free_shape:
            n *= s
        nb = n * (2 if dtype == BF16 else 4)
        nb = (nb + 31) // 32 * 32
        assert self.off + nb <= self.cap, f"arena overflow {self.off}+{nb}>{self.cap}"
        a = self.t[:, self.off // 4:(self.off + nb) // 4]
        self.off += nb
        if dtype != F32:
            a = a.bitcast(dtype)
        a = a[:, 0:n]
        if len(free_shape) == 2:
            a = a.rearrange("p (a b) -> p a b", b=free_shape[1])
        elif len(free_shape) == 3:
            a = a.rearrange("p (a b c) -> p a b c", b=free_shape[1], c=free_shape[2])
        return a


def bc(ap, shape):
    return ap.broadcast_to(list(shape))


def rev2d(ap2d):
    a = ap2d.ap
    n = a[1][1]
    st = a[1][0]
    return AP(ap2d.tensor, ap2d.offset + (n - 1) * st, [list(a[0]), [-st, n]])


D = 1024
KD = 8
S = 2048
CT = 256
NTILE = 18
TB = NTILE * 128
EPS = 1e-6
NL = 4
N_EXP = 16
DE = 512
LRU_W = 1280
LW = 2310
LAT0 = 2
CTX0 = 2053


def build(layers=(0, 1, 2, 3), dbg=False, do_mixer=True, do_moe=True):
    nc = bass.Bass("TRN2", target_bir_lowering=False)
    P = Prog(nc)

    def din(name, shape):
        return nc.dram_tensor(name, list(shape), F32, kind="ExternalInput").ap()

    resin = din("resin", [4608, D])
    cT_d = din("cT", [D, 3])
    w_ada = din("w_ada", [NL, D, 6 * D])
    badaT_d = din("badaT", [128, NL, 48])
    normgT_d = din("normgT", [128, NL, 2, 8])
    ident_d = din("ident", [128, 128])
    attn_w_in = din("attn_w_in", [2, D, 3 * D])
    attn_qg = din("attn_q_gain", [2, 64])
    attn_kg = din("attn_k_gain", [2, 64])
    attn_lam = din("attn_lam", [2, 256])
    attn_sgT = din("attn_sgT", [128, 2])
    attn_w_out = din("attn_w_out", [2, D, D])
    rope_cos = din("rope_cos", [128, NTILE, 32])
    rope_sin = din("rope_sin", [128, NTILE, 32])
    lru_w_in = din("lru_w_in", [1, D, 2 * LRU_W])
    lru_cwT = din("lru_cwT", [128, 10, 4])
    lru_cbT = din("lru_cbT", [128, 10])
    lru_w_a = din("lru_w_a", [1, 2, 10, 128, 128])
    lru_w_x = din("lru_w_x", [1, 2, 10, 128, 128])
    lru_baT = din("lru_baT", [128, 2, 10])
    lru_bxT = din("lru_bxT", [128, 2, 10])
    lru_lamT = din("lru_lamT", [128, 2, 10])
    lru_w_out = din("lru_w_out", [1, LRU_W, D])
    pool_w_in = din("pool_w_in", [1, D, D])
    pool_w_grp = din("pool_w_grp", [1, 4, 256, 256])
    pool_scale = din("pool_scale", [1, D])
    pool_edge = din("pool_edge", [128, 4, 2, 8])
    moe_router = din("moe_router", [NL, D, 20])
    moe_w1r = din("moe_w1r", [NL, N_EXP * 128 * 2, 2048])
    moe_w3r = din("moe_w3r", [NL, N_EXP * 128 * 2, 2048])
    moe_w2r = din("moe_w2r", [NL, N_EXP * 128 * 2, 2048])
    normg_raw = din("normg_raw", [NL, 2, D])
    utri_d = din("utri", [128, 128])
    tvals_d = din("tvals", [128, 40])
    pidx_d = din("pidx", [128, 1])
    NSLOT = 34 * 512
    xg = nc.dram_tensor("xg", [NSLOT, D], BF16, kind="Internal").ap()
    yg = nc.dram_tensor("yg", [NSLOT, D], F32, kind="Internal").ap()
    out = nc.dram_tensor("out", [4096, D], F32, kind="ExternalOutput").ap()
    if dbg:
        outc = nc.dram_tensor("outc", [512, D], F32, kind="ExternalOutput").ap()
    res = nc.dram_tensor("res", [4608, D], F32, kind="Internal").ap()
    mods_d = nc.dram_tensor("mods_d", [NL, 144, 128], F32, kind="Internal").ap()

    ident32 = P.sbuf([128, 128], F32, "sb_ident32")
    identb = P.sbuf([128, 128], BF16, "sb_identb")
    modsT = P.sbuf([128, NL, 3, 48], F32, "sb_modsT")
    normgT = P.sbuf([128, NL, 2, 8], F32, "sb_normgT")
    Amod = P.sbuf([128, 2, 3, 8], F32, "sb_Amod")
    cm05 = P.sbuf([128, 16], F32, "sb_cm05")
    t_const = Tok("const")
    t_mods = Tok("modsT")
    t_amod = Tok("amod")
    t_modsd = [Tok(f"modsd{l}") for l in range(NL)]
    psum = P.psum([128, 8, 512], F32, "ps_all")
    t_ps = [Tok(f"psb{i}") for i in range(8)]
    ar = Arena(P, 196 * 1024)
    t_res = [[Tok(f"res{b}_{i}") for i in range(NTILE)] for b in range(2)]

    dumps = {}

    def dump(name, ap, reads):
        if not dbg or name in dumps:
            return
        shp = list(ap.shape)
        d = nc.dram_tensor("dump_" + name, shp, F32, kind="ExternalOutput").ap()
        dumps[name] = d
        P.dma("pool", d, ap, reads=reads, allow_slow_non_contiguous=True)

    def bank(i):
        return psum[:, i, :]

    def bankb(i):
        return psum[:, i, :].bitcast(BF16)

    def rows(b, i):
        if i < 16:
            return b * S + i * 128
        return 4096 + b * CT + (i - 16) * 128

    def mset(b, i):
        return b if i < 16 else 2

    def vec_op(eng):
        return "dve" if eng == 0 else "pool"

    P.dma("sp", ident32[:], ident_d, writes=[t_const])
    P.op("dve", lambda e: e.tensor_copy(out=identb[:], in_=ident32[:]), reads=[t_const], writes=[t_const])
    P.op("dve", lambda e: e.memset(cm05[:], -0.5), writes=[t_const])
    P.dma("sp", normgT[:], normgT_d, writes=[t_const])
    for b in range(2):
        for i in range(NTILE):
            r0 = rows(b, i)
            P.dma("sp", res[r0:r0 + 128, :], resin[r0:r0 + 128, :], writes=[t_res[b][i]])

    def phase_ada():
        ar.off = 0
        scT = ar.alloc([8, 3], F32)
        t_sc = Tok("scT")
        P.dma("sp", scT, cT_d.rearrange("(k p) s -> p k s", p=128), writes=[t_sc])
        P.op("act", lambda e: e.activation(out=scT, in_=scT, func=AF.Silu), reads=[t_sc], writes=[t_sc])
        badaT = ar.alloc([NL, 48], F32)
        t_b = Tok("bada")
        P.dma("sp", badaT, badaT_d, writes=[t_b])
        NW = 3
        Wt = [ar.alloc([8, 512], F32) for _ in range(NW)]
        t_W = [Tok(f"W{i}") for i in range(NW)]
        tmpT = ar.alloc([128], F32)
        t_tmp = Tok("tmpT")
        wi = 0
        for l in range(NL):
            if l not in layers:
                continue
            pb = l % 2
            wsrc = w_ada[l].rearrange("(k p) n -> p k n", p=128)
            for nch in range(12):
                s = wi % NW
                wi += 1
                P.dma("sp", Wt[s], wsrc[:, :, nch * 512:(nch + 1) * 512], writes=[t_W[s]])
                for cc in range(4):
                    c = nch * 4 + cc
                    for k in range(KD):
                        P.op("pe", lambda e, s=s, k=k, cc=cc, c=c, pb=pb: e.matmul(
                            psum[:, pb, c:c + 97:48], lhsT=Wt[s][:, k, cc * 128:(cc + 1) * 128],
                            rhs=scT[:, k, :], start=(k == 0), stop=(k == KD - 1)),
                            reads=[t_W[s], t_sc], writes=[t_ps[pb]])
            P.op("dve", lambda e, l=l, pb=pb: e.tensor_tensor(
                out=modsT[:, l], in0=psum[:, pb, 0:144].rearrange("p (s c) -> p s c", c=48),
                in1=bc(badaT[:, l:l + 1, :], [128, 3, 48]), op=ALU.add),
                reads=[t_ps[pb], t_b], writes=[t_mods])
            src = modsT[:, l].rearrange("p s c -> p (s c)")
            for (r0, r1) in ((0, 128), (128, 144)):
                n = r1 - r0
                P.op("pe", lambda e, r0=r0, r1=r1, n=n, src=src: e.transpose(
                    out=psum[0:n, 2, 0:128], in_=src[:, r0:r1], identity=ident32[:]),
                    reads=[t_mods, t_const], writes=[t_ps[2]])
                P.op("act", lambda e, n=n: e.activation(out=tmpT[0:n, :], in_=psum[0:n, 2, 0:128], func=AF.Copy),
                     reads=[t_ps[2]], writes=[t_tmp])
                P.dma("sp", mods_d[l, r0:r1, :], tmpT[0:n, :], reads=[t_tmp], writes=[t_modsd[l]])
            dump("modsT", modsT[:, l], [t_mods])
        P.barrier()

    def layer_setup(l):
        for n in range(2):
            for s in range(3):
                j = 1 + 3 * n
                P.op("dve", lambda e, n=n, s=s, j=j: e.scalar_tensor_tensor(
                    out=Amod[:, n, s, :], in0=modsT[:, l, s, j * 8:(j + 1) * 8], scalar=1.0,
                    in1=normgT[:, l, n, :], op0=ALU.add, op1=ALU.mult),
                    reads=[t_mods, t_const], writes=[t_amod])

    def Bmod(l, n, s):
        j = 3 * n
        return modsT[:, l, s, j * 8:(j + 1) * 8]

    def load_gate(l, which, s, dst, tok):
        r0 = s * 48 + which * 8
        src = mods_d[l, r0:r0 + 8, :]
        a = AP(src.tensor, src.offset, [[0, 128], [1, 1024]])
        P.dma("sp", dst, a, reads=[t_modsd[l]], writes=[tok])

    class NormBufs:
        def __init__(self, want32):
            self.NX = 3
            self.xt = [ar.alloc([D], F32) for _ in range(self.NX)]
            self.t_xt = [Tok() for _ in range(self.NX)]
            self.junk = ar.alloc([D], BF16)
            self.t_junk = Tok()
            self.st = [ar.alloc([4], F32) for _ in range(self.NX)]
            self.t_st = [Tok() for _ in range(self.NX)]
            self.xh = [ar.alloc([D], F32) for _ in range(2)]
            self.t_xh = [Tok() for _ in range(2)]
            self.cnt = 0
            if want32:
                self.u32 = [ar.alloc([KD, 128], F32) for _ in range(2)]
                self.t_u32 = [Tok() for _ in range(2)]

    def norm_tile(nb, l, n, b, i, uT, t_uT, col, want32=False):
        c = nb.cnt
        nb.cnt += 1
        sx = c % nb.NX
        s2 = c % 2
        xt, st, xh = nb.xt[sx], nb.st[sx], nb.xh[s2]
        r0 = rows(b, i)
        s = mset(b, i)
        P.dma("sp", xt, res[r0:r0 + 128, :], reads=[t_res[b][i]], writes=[nb.t_xt[sx]])
        P.op("act", lambda e: e.activation(out=nb.junk, in_=xt, func=AF.Square, accum_out=st[:, 0:1]),
             reads=[nb.t_xt[sx]], writes=[nb.t_junk, nb.t_st[sx]])
        P.op("dve", lambda e: e.tensor_scalar(out=st[:, 1:2], in0=st[:, 0:1], scalar1=1.0 / D, scalar2=EPS,
                                              op0=ALU.mult, op1=ALU.add),
             reads=[nb.t_st[sx]], writes=[nb.t_st[sx]])
        P.op("pool", lambda e: e.tensor_tensor(out=st[:, 2:3], in0=st[:, 1:2], in1=cm05[:, 0:1], op=ALU.pow),
             reads=[nb.t_st[sx], t_const], writes=[nb.t_st[sx]])
        P.op("act", lambda e: e.activation(out=xh, in_=xt, func=AF.Copy, scale=st[:, 2:3]),
             reads=[nb.t_xt[sx], nb.t_st[sx]], writes=[nb.t_xh[s2]])
        pb0 = 2 * (c % 2)
        for k in range(KD):
            pb = pb0 + k // 4
            P.op("pe", lambda e, k=k, pb=pb: e.transpose(
                out=psum[:, pb, (k % 4) * 128:(k % 4 + 1) * 128], in_=xh[:, k * 128:(k + 1) * 128],
                identity=ident32[:]), reads=[nb.t_xh[s2], t_const], writes=[t_ps[pb]])
        A = Amod[:, n, s, :]
        B = Bmod(l, n, s)
        if want32:
            u32 = nb.u32[s2]
            for k in range(KD):
                pb = pb0 + k // 4
                P.op("dve", lambda e, k=k, pb=pb: e.tensor_scalar(
                    out=u32[:, k, :], in0=psum[:, pb, (k % 4) * 128:(k % 4 + 1) * 128],
                    scalar1=A[:, k:k + 1], scalar2=B[:, k:k + 1], op0=ALU.mult, op1=ALU.add),
                    reads=[t_ps[pb], t_amod, t_mods], writes=[nb.t_u32[s2]])
            P.op("act", lambda e: e.activation(out=uT[:, :, col:col + 128], in_=u32, func=AF.Copy),
                 reads=[nb.t_u32[s2]], writes=[t_uT])
            return u32, nb.t_u32[s2]
        for k in range(KD):
            pb = pb0 + k // 4
            src = psum[:, pb, (k % 4) * 128:(k % 4 + 1) * 128]
            dst = uT[:, k, col:col + 128]
            if k % 2 == 0:
                P.op("act", lambda e, k=k, src=src, dst=dst: e.activation(
                    out=dst, in_=src, func=AF.Identity, scale=A[:, k:k + 1], bias=B[:, k:k + 1]),
                    reads=[t_ps[pb], t_amod, t_mods], writes=[t_uT])
            else:
                P.op("dve", lambda e, k=k, src=src, dst=dst: e.tensor_scalar(
                    out=dst, in0=src, scalar1=A[:, k:k + 1], scalar2=B[:, k:k + 1], op0=ALU.mult, op1=ALU.add),
                    reads=[t_ps[pb], t_amod, t_mods], writes=[t_uT])
        return None, None

    class ResUpd:
        def __init__(self):
            self.N = 3
            self.xt = [ar.alloc([D], F32) for _ in range(self.N)]
            self.t_xt = [Tok() for _ in range(self.N)]
            self.tmp = [ar.alloc([D], F32) for _ in range(2)]
            self.t_tmp = [Tok() for _ in range(2)]
            self.cnt = 0

        def prefetch(self, b, i):
            sx = self.cnt % self.N
            r0 = rows(b, i)
            P.dma("sp", self.xt[sx], res[r0:r0 + 128, :], reads=[t_res[b][i]], writes=[self.t_xt[sx]])

        def apply(self, b, i, ysrc, yreads, G, t_G, dst=None):
            sx = self.cnt % self.N
            s2 = self.cnt % 2
            self.cnt += 1
            xt, tmp = self.xt[sx], self.tmp[s2]
            for (ap_in, c0, n) in ysrc:
                P.op("dve", lambda e, ap_in=ap_in, c0=c0, n=n: e.tensor_tensor(
                    out=tmp[:, c0:c0 + n], in0=ap_in, in1=G[:, c0:c0 + n], op=ALU.mult),
                    reads=list(yreads) + [t_G], writes=[self.t_tmp[s2]])
            P.op("dve", lambda e: e.tensor_tensor(out=xt, in0=xt, in1=tmp, op=ALU.add),
                 reads=[self.t_tmp[s2], self.t_xt[sx]], writes=[self.t_xt[sx]])
            r0 = rows(b, i)
            if dst is None:
                P.dma("sp", res[r0:r0 + 128, :], xt, reads=[self.t_xt[sx]], writes=[t_res[b][i]])
            else:
                P.dma("sp", dst, xt, reads=[self.t_xt[sx]], writes=[t_res[b][i]])

    def attn_layer(l, b, need_ctx):
        j = l // 3
        lam_init = 0.8 - 0.6 * math.exp(-0.3 * l)
        ar.off = 0
        qT = ar.alloc([KD, TB], BF16)
        kT = ar.alloc([KD, TB], BF16)
        vx = ar.alloc([NTILE, 8, 130], BF16)
        t_qT, t_kT, t_vx = Tok("qT"), Tok("kT"), Tok("vx")
        small = ar.alloc([16], F32)
        t_small = Tok("small")
        mark = ar.off
        uT = ar.alloc([KD, TB], BF16)
        t_uT = Tok("uT")
        mark1 = ar.off
        nb = NormBufs(False)
        layer_setup(l)
        for i in range(NTILE):
            norm_tile(nb, l, 0, b, i, uT, t_uT, i * 128)
        dump("uT", uT, [t_uT])
        P.barrier()
        ar.off = mark1
        P.op("pool", lambda e: e.memset(vx[:, :, :, 128:130], 1.0), writes=[t_vx])
        cs_t = ar.alloc([2, NTILE, 32], F32)
        t_cs = Tok("cs")
        P.dma("sp", cs_t[:, 0], rope_cos, writes=[t_cs])
        P.dma("sp", cs_t[:, 1], rope_sin, writes=[t_cs])
        gt = ar.alloc([2, 512], F32)
        t_gt = Tok("gt")
        for qk, src in enumerate((attn_qg, attn_kg)):
            a = src[j:j + 1, :]
            P.dma("sp", gt[:, qk, :].rearrange("p (a b) -> p a b", b=64),
                  AP(a.tensor, a.offset, [[0, 128], [0, 8], [1, 64]]), writes=[t_gt])
        lq = ar.alloc([256], F32)
        t_lq = Tok("lq")
        a = attn_lam[j:j + 1, :]
        P.dma("sp", lq, AP(a.tensor, a.offset, [[0, 128], [1, 256]]), writes=[t_lq])
        lqj = ar.alloc([128], F32)
        P.op("dve", lambda e: e.tensor_tensor(out=lqj[:, 0:64], in0=lq[:, 0:64], in1=lq[:, 64:128], op=ALU.mult),
             reads=[t_lq], writes=[t_lq])
        P.op("dve", lambda e: e.tensor_tensor(out=lqj[:, 64:128], in0=lq[:, 128:192], in1=lq[:, 192:256], op=ALU.mult),
             reads=[t_lq], writes=[t_lq])
        P.op("dve", lambda e: e.tensor_reduce(out=small[:, 0:2], in_=lqj.rearrange("p (a b) -> p a b", b=64),
                                              axis=AX.X, op=ALU.add), reads=[t_lq], writes=[t_small])
        P.op("act", lambda e: e.activation(out=small[:, 2:4], in_=small[:, 0:2], func=AF.Exp),
             reads=[t_small], writes=[t_small])
        P.op("dve", lambda e: e.tensor_tensor(out=small[:, 4:5], in0=small[:, 3:4], in1=small[:, 2:3], op=ALU.subtract),
             reads=[t_small], writes=[t_small])
        P.op("dve", lambda e: e.tensor_scalar(out=small[:, 5:6], in0=small[:, 4:5], scalar1=-lam_init, scalar2=None,
                                              op0=ALU.add), reads=[t_small], writes=[t_small])
        neglam = small[:, 5:6]
        P.dma("sp", small[:, 8:10], attn_sgT, writes=[t_small])
        P.op("dve", lambda e: e.tensor_scalar(out=small[:, 6:7], in0=small[:, 8 + j:9 + j], scalar1=1.0 - lam_init,
                                              scalar2=None, op0=ALU.mult), reads=[t_small], writes=[t_small])
        sgs = small[:, 6:7]

        NWB = 2
        wch = [ar.alloc([KD, 512], BF16) for _ in range(NWB)]
        t_wch = [Tok() for _ in range(NWB)]
        qs = [ar.alloc([512], F32) for _ in range(2)]
        ta = [ar.alloc([512], F32) for _ in range(2)]
        sq = ta
        qn = qs
        qr = [ar.alloc([512], BF16) for _ in range(2)]
        stt = [ar.alloc([32], F32) for _ in range(2)]
        t_q = [Tok() for _ in range(2)]
        wsrc = attn_w_in[j].rearrange("(k p) n -> p k n", p=128)
        cnt = 0
        for nch in range(6):
            ws = nch % NWB
            P.dma("pool", wch[ws], wsrc[:, :, nch * 512:(nch + 1) * 512], writes=[t_wch[ws]])
            for i in range(NTILE):
                pb = 4 + (cnt % 2)
                s2 = cnt % 2
                cnt += 1
                for k in range(KD):
                    P.op("pe", lambda e, k=k, pb=pb, ws=ws, i=i: e.matmul(
                        bank(pb), lhsT=uT[:, k, i * 128:(i + 1) * 128], rhs=wch[ws][:, k, :],
                        start=(k == 0), stop=(k == KD - 1)), reads=[t_uT, t_wch[ws]], writes=[t_ps[pb]])
                if nch >= 4:
                    h0 = (nch - 4) * 4
                    P.op("act", lambda e, pb=pb, i=i, h0=h0: e.activation(
                        out=vx[:, i, h0:h0 + 4, 0:128], in_=bank(pb).rearrange("p (h d) -> p h d", d=128),
                        func=AF.Copy), reads=[t_ps[pb]], writes=[t_vx])
                    continue
                qk = nch // 2
                h0 = (nch % 2) * 4
                tq = t_q[s2]
                P.op("act", lambda e, pb=pb, s2=s2: e.activation(out=qs[s2], in_=bank(pb), func=AF.Copy),
                     reads=[t_ps[pb]], writes=[tq])
                P.op("act", lambda e, pb=pb, s2=s2: e.activation(out=sq[s2], in_=bank(pb), func=AF.Square),
                     reads=[t_ps[pb]], writes=[tq])
                P.op("dve", lambda e, s2=s2: e.tensor_reduce(
                    out=stt[s2][:, 0:8], in_=sq[s2].rearrange("p (a b) -> p a b", b=64), axis=AX.X, op=ALU.add),
                    reads=[tq], writes=[tq])
                P.op("dve", lambda e, s2=s2: e.tensor_scalar(
                    out=stt[s2][:, 8:16], in0=stt[s2][:, 0:8], scalar1=1.0 / 64, scalar2=EPS, op0=ALU.mult, op1=ALU.add),
                    reads=[tq], writes=[tq])
                P.op("pool", lambda e, s2=s2: e.tensor_tensor(
                    out=stt[s2][:, 16:24], in0=stt[s2][:, 8:16], in1=cm05[:, 0:8], op=ALU.pow),
                    reads=[tq, t_const], writes=[tq])
                P.op("dve", lambda e, s2=s2: e.tensor_tensor(
                    out=qn[s2].rearrange("p (a b) -> p a b", b=64), in0=qs[s2].rearrange("p (a b) -> p a b", b=64),
                    in1=bc(stt[s2][:, 16:24].unsqueeze(2), [128, 8, 64]), op=ALU.mult),
                    reads=[tq], writes=[tq])
                P.op("pool", lambda e, s2=s2, qk=qk: e.tensor_tensor(out=qn[s2], in0=qn[s2], in1=gt[:, qk, :], op=ALU.mult),
                     reads=[tq, t_gt], writes=[tq])
                x4 = qn[s2].rearrange("p (a m d) -> p a m d", m=2, d=32)
                t4 = ta[s2].rearrange("p (a m d) -> p a m d", m=2, d=32)
                r4 = qr[s2].rearrange("p (a m d) -> p a m d", m=2, d=32)

                def tb(ti, i=i):
                    return bc(cs_t[:, (0, 1, 1, 0)[ti], i, :].unsqueeze(1), [128, 8, 32])
                P.op("pool", lambda e, x4=x4, t4=t4, tb=tb: e.tensor_tensor(out=t4[:, :, 0, :], in0=x4[:, :, 0, :], in1=tb(0), op=ALU.mult),
                     reads=[tq, t_cs], writes=[tq])
                P.op("pool", lambda e, x4=x4, t4=t4, tb=tb: e.tensor_tensor(out=t4[:, :, 1, :], in0=x4[:, :, 1, :], in1=tb(1), op=ALU.mult),
                     reads=[tq, t_cs], writes=[tq])
                P.op("dve", lambda e, t4=t4, r4=r4: e.tensor_tensor(out=r4[:, :, 0, :], in0=t4[:, :, 0, :], in1=t4[:, :, 1, :], op=ALU.subtract),
                     reads=[tq], writes=[tq])
                P.op("pool", lambda e, x4=x4, t4=t4, tb=tb: e.tensor_tensor(out=t4[:, :, 0, :], in0=x4[:, :, 0, :], in1=tb(2), op=ALU.mult),
                     reads=[tq, t_cs], writes=[tq])
                P.op("pool", lambda e, x4=x4, t4=t4, tb=tb: e.tensor_tensor(out=t4[:, :, 1, :], in0=x4[:, :, 1, :], in1=tb(3), op=ALU.mult),
                     reads=[tq, t_cs], writes=[tq])
                P.op("dve", lambda e, t4=t4, r4=r4: e.tensor_tensor(out=r4[:, :, 1, :], in0=t4[:, :, 0, :], in1=t4[:, :, 1, :], op=ALU.add),
                     reads=[tq], writes=[tq])
                pbt = 6 + (cnt % 2)
                for hh in range(4):
                    P.op("pe", lambda e, hh=hh, pbt=pbt, s2=s2: e.transpose(
                        out=bankb(pbt)[:, hh * 128:(hh + 1) * 128], in_=qr[s2][:, hh * 128:(hh + 1) * 128],
                        identity=identb[:]), reads=[tq, t_const], writes=[t_ps[pbt]])
                dstT = (qT if qk == 0 else kT)
                P.op("act", lambda e, pbt=pbt, dstT=dstT, h0=h0, i=i: e.activation(
                    out=dstT[:, h0:h0 + 4, i * 128:(i + 1) * 128],
                    in_=bankb(pbt)[:, 0:512].rearrange("p (h t) -> p h t", t=128), func=AF.Copy),
                    reads=[t_ps[pbt]], writes=[t_qT if qk == 0 else t_kT])
        dump("qT", qT, [t_qT])
        dump("kT", kT, [t_kT])
        dump("vx", vx, [t_vx])
        P.barrier()
        ar.off = mark
        wout = ar.alloc([KD, D], BF16)
        t_wout = Tok("wout")
        P.dma("pool", wout, attn_w_out[j].rearrange("(k p) n -> p k n", p=128), writes=[t_wout])
        G1 = [ar.alloc([D], F32) for _ in range(2)]
        t_G1 = [Tok(), Tok()]
        load_gate(l, 2, b, G1[0], t_G1[0])
        load_gate(l, 2, 2, G1[1], t_G1[1])
        ru = ResUpd()
        osb = ar.alloc([4, D], F32)
        t_osb = Tok("osb")
        es = [[ar.alloc([512], BF16) for _ in range(2)] for _ in range(2)]
        t_es = [[Tok(), Tok()], [Tok(), Tok()]]
        fin = ar.alloc([8], F32)
        t_fin = Tok("fin")
        tsb = ar.alloc([128], F32)
        sqt = ar.alloc([D], F32)
        onb = ar.alloc([D], BF16)
        oT = ar.alloc([KD, 128], BF16)
        t_on = Tok("on")
        t_oT = Tok("oT")
        s8 = ar.alloc([32], F32)

        qranges = [(q0, 512, list(range(NTILE))) for q0 in range(0, S, 512)]
        if need_ctx:
            qranges.append((S, CT, [16, 17]))
        def head(h, q0, nq, ktiles, nqi):
            if True:
                def s_mm(ki):
                    kt = ktiles[ki]
                    for m in range(2):
                        pb = m * 2 + (ki % 2)
                        P.op("pe", lambda e, m=m, pb=pb, kt=kt: e.matmul(
                            psum[:, pb, 0:nq], lhsT=kT[m * 64:(m + 1) * 64, h, kt * 128:(kt + 1) * 128],
                            rhs=qT[m * 64:(m + 1) * 64, h, q0:q0 + nq], start=True, stop=True),
                            reads=[t_kT, t_qT], writes=[t_ps[pb]])
                        P.op("act", lambda e, m=m, pb=pb, ki=ki: e.activation(
                            out=es[m][ki % 2][:, 0:nq], in_=psum[:, pb, 0:nq], func=AF.Exp, scale=0.125),
                            reads=[t_ps[pb]], writes=[t_es[m][ki % 2]])
                s_mm(0)
                for ki in range(len(ktiles)):
                    if ki + 1 < len(ktiles):
                        s_mm(ki + 1)
                    kt = ktiles[ki]
                    for m in range(2):
                        for qi in range(nqi):
                            gi = m * 4 + qi
                            pb = 4 + gi // 3
                            c0 = (gi % 3) * 129
                            P.op("pe", lambda e, m=m, qi=qi, pb=pb, c0=c0, kt=kt, ki=ki, gi=gi: e.matmul(
                                psum[:, pb, c0:c0 + 129], lhsT=es[m][ki % 2][:, qi * 128:(qi + 1) * 128],
                                rhs=vx[:, kt, h, 0:129], start=(ki == 0 and (gi % 3 == 0 or (nqi < 4 and qi == 0))),
                                stop=(ki == len(ktiles) - 1), skip_group_check=True),
                                reads=[t_es[m][ki % 2], t_vx], writes=[t_ps[pb]])
                for qi in range(nqi):
                    g0, g1 = qi, 4 + qi
                    pb0, c00 = 4 + g0 // 3, (g0 % 3) * 129
                    pb1, c01 = 4 + g1 // 3, (g1 % 3) * 129
                    P.op("dve", lambda e, pb0=pb0, c00=c00: e.reciprocal(out=fin[:, 0:1], in_=psum[:, pb0, c00 + 128:c00 + 129]),
                         reads=[t_ps[pb0]], writes=[t_fin])
                    P.op("dve", lambda e, pb1=pb1, c01=c01: e.reciprocal(out=fin[:, 1:2], in_=psum[:, pb1, c01 + 128:c01 + 129]),
                         reads=[t_ps[pb1]], writes=[t_fin])
                    P.op("dve", lambda e: e.tensor_tensor(out=fin[:, 2:3], in0=fin[:, 1:2], in1=neglam, op=ALU.mult),
                         reads=[t_fin, t_small], writes=[t_fin])
                    P.op("dve", lambda e, pb1=pb1, c01=c01: e.tensor_scalar(
                        out=tsb, in0=psum[:, pb1, c01:c01 + 128], scalar1=fin[:, 2:3], scalar2=None, op0=ALU.mult),
                        reads=[t_ps[pb1], t_fin], writes=[t_fin])
                    P.op("dve", lambda e, pb0=pb0, c00=c00, qi=qi: e.scalar_tensor_tensor(
                        out=osb[:, qi, h * 128:(h + 1) * 128], in0=psum[:, pb0, c00:c00 + 128], scalar=fin[:, 0:1],
                        in1=tsb, op0=ALU.mult, op1=ALU.add), reads=[t_ps[pb0], t_fin], writes=[t_osb])
        for (q0, nq, ktiles) in qranges:
            nqi = nq // 128
            for h in range(8):
                head(h, q0, nq, ktiles, nqi)
            dump("osb", osb, [t_osb])
            for qi in range(nqi):
                i = q0 // 128 + qi
                ru.prefetch(b, i)
                P.op("dve", lambda e, qi=qi: e.tensor_tensor(out=sqt, in0=osb[:, qi, :], in1=osb[:, qi, :], op=ALU.mult),
                     reads=[t_osb], writes=[t_on])
                P.op("dve", lambda e: e.tensor_reduce(out=s8[:, 0:8], in_=sqt.rearrange("p (a b) -> p a b", b=128),
                                                      axis=AX.X, op=ALU.add), reads=[t_on], writes=[t_on])
                P.op("dve", lambda e: e.tensor_scalar(out=s8[:, 8:16], in0=s8[:, 0:8], scalar1=1.0 / 128, scalar2=EPS,
                                                      op0=ALU.mult, op1=ALU.add), reads=[t_on], writes=[t_on])
                P.op("pool", lambda e: e.tensor_tensor(out=s8[:, 16:24], in0=s8[:, 8:16], in1=cm05[:, 0:8], op=ALU.pow),
                     reads=[t_on, t_const], writes=[t_on])
                P.op("dve", lambda e, qi=qi: e.tensor_tensor(
                    out=onb.rearrange("p (a b) -> p a b", b=128), in0=osb[:, qi, :].rearrange("p (a b) -> p a b", b=128),
                    in1=bc(s8[:, 16:24].unsqueeze(2), [128, 8, 128]), op=ALU.mult), reads=[t_on, t_osb], writes=[t_on])
                for hh in range(8):
                    P.op("pe", lambda e, hh=hh: e.transpose(
                        out=bankb(7)[:, hh * 128:(hh + 1) * 128], in_=onb[:, hh * 128:(hh + 1) * 128],
                        identity=identb[:]), reads=[t_on, t_const], writes=[t_ps[7]])
                P.op("act", lambda e: e.activation(out=oT, in_=bankb(7).rearrange("p (h t) -> p h t", t=128),
                                                   func=AF.Copy, scale=sgs), reads=[t_ps[7], t_small], writes=[t_oT])
                for nh in range(2):
                    for hh in range(8):
                        P.op("pe", lambda e, nh=nh, hh=hh: e.matmul(
                            bank(nh), lhsT=oT[:, hh, :], rhs=wout[:, hh, nh * 512:(nh + 1) * 512],
                            start=(hh == 0), stop=(hh == 7)), reads=[t_oT, t_wout], writes=[t_ps[nh]])
                gs = 0 if i < 16 else 1
                ru.apply(b, i, [(bank(0), 0, 512), (bank(1), 512, 512)], [t_ps[0], t_ps[1]], G1[gs], t_G1[gs])
        P.barrier()

    def lru_layer(l, b, need_ctx):
        ar.off = 0
        layer_setup(l)
        uT = ar.alloc([KD, TB], BF16)
        t_uT = Tok("uT")
        mark0 = ar.off
        nb = NormBufs(False)
        for i in range(NTILE):
            norm_tile(nb, l, 0, b, i, uT, t_uT, i * 128)
        P.barrier()
        ar.off = mark0
        mT = ar.alloc([10, TB], BF16)
        t_mT = Tok("mT")
        cw = ar.alloc([10, 4], F32)
        cb = ar.alloc([10], F32)
        bax = ar.alloc([2, 2, 10], F32)
        lam = ar.alloc([2, 10], F32)
        nsp = ar.alloc([2, 2, 10], F32)
        t_c = Tok("lruc")
        P.dma("sp", cw, lru_cwT, writes=[t_c])
        P.dma("sp", cb, lru_cbT, writes=[t_c])
        P.dma("sp", bax[:, 0], lru_baT, writes=[t_c])
        P.dma("sp", bax[:, 1], lru_bxT, writes=[t_c])
        P.dma("sp", lam, lru_lamT, writes=[t_c])
        P.op("act", lambda e: e.activation(out=lam, in_=lam, func=AF.Exp, scale=-1.0), reads=[t_c], writes=[t_c])
        P.op("act", lambda e: e.activation(out=lam, in_=lam, func=AF.Ln, bias=1.0), reads=[t_c], writes=[t_c])
        P.op("dve", lambda e: e.tensor_scalar(out=nsp[:, 0], in0=lam, scalar1=-8.0, scalar2=None, op0=ALU.mult),
             reads=[t_c], writes=[t_c])
        P.op("dve", lambda e: e.tensor_scalar(out=nsp[:, 1], in0=lam, scalar1=-16.0, scalar2=None, op0=ALU.mult),
             reads=[t_c], writes=[t_c])
        NWB = 2
        win = [ar.alloc([KD, 256], BF16) for _ in range(NWB)]
        t_win = [Tok() for _ in range(NWB)]
        wbd = [ar.alloc([4, 128], BF16) for _ in range(NWB)]
        t_wbd = [Tok() for _ in range(NWB)]
        xr = ar.alloc([LW], F32)
        xc = ar.alloc([LW], F32)
        xcb = ar.alloc([LW], BF16)
        Abuf = ar.alloc([LW], F32)
        Bbuf = ar.alloc([LW], F32)
        Tbuf = ar.alloc([LW], F32)
        H0 = ar.alloc([LW], F32)
        gate = ar.alloc([TB], BF16)
        t_x = Tok("lrux")
        t_A, t_B, t_T, t_H0, t_g = Tok(), Tok(), Tok(), Tok(), Tok()
        P.op("pool", lambda e: e.memset(xr, 0.0), writes=[t_x])
        wsrc = lru_w_in[0].rearrange("(k p) n -> p k n", p=128)
        segs = [(LAT0 + q, q, 512) for q in range(0, S, 512)] + [(CTX0, S, CT)]
        cnt = 0
        for g in range(10):
            ws = g % NWB
            P.dma("pool", win[ws][:, :, 0:128], wsrc[:, :, g * 128:(g + 1) * 128], writes=[t_win[ws]])
            P.dma("pool", win[ws][:, :, 128:256], wsrc[:, :, LRU_W + g * 128:LRU_W + (g + 1) * 128], writes=[t_win[ws]])
            for d in range(2):
                P.dma("pool", wbd[ws][:, 2 * d, :], lru_w_a[0, d, g], writes=[t_wbd[ws]])
                P.dma("pool", wbd[ws][:, 2 * d + 1, :], lru_w_x[0, d, g], writes=[t_wbd[ws]])
            for (pc, tc, n) in segs:
                for half in range(2):
                    pb = cnt % 4
                    cnt += 1
                    for k in range(KD):
                        P.op("pe", lambda e, k=k, pb=pb, half=half, tc=tc, n=n, ws=ws: e.matmul(
                            psum[:, pb, 0:n], lhsT=win[ws][:, k, half * 128:(half + 1) * 128],
                            rhs=uT[:, k, tc:tc + n], start=(k == 0), stop=(k == KD - 1)),
                            reads=[t_uT, t_win[ws]], writes=[t_ps[pb]])
                    if half == 0:
                        P.op("act", lambda e, pb=pb, tc=tc, n=n: e.activation(
                            out=gate[:, tc:tc + n], in_=psum[:, pb, 0:n], func=AF.Gelu_apprx_tanh),
                            reads=[t_ps[pb]], writes=[t_g])
                    else:
                        P.op("act", lambda e, pb=pb, pc=pc, n=n: e.activation(
                            out=xr[:, pc:pc + n], in_=psum[:, pb, 0:n], func=AF.Copy),
                            reads=[t_ps[pb]], writes=[t_x])
            W0, W1 = 2, 2309
            nW = W1 - W0
            P.op("dve", lambda e, g=g: e.tensor_scalar(
                out=xc[:, W0:W1], in0=xr[:, W0 - 2:W1 - 2], scalar1=cw[:, g, 0:1], scalar2=cb[:, g:g + 1],
                op0=ALU.mult, op1=ALU.add), reads=[t_x, t_c], writes=[t_x])
            for k in range(1, 4):
                P.op("dve", lambda e, g=g, k=k: e.scalar_tensor_tensor(
                    out=xc[:, W0:W1], in0=xr[:, W0 - 2 + k:W1 - 2 + k], scalar=cw[:, g, k:k + 1], in1=xc[:, W0:W1],
                    op0=ALU.mult, op1=ALU.add), reads=[t_x, t_c], writes=[t_x])
            P.op("pool", lambda e: e.tensor_copy(out=xcb[:, W0:W1], in_=xc[:, W0:W1]), reads=[t_x], writes=[t_x])
            for d in range(2):
                for (pc, tc, n) in segs:
                    for ax in range(2):
                        pb = 4 + cnt % 4
                        cnt += 1
                        P.op("pe", lambda e, pb=pb, pc=pc, n=n, ws=ws, d=d, ax=ax: e.matmul(
                            psum[:, pb, 0:n], lhsT=wbd[ws][:, 2 * d + ax, :], rhs=xcb[:, pc:pc + n],
                            start=True, stop=True), reads=[t_x, t_wbd[ws]], writes=[t_ps[pb]])
                        dstb = Abuf if ax == 0 else Bbuf
                        P.op("act", lambda e, pb=pb, pc=pc, n=n, dstb=dstb, ax=ax, d=d, g=g: e.activation(
                            out=dstb[:, pc:pc + n], in_=psum[:, pb, 0:n], func=AF.Sigmoid,
                            bias=bax[:, ax, d, g:g + 1]), reads=[t_ps[pb], t_c], writes=[t_A if ax == 0 else t_B])
                P.op("pool", lambda e: e.tensor_tensor(out=Bbuf[:, W0:W1], in0=Bbuf[:, W0:W1], in1=xc[:, W0:W1], op=ALU.mult),
                     reads=[t_B, t_x], writes=[t_B])
                P.op("act", lambda e, d=d, g=g: e.activation(out=Tbuf[:, W0:W1], in_=Abuf[:, W0:W1], func=AF.Exp,
                                                             scale=nsp[:, 1, d, g:g + 1]), reads=[t_A, t_c], writes=[t_T])
                P.op("act", lambda e: e.activation(out=Tbuf[:, W0:W1], in_=Tbuf[:, W0:W1], func=AF.Sqrt, scale=-1.0, bias=1.0),
                     reads=[t_T], writes=[t_T])
                rc = CTX0 if d == 0 else CTX0 + CT - 1
                P.op("dve", lambda e, rc=rc: e.memset(Tbuf[:, rc:rc + 1], 1.0), writes=[t_T])
                P.op("dve", lambda e: e.tensor_tensor(out=Bbuf[:, W0:W1], in0=Bbuf[:, W0:W1], in1=Tbuf[:, W0:W1], op=ALU.mult),
                     reads=[t_B, t_T], writes=[t_B])
                P.op("act", lambda e, d=d, g=g: e.activation(out=Abuf[:, W0:W1], in_=Abuf[:, W0:W1], func=AF.Exp,
                                                             scale=nsp[:, 0, d, g:g + 1]), reads=[t_A, t_c], writes=[t_A])
                Hd = H0 if d == 0 else Tbuf
                t_Hd = t_H0 if d == 0 else t_T
                csl = slice(CTX0, CTX0 + CT)
                lsl = slice(LAT0, LAT0 + S)
                if d == 0:
                    P.op("dve", lambda e, Hd=Hd: e.tensor_tensor_scan(
                        out=Hd[:, csl], data0=Abuf[:, csl], data1=Bbuf[:, csl], initial=0.0, op0=ALU.mult, op1=ALU.add),
                        reads=[t_A, t_B], writes=[t_Hd])
                    P.op("dve", lambda e, Hd=Hd: e.tensor_tensor_scan(
                        out=Hd[:, lsl], data0=Abuf[:, lsl], data1=Bbuf[:, lsl], initial=Hd[:, CTX0 + CT - 1:CTX0 + CT],
                        op0=ALU.mult, op1=ALU.add), reads=[t_A, t_B, t_Hd], writes=[t_Hd])
                else:
                    P.op("dve", lambda e, Hd=Hd: e.tensor_tensor_scan(
                        out=rev2d(Hd[:, csl]), data0=rev2d(Abuf[:, csl]), data1=rev2d(Bbuf[:, csl]), initial=0.0,
                        op0=ALU.mult, op1=ALU.add), reads=[t_A, t_B], writes=[t_Hd])
                    P.op("dve", lambda e, Hd=Hd: e.tensor_tensor_scan(
                        out=rev2d(Hd[:, lsl]), data0=rev2d(Abuf[:, lsl]), data1=rev2d(Bbuf[:, lsl]),
                        initial=Hd[:, CTX0:CTX0 + 1], op0=ALU.mult, op1=ALU.add), reads=[t_A, t_B, t_Hd], writes=[t_Hd])
            P.op("pool", lambda e: e.tensor_tensor(out=H0[:, W0:W1], in0=H0[:, W0:W1], in1=Tbuf[:, W0:W1], op=ALU.add),
                 reads=[t_H0, t_T], writes=[t_H0])
            P.op("dve", lambda e, g=g: e.tensor_tensor(out=mT[:, g, 0:S], in0=H0[:, LAT0:LAT0 + S], in1=gate[:, 0:S], op=ALU.mult),
                 reads=[t_H0, t_g], writes=[t_mT])
            P.op("dve", lambda e, g=g: e.tensor_tensor(out=mT[:, g, S:TB], in0=H0[:, CTX0:CTX0 + CT], in1=gate[:, S:TB], op=ALU.mult),
                 reads=[t_H0, t_g], writes=[t_mT])
        P.barrier()
        ar.off = mark0 + 10 * TB * 2
        wout = ar.alloc([10, D], BF16)
        t_wout = Tok("wout")
        P.dma("pool", wout, lru_w_out[0].rearrange("(k p) n -> p k n", p=128), writes=[t_wout])
        G1 = [ar.alloc([D], F32) for _ in range(2)]
        t_G1 = [Tok(), Tok()]
        load_gate(l, 2, b, G1[0], t_G1[0])
        load_gate(l, 2, 2, G1[1], t_G1[1])
        ru = ResUpd()
        ntl = NTILE if need_ctx else 16
        for i in range(ntl):
            ru.prefetch(b, i)
            pbs = (2 * (i % 2), 2 * (i % 2) + 1)
            for nh in range(2):
                for g in range(10):
                    P.op("pe", lambda e, nh=nh, g=g, i=i, pbs=pbs: e.matmul(
                        bank(pbs[nh]), lhsT=mT[:, g, i * 128:(i + 1) * 128], rhs=wout[:, g, nh * 512:(nh + 1) * 512],
                        start=(g == 0), stop=(g == 9)), reads=[t_mT, t_wout], writes=[t_ps[pbs[nh]]])
            gs = 0 if i < 16 else 1
            ru.apply(b, i, [(bank(pbs[0]), 0, 512), (bank(pbs[1]), 512, 512)], [t_ps[pbs[0]], t_ps[pbs[1]]],
                     G1[gs], t_G1[gs])
        P.barrier()

    def pool_layer(l, b, need_ctx):
        ar.off = 0
        layer_setup(l)
        uT = ar.alloc([KD, TB], BF16)
        t_uT = Tok("uT")
        mark0 = ar.off
        nb = NormBufs(False)
        for i in range(NTILE):
            norm_tile(nb, l, 0, b, i, uT, t_uT, i * 128)
        P.barrier()
        ar.off = mark0
        pT = ar.alloc([KD, TB], BF16)
        t_pT = Tok("pT")
        edge = ar.alloc([4, 2, 8], F32)
        t_e = Tok("edge")
        P.dma("sp", edge, pool_edge, writes=[t_e])
        NWB = 2
        win = [ar.alloc([KD, 128], BF16) for _ in range(NWB)]
        t_win = [Tok() for _ in range(NWB)]
        PADW = 8
        XL = [ar.alloc([S + 16], F32) for _ in range(3)]
        XC = [ar.alloc([CT + 16], F32) for _ in range(3)]
        t_X = Tok("poolx")
        for t in XL + XC:
            P.op("pool", lambda e, t=t: e.memset(t, 0.0), writes=[t_X])
        wsrc = pool_w_in[0].rearrange("(k p) n -> p k n", p=128)
        cnt = 0
        for c in range(8):
            gidx = c // 2
            w = (2, 4, 8, 16)[gidx]
            ws = c % NWB
            P.dma("pool", win[ws], wsrc[:, :, c * 128:(c + 1) * 128], writes=[t_win[ws]])
            for (tc, n, X, off) in [(q, 512, XL, q) for q in range(0, S, 512)] + [(S, CT, XC, 0)]:
                pb = cnt % 4
                cnt += 1
                for k in range(KD):
                    P.op("pe", lambda e, k=k, pb=pb, tc=tc, n=n, ws=ws: e.matmul(
                        psum[:, pb, 0:n], lhsT=win[ws][:, k, :], rhs=uT[:, k, tc:tc + n],
                        start=(k == 0), stop=(k == KD - 1)), reads=[t_uT, t_win[ws]], writes=[t_ps[pb]])
                P.op("act", lambda e, pb=pb, n=n, X=X, off=off: e.activation(
                    out=X[0][:, PADW + off:PADW + off + n], in_=psum[:, pb, 0:n], func=AF.Copy),
                    reads=[t_ps[pb]], writes=[t_X])
            for (X, n, tc) in ((XL, S, 0), (XC, CT, S)):
                x0 = X[0]
                cur, other = X[1], X[2]
                lo, hi = 1, PADW + n + 8
                P.op("dve", lambda e, x0=x0, cur=cur, lo=lo, hi=hi: e.tensor_tensor(
                    out=cur[:, lo:hi], in0=x0[:, lo - 1:hi - 1], in1=x0[:, lo:hi], op=ALU.add), reads=[t_X], writes=[t_X])
                step = 1
                ww = 2
                while ww < w:
                    sp_ = ww // 2
                    lo, hi = lo + sp_, hi - sp_
                    P.op("dve", lambda e, cur=cur, other=other, lo=lo, hi=hi, sp_=sp_: e.tensor_tensor(
                        out=other[:, lo:hi], in0=cur[:, lo - sp_:hi - sp_], in1=cur[:, lo + sp_:hi + sp_], op=ALU.add),
                        reads=[t_X], writes=[t_X])
                    cur, other = other, cur
                    ww *= 2
                P.op("dve", lambda e, cur=cur, x0=x0, n=n, tc=tc, c=c, w=w: e.scalar_tensor_tensor(
                    out=pT[:, c, tc:tc + n], in0=cur[:, PADW:PADW + n], scalar=1.0 / w, in1=x0[:, PADW:PADW + n],
                    op0=ALU.mult, op1=ALU.subtract), reads=[t_X], writes=[t_pT])
                hw = w // 2
                for side in range(2):
                    a0 = PADW if side == 0 else PADW + n - 8
                    o0 = tc if side == 0 else tc + n - 8
                    P.op("dve", lambda e, cur=cur, a0=a0, side=side, gidx=gidx, other=other: e.tensor_tensor(
                        out=other[:, 0:8], in0=cur[:, a0:a0 + 8], in1=edge[:, gidx, side, :], op=ALU.mult),
                        reads=[t_X, t_e], writes=[t_X])
                    P.op("dve", lambda e, x0=x0, a0=a0, o0=o0, c=c, other=other: e.tensor_tensor(
                        out=pT[:, c, o0:o0 + 8], in0=other[:, 0:8], in1=x0[:, a0:a0 + 8], op=ALU.subtract),
                        reads=[t_X], writes=[t_pT])
        P.barrier()
        ar.off = mark0 + KD * TB * 2
        wg = ar.alloc([4, 2, 256], BF16)
        t_wg = Tok("wg")
        for g in range(4):
            P.dma("pool", wg[:, g], pool_w_grp[0, g].rearrange("(k p) n -> p k n", p=128), writes=[t_wg])
        G1 = [ar.alloc([D], F32) for _ in range(2)]
        t_G1 = [Tok(), Tok()]
        load_gate(l, 2, b, G1[0], t_G1[0])
        load_gate(l, 2, 2, G1[1], t_G1[1])
        psc = ar.alloc([D], F32)
        t_psc = Tok("psc")
        a = pool_scale[0:1, :]
        P.dma("sp", psc, AP(a.tensor, a.offset, [[0, 128], [1, D]]), writes=[t_psc])
        for s in range(2):
            P.op("dve", lambda e, s=s: e.tensor_tensor(out=G1[s], in0=G1[s], in1=psc, op=ALU.mult),
                 reads=[t_psc, t_G1[s]], writes=[t_G1[s]])
        ru = ResUpd()
        ntl = NTILE if need_ctx else 16
        for i in range(ntl):
            ru.prefetch(b, i)
            pbs = (2 * (i % 2), 2 * (i % 2) + 1)
            for g in range(4):
                pb = pbs[g // 2]
                c0 = (g % 2) * 256
                for kc in range(2):
                    P.op("pe", lambda e, g=g, kc=kc, pb=pb, c0=c0, i=i: e.matmul(
                        psum[:, pb, c0:c0 + 256], lhsT=pT[:, 2 * g + kc, i * 128:(i + 1) * 128], rhs=wg[:, g, kc, :],
                        start=(kc == 0 and g % 2 == 0), stop=(kc == 1), skip_group_check=True),
                        reads=[t_pT, t_wg], writes=[t_ps[pb]])
            gs = 0 if i < 16 else 1
            ru.apply(b, i, [(bank(pbs[0]), 0, 512), (bank(pbs[1]), 512, 512)], [t_ps[pbs[0]], t_ps[pbs[1]]],
                     G1[gs], t_G1[gs])
        P.barrier()

    I32 = mybir.dt.int32

    def moe_sparse(l, last):
        tl = [(b, i) for b in range(2) for i in range(NTILE if not last else 16)]
        NTT = len(tl)
        NTE = NTT * 2 * 128 // 512 + 15
        ar.off = 0
        layer_setup(l)
        S1a = ar.alloc([NTT, 16], F32)
        S2a = ar.alloc([NTT, 16], F32)
        wts = ar.alloc([NTT, 2], F32)
        posa = ar.alloc([NTT, 16], F32)
        base = ar.alloc([16], F32)
        sidx = ar.alloc([NTT, 2], I32)
        widx = ar.alloc([NTE, 2], I32)
        t_S, t_base, t_idx = Tok("S"), Tok("base"), Tok("idx")
        G2 = [ar.alloc([D], F32) for _ in range(3)]
        t_G2 = [Tok() for _ in range(3)]
        for s_ in range(3):
            load_gate(l, 5, s_, G2[s_], t_G2[s_])
        mark_keep = ar.off
        ub = ar.alloc([NTT, D], BF16)
        t_ub = [Tok() for _ in range(NTT)]
        At = [ar.alloc([D], F32) for _ in range(3)]
        Bt = [ar.alloc([D], F32) for _ in range(3)]
        t_AB = Tok("ABtok")
        gn = ar.alloc([D], F32)
        a_ = normg_raw[l, 1:2, :]
        P.dma("sp", gn, AP(a_.tensor, a_.offset, [[0, 128], [1, D]]), writes=[t_AB])
        for s_ in range(3):
            load_gate(l, 4, s_, At[s_], t_AB)
            load_gate(l, 3, s_, Bt[s_], t_AB)
            P.op("dve", lambda e, s_=s_: e.scalar_tensor_tensor(out=At[s_], in0=At[s_], scalar=1.0, in1=gn,
                                                                op0=ALU.add, op1=ALU.mult), reads=[t_AB], writes=[t_AB])
        wr = ar.alloc([KD, 20], F32)
        t_wr = Tok("wr")
        P.dma("sp", wr, moe_router[l].rearrange("(k p) n -> p k n", p=128), writes=[t_wr])
        utri = ar.alloc([128], BF16)
        onesb = ar.alloc([128], BF16)
        cst = ar.alloc([128 + 40 + 1], F32)
        t_cst = Tok("cst")
        P.dma("sp", cst[:, 0:128], utri_d, writes=[t_cst])
        P.dma("sp", cst[:, 128:168], tvals_d, writes=[t_cst])
        P.dma("sp", cst[:, 168:169], pidx_d, writes=[t_cst])
        P.op("dve", lambda e: e.tensor_copy(out=utri, in_=cst[:, 0:128]), reads=[t_cst], writes=[t_cst])
        P.op("dve", lambda e: e.memset(onesb, 1.0), writes=[t_cst])
        P.op("dve", lambda e: e.memset(base, 0.0), writes=[t_base])
        nb = NormBufs(True)
        rt = [ar.alloc([96], F32) for _ in range(2)]
        t_rt = [Tok(), Tok()]
        mb = [ar.alloc([16], BF16) for _ in range(2)]
        utmp = [ar.alloc([D], F32) for _ in range(2)]
        t_ut = [Tok(), Tok()]

        def route_tile(ti, b, i):
            c = nb.cnt
            nb.cnt += 1
            sx, s2 = c % nb.NX, c % 2
            xt, st, xh, u32 = nb.xt[sx], nb.st[sx], nb.xh[s2], nb.u32[s2]
            r0 = rows(b, i)
            s_ = mset(b, i)
            P.dma("sp", xt, res[r0:r0 + 128, :], reads=[t_res[b][i]], writes=[nb.t_xt[sx]])
            P.op("act", lambda e: e.activation(out=nb.junk, in_=xt, func=AF.Square, accum_out=st[:, 0:1]),
                 reads=[nb.t_xt[sx]], writes=[nb.t_junk, nb.t_st[sx]])
            P.op("dve", lambda e: e.tensor_scalar(out=st[:, 1:2], in0=st[:, 0:1], scalar1=1.0 / D, scalar2=EPS,
                                                  op0=ALU.mult, op1=ALU.add), reads=[nb.t_st[sx]], writes=[nb.t_st[sx]])
            P.op("pool", lambda e: e.tensor_tensor(out=st[:, 2:3], in0=st[:, 1:2], in1=cm05[:, 0:1], op=ALU.pow),
                 reads=[nb.t_st[sx], t_const], writes=[nb.t_st[sx]])
            P.op("act", lambda e: e.activation(out=xh, in_=xt, func=AF.Copy, scale=st[:, 2:3]),
                 reads=[nb.t_xt[sx], nb.t_st[sx]], writes=[nb.t_xh[s2]])
            pb0 = 2 * (c % 2)
            for k in range(KD):
                pb = pb0 + k // 4
                P.op("pe", lambda e, k=k, pb=pb: e.transpose(
                    out=psum[:, pb, (k % 4) * 128:(k % 4 + 1) * 128], in_=xh[:, k * 128:(k + 1) * 128],
                    identity=ident32[:]), reads=[nb.t_xh[s2], t_const], writes=[t_ps[pb]])
            A = Amod[:, 1, s_, :]
            B = Bmod(l, 1, s_)
            for k in range(KD):
                pb = pb0 + k // 4
                P.op("dve", lambda e, k=k, pb=pb: e.tensor_scalar(
                    out=u32[:, k, :], in0=psum[:, pb, (k % 4) * 128:(k % 4 + 1) * 128],
                    scalar1=A[:, k:k + 1], scalar2=B[:, k:k + 1], op0=ALU.mult, op1=ALU.add),
                    reads=[t_ps[pb], t_amod, t_mods], writes=[nb.t_u32[s2]])
            P.op("pool", lambda e: e.tensor_tensor(out=utmp[s2], in0=xh, in1=At[s_], op=ALU.mult),
                 reads=[nb.t_xh[s2], t_AB], writes=[t_ut[s2]])
            P.op("pool", lambda e: e.tensor_tensor(out=ub[:, ti, :], in0=utmp[s2], in1=Bt[s_], op=ALU.add),
                 reads=[t_ut[s2], t_AB], writes=[t_ub[ti]])
            R = rt[s2]
            tr = t_rt[s2]
            pb = 4 + s2
            for k in range(KD):
                P.op("pe", lambda e, k=k: e.matmul(
                    psum[:, pb, 0:20], lhsT=u32[:, k, :], rhs=wr[:, k, :], start=(k == 0), stop=(k == KD - 1)),
                    reads=[nb.t_u32[s2], t_wr], writes=[t_ps[pb]])
            P.op("dve", lambda e: e.tensor_copy(out=R[:, 0:20], in_=psum[:, pb, 0:20]), reads=[t_ps[pb]], writes=[tr])
            P.op("dve", lambda e: e.tensor_reduce(out=R[:, 20:21], in_=R[:, 0:4], axis=AX.X, op=ALU.max), reads=[tr], writes=[tr])
            P.op("dve", lambda e: e.tensor_scalar(out=R[:, 21:22], in0=R[:, 20:21], scalar1=-1.0, scalar2=None, op0=ALU.mult), reads=[tr], writes=[tr])
            P.op("dve", lambda e: e.tensor_scalar(out=R[:, 24:28], in0=R[:, 0:4], scalar1=R[:, 20:21], scalar2=None, op0=ALU.is_equal), reads=[tr], writes=[tr])
            P.op("act", lambda e: e.activation(out=R[:, 28:32], in_=R[:, 0:4], func=AF.Exp, bias=R[:, 21:22], accum_out=R[:, 32:33]), reads=[tr], writes=[tr])
            P.op("dve", lambda e: e.reciprocal(out=R[:, 33:34], in_=R[:, 32:33]), reads=[tr], writes=[tr])
            P.op("dve", lambda e: e.tensor_tensor(
                out=R[:, 36:52].rearrange("p (g j) -> p g j", j=4), in0=R[:, 4:20].rearrange("p (g j) -> p g j", j=4),
                in1=bc(R[:, 24:28].unsqueeze(2), [128, 4, 4]), op=ALU.mult), reads=[tr], writes=[tr])
            P.op("dve", lambda e: e.tensor_reduce(out=R[:, 52:56], in_=R[:, 36:52].rearrange("p (g j) -> p j g", j=4),
                                                  axis=AX.X, op=ALU.add), reads=[tr], writes=[tr])
            P.op("dve", lambda e: e.tensor_reduce(out=R[:, 56:57], in_=R[:, 52:56], axis=AX.X, op=ALU.max), reads=[tr], writes=[tr])
            P.op("dve", lambda e: e.tensor_scalar(out=R[:, 57:58], in0=R[:, 56:57], scalar1=-1.0, scalar2=None, op0=ALU.mult), reads=[tr], writes=[tr])
            P.op("dve", lambda e: e.tensor_scalar(out=R[:, 60:64], in0=R[:, 52:56], scalar1=R[:, 56:57], scalar2=None, op0=ALU.is_equal), reads=[tr], writes=[tr])
            P.op("dve", lambda e: e.scalar_tensor_tensor(out=R[:, 64:68], in0=R[:, 60:64], scalar=-1e30, in1=R[:, 52:56],
                                                         op0=ALU.mult, op1=ALU.add), reads=[tr], writes=[tr])
            P.op("dve", lambda e: e.tensor_reduce(out=R[:, 68:69], in_=R[:, 64:68], axis=AX.X, op=ALU.max), reads=[tr], writes=[tr])
            P.op("dve", lambda e: e.tensor_scalar(out=R[:, 72:76], in0=R[:, 64:68], scalar1=R[:, 68:69], scalar2=None, op0=ALU.is_equal), reads=[tr], writes=[tr])
            P.op("act", lambda e: e.activation(out=R[:, 76:77], in_=R[:, 68:69], func=AF.Exp, bias=R[:, 57:58]), reads=[tr], writes=[tr])
            P.op("dve", lambda e: e.tensor_scalar(out=R[:, 77:78], in0=R[:, 76:77], scalar1=1.0, scalar2=None, op0=ALU.add), reads=[tr], writes=[tr])
            P.op("dve", lambda e: e.reciprocal(out=R[:, 78:79], in_=R[:, 77:78]), reads=[tr], writes=[tr])
            P.op("dve", lambda e: e.tensor_tensor(out=wts[:, ti, 0:1], in0=R[:, 78:79], in1=R[:, 33:34], op=ALU.mult), reads=[tr], writes=[tr, t_S])
            P.op("dve", lambda e: e.tensor_tensor(out=wts[:, ti, 1:2], in0=wts[:, ti, 0:1], in1=R[:, 76:77], op=ALU.mult), reads=[tr, t_S], writes=[t_S])
            P.op("dve", lambda e: e.tensor_tensor(
                out=S1a[:, ti, :].rearrange("p (g j) -> p g j", j=4), in0=bc(R[:, 24:28].unsqueeze(2), [128, 4, 4]),
                in1=bc(R[:, 60:64].unsqueeze(1), [128, 4, 4]), op=ALU.mult), reads=[tr], writes=[t_S])
            P.op("dve", lambda e: e.tensor_tensor(
                out=S2a[:, ti, :].rearrange("p (g j) -> p g j", j=4), in0=bc(R[:, 24:28].unsqueeze(2), [128, 4, 4]),
                in1=bc(R[:, 72:76].unsqueeze(1), [128, 4, 4]), op=ALU.mult), reads=[tr], writes=[t_S])
            P.op("dve", lambda e: e.tensor_tensor(out=mb[s2], in0=S1a[:, ti, :], in1=S2a[:, ti, :], op=ALU.add),
                 reads=[t_S], writes=[tr])
            pbp = 6 + s2
            P.op("pe", lambda e: e.matmul(psum[:, pbp, 0:16], lhsT=utri, rhs=mb[s2], start=True, stop=True),
                 reads=[tr, t_cst], writes=[t_ps[pbp]])
            P.op("pe", lambda e: e.matmul(psum[:, pbp, 16:32], lhsT=onesb, rhs=mb[s2], start=False, stop=True, skip_group_check=True),
                 reads=[tr, t_cst], writes=[t_ps[pbp]])
            P.op("dve", lambda e: e.tensor_tensor(out=posa[:, ti, :], in0=psum[:, pbp, 0:16], in1=base, op=ALU.add),
                 reads=[t_ps[pbp], t_base], writes=[t_S])
            P.op("dve", lambda e: e.tensor_tensor(out=base, in0=psum[:, pbp, 16:32], in1=base, op=ALU.add),
                 reads=[t_ps[pbp], t_base], writes=[t_base])

        for ti, (b, i) in enumerate(tl):
            route_tile(ti, b, i)
        sg = ar.alloc([96], F32)
        sgi = ar.alloc([16], I32)
        t_sg = Tok("sg")
        P.op("dve", lambda e: e.tensor_scalar(out=sg[:, 0:16], in0=base, scalar1=511.0, scalar2=None, op0=ALU.add),
             reads=[t_base], writes=[t_sg])
        P.op("dve", lambda e: e.tensor_copy(out=sgi, in_=sg[:, 0:16]), reads=[t_sg], writes=[t_sg])
        P.op("dve", lambda e: e.tensor_scalar(out=sgi, in0=sgi, scalar1=9, scalar2=9, op0=ALU.arith_shift_right,
                                              op1=ALU.logical_shift_left), reads=[t_sg], writes=[t_sg])
        P.op("dve", lambda e: e.tensor_copy(out=sg[:, 0:16], in_=sgi), reads=[t_sg], writes=[t_sg])
        P.op("dve", lambda e: e.memset(sg[:, 48:64], 1.0), writes=[t_sg])
        P.op("dve", lambda e: e.tensor_tensor_scan(out=sg[:, 16:32], data0=sg[:, 48:64], data1=sg[:, 0:16], initial=0.0,
                                                   op0=ALU.mult, op1=ALU.add), reads=[t_sg], writes=[t_sg])
        P.op("dve", lambda e: e.tensor_tensor(out=sg[:, 32:48], in0=sg[:, 16:32], in1=sg[:, 0:16], op=ALU.subtract),
             reads=[t_sg], writes=[t_sg])
        big = ar.alloc([NTT, 16], F32)
        slf = ar.alloc([NTT, 2], F32)
        P.op("dve", lambda e: e.tensor_tensor(out=posa, in0=posa, in1=bc(sg[:, 32:48].unsqueeze(1), [128, NTT, 16]), op=ALU.add),
             reads=[t_S, t_sg], writes=[t_S])
        for k_, Sk in enumerate((S1a, S2a)):
            P.op("dve", lambda e, Sk=Sk: e.tensor_tensor(out=big, in0=posa, in1=Sk, op=ALU.mult), reads=[t_S], writes=[t_sg])
            P.op("dve", lambda e, k_=k_: e.tensor_reduce(out=slf[:, :, k_], in_=big, axis=AX.X, op=ALU.add), reads=[t_sg], writes=[t_sg])
        P.op("dve", lambda e: e.tensor_copy(out=sidx, in_=slf), reads=[t_sg], writes=[t_idx])
        cmp = ar.alloc([NTE, 16], F32)
        etf = ar.alloc([NTE, 4], F32)
        P.op("dve", lambda e: e.tensor_tensor(out=cmp, in0=bc(sg[:, 16:32].unsqueeze(1), [128, NTE, 16]),
                                              in1=bc(cst[:, 128:128 + NTE].unsqueeze(2), [128, NTE, 16]), op=ALU.is_le),
             reads=[t_sg, t_cst], writes=[t_sg])
        P.op("dve", lambda e: e.tensor_reduce(out=etf[:, :, 0], in_=cmp, axis=AX.X, op=ALU.add), reads=[t_sg], writes=[t_sg])
        P.op("dve", lambda e: e.tensor_scalar(out=etf[:, :, 1], in0=etf[:, :, 0], scalar1=15.0, scalar2=256.0, op0=ALU.min, op1=ALU.mult),
             reads=[t_sg], writes=[t_sg])
        P.op("dve", lambda e: e.tensor_scalar(out=etf[:, :, 1], in0=etf[:, :, 1], scalar1=float(l * N_EXP * 256), scalar2=None, op0=ALU.add),
             reads=[t_sg], writes=[t_sg])
        P.op("dve", lambda e: e.scalar_tensor_tensor(out=etf[:, :, 2], in0=bc(cst[:, 168:169], [128, NTE]), scalar=2.0, in1=etf[:, :, 1],
                                                     op0=ALU.mult, op1=ALU.add), reads=[t_sg, t_cst], writes=[t_sg])
        P.op("dve", lambda e: e.tensor_scalar(out=etf[:, :, 3], in0=etf[:, :, 2], scalar1=1.0, scalar2=None, op0=ALU.add),
             reads=[t_sg], writes=[t_sg])
        P.op("dve", lambda e: e.tensor_copy(out=widx, in_=etf[:, :, 2:4]), reads=[t_sg], writes=[t_idx])
        t_xg = Tok("xg")
        for ti in range(NTT):
            for k_ in range(2):
                P.op("pool", lambda e, ti=ti, k_=k_: e.indirect_dma_start(
                    out=xg, out_offset=bass.IndirectOffsetOnAxis(ap=sidx[:, ti, k_:k_ + 1], axis=0),
                    in_=ub[:, ti, :], in_offset=None), reads=[t_ub[ti], t_idx], writes=[Tok()], dma=True)
        dump("sidx", slf, [t_sg])
        dump("etf", etf[:, :, 0], [t_sg])
        dump("wts", wts, [t_S])
        P.barrier()
        ar.off = mark_keep
        NWB = 2
        w1 = [ar.alloc([KD, DE], BF16) for _ in range(NWB)]
        w3 = [ar.alloc([KD, DE], BF16) for _ in range(NWB)]
        w2 = [ar.alloc([4, D], BF16) for _ in range(NWB)]
        t_w1 = [Tok() for _ in range(NWB)]
        t_w3 = [Tok() for _ in range(NWB)]
        t_w2 = [Tok() for _ in range(NWB)]
        NSTG = 8
        stage = [ar.alloc([2048], F32) for _ in range(NSTG)]
        t_stage = [Tok() for _ in range(NSTG)]
        scnt = 0
        NXT = 8
        xtok = [ar.alloc([D], BF16) for _ in range(NXT)]
        t_xtok = [Tok() for _ in range(NXT)]
        xT = [ar.alloc([KD, 512], BF16) for _ in range(2)]
        t_xT = [Tok(), Tok()]
        hT = [ar.alloc([4, 512], BF16) for _ in range(2)]
        t_hT = [Tok(), Tok()]
        sl = [ar.alloc([512], F32) for _ in range(2)]
        t_sl = [Tok(), Tok()]
        ysb = [ar.alloc([D], F32) for _ in range(3)]
        t_ysb = [Tok() for _ in range(3)]
        t_yg = Tok("yg")
        w1r = moe_w1r.rearrange("l r n -> (l r) n")
        w3r = moe_w3r.rearrange("l r n -> (l r) n")
        w2r = moe_w2r.rearrange("l r n -> (l r) n")
        cntb = [0, 0]

        def issue_loads(t):
            ws = t % NWB
            for wi_, wsrc in enumerate((w1r, w3r, w2r)):
                for hf in range(2):
                    si = (6 * t + 2 * wi_ + hf) % NSTG
                    P.op("pool", lambda e, si=si, wsrc=wsrc, t=t, hf=hf: e.indirect_dma_start(
                        out=stage[si], out_offset=None, in_=wsrc,
                        in_offset=bass.IndirectOffsetOnAxis(ap=widx[:, t, hf:hf + 1], axis=0)),
                        reads=[t_idx], writes=[t_stage[si]], dma=True)
            for i4 in range(4):
                xi = (4 * t + i4) % NXT
                r0 = t * 512 + i4 * 128
                P.dma("sp", xtok[xi], xg[r0:r0 + 128, :], reads=[t_xg], writes=[t_xtok[xi]])

        def casts(t):
            ws = t % NWB
            for wi_, (wt, tw, cengs) in enumerate(((w1, t_w1, ("dve", "dve")), (w3, t_w3, ("act", "act")),
                                                   (w2, t_w2, ("act", "dve")))):
                for hf in range(2):
                    si = (6 * t + 2 * wi_ + hf) % NSTG
                    dst = wt[ws].rearrange("p a b -> p (a b)")[:, hf * 2048:(hf + 1) * 2048]
                    if cengs[hf] == "act":
                        P.op("act", lambda e, si=si, dst=dst: e.activation(out=dst, in_=stage[si], func=AF.Copy),
                             reads=[t_stage[si]], writes=[tw[ws]])
                    else:
                        P.op("dve", lambda e, si=si, dst=dst: e.tensor_copy(out=dst, in_=stage[si]),
                             reads=[t_stage[si]], writes=[tw[ws]])

        def trans(t):
            xs_ = t % 2
            for i4 in range(4):
                xi = (4 * t + i4) % NXT
                pbt = i4 % 2
                for k in range(KD):
                    P.op("pe", lambda e, k=k, pbt=pbt, xi=xi: e.transpose(
                        out=bankb(pbt)[:, k * 128:(k + 1) * 128], in_=xtok[xi][:, k * 128:(k + 1) * 128],
                        identity=identb[:]), reads=[t_xtok[xi], t_const], writes=[t_ps[pbt]])
                src_ = bankb(pbt).rearrange("p (k t) -> p k t", t=128)
                dst_ = xT[xs_][:, :, i4 * 128:(i4 + 1) * 128]
                if i4 % 2 == 0:
                    P.op("act", lambda e, src_=src_, dst_=dst_: e.activation(out=dst_, in_=src_, func=AF.Copy),
                         reads=[t_ps[pbt]], writes=[t_xT[xs_]])
                else:
                    P.op("dve", lambda e, src_=src_, dst_=dst_: e.tensor_copy(out=dst_, in_=src_),
                         reads=[t_ps[pbt]], writes=[t_xT[xs_]])

        def mm1(t):
            ws, xs_, hs = t % NWB, t % 2, t % 2
            for hc in range(4):
                pb1 = 2 + (cntb[0] % 2) * 2
                pb3 = pb1 + 1
                s2 = cntb[0] % 2
                cntb[0] += 1
                for (wt, pb, twt) in ((w1, pb1, t_w1), (w3, pb3, t_w3)):
                    for k in range(KD):
                        P.op("pe", lambda e, wt=wt, pb=pb, k=k, hc=hc, ws=ws, xs_=xs_: e.matmul(
                            bank(pb), lhsT=wt[ws][:, k, hc * 128:(hc + 1) * 128], rhs=xT[xs_][:, k, :],
                            start=(k == 0), stop=(k == KD - 1)), reads=[t_xT[xs_], twt[ws]], writes=[t_ps[pb]])
                P.op("act", lambda e, pb1=pb1, s2=s2: e.activation(out=sl[s2], in_=bank(pb1), func=AF.Silu),
                     reads=[t_ps[pb1]], writes=[t_sl[s2]])
                P.op("dve", lambda e, pb3=pb3, s2=s2, hs=hs, hc=hc: e.tensor_tensor(
                    out=hT[hs][:, hc, :], in0=bank(pb3), in1=sl[s2], op=ALU.mult),
                    reads=[t_ps[pb3], t_sl[s2]], writes=[t_hT[hs]])

        def mm2(t):
            ws, hs = t % NWB, t % 2
            for tq in range(4):
                yi = cntb[1] % 3
                cntb[1] += 1
                for nh in range(2):
                    pb = 6 + nh
                    for hc in range(4):
                        P.op("pe", lambda e, pb=pb, hc=hc, tq=tq, nh=nh, hs=hs, ws=ws: e.matmul(
                            bank(pb), lhsT=hT[hs][:, hc, tq * 128:(tq + 1) * 128], rhs=w2[ws][:, hc, nh * 512:(nh + 1) * 512],
                            start=(hc == 0), stop=(hc == 3)), reads=[t_hT[hs], t_w2[ws]], writes=[t_ps[pb]])
                    if nh == 0:
                        P.op("act", lambda e, pb=pb, yi=yi: e.activation(out=ysb[yi][:, 0:512], in_=bank(pb), func=AF.Copy),
                             reads=[t_ps[pb]], writes=[t_ysb[yi]])
                    else:
                        P.op("dve", lambda e, pb=pb, yi=yi: e.tensor_copy(out=ysb[yi][:, 512:1024], in_=bank(pb)),
                             reads=[t_ps[pb]], writes=[t_ysb[yi]])
                r0 = t * 512 + tq * 128
                P.dma("sp", yg[r0:r0 + 128, :], ysb[yi], reads=[t_ysb[yi]], writes=[Tok()])

        issue_loads(0)
        casts(0)
        trans(0)
        for t in range(NTE):
            if t + 1 < NTE:
                issue_loads(t + 1)
            mm1(t)
            if t + 1 < NTE:
                trans(t + 1)
                casts(t + 1)
            mm2(t)
        P.barrier()
        ar.off = mark_keep
        ru = ResUpd()
        NY = 3
        yk = [[ar.alloc([D], F32) for _ in range(2)] for _ in range(NY)]
        t_yk = [[Tok(), Tok()] for _ in range(NY)]
        yc = [ar.alloc([D], F32) for _ in range(2)]
        t_yc = [Tok(), Tok()]

        def gath(ti):
            s3 = ti % NY
            for k_ in range(2):
                P.op("pool", lambda e, ti=ti, k_=k_, s3=s3: e.indirect_dma_start(
                    out=yk[s3][k_], out_offset=None, in_=yg,
                    in_offset=bass.IndirectOffsetOnAxis(ap=sidx[:, ti, k_:k_ + 1], axis=0)),
                    reads=[t_idx, t_yg], writes=[t_yk[s3][k_]], dma=True)

        gath(0)
        gath(1)
        for ti, (b, i) in enumerate(tl):
            s2 = ti % 2
            s3 = ti % NY
            ru.prefetch(b, i)
            P.op("dve", lambda e, ti=ti, s2=s2, s3=s3: e.tensor_scalar(out=yc[s2], in0=yk[s3][0], scalar1=wts[:, ti, 0:1], scalar2=None,
                                                                         op0=ALU.mult), reads=[t_yk[s3][0], t_S], writes=[t_yc[s2]])
            P.op("dve", lambda e, ti=ti, s2=s2, s3=s3: e.scalar_tensor_tensor(out=yc[s2], in0=yk[s3][1], scalar=wts[:, ti, 1:2], in1=yc[s2],
                                                                                op0=ALU.mult, op1=ALU.add),
                 reads=[t_yk[s3][1], t_S, t_yc[s2]], writes=[t_yc[s2]])
            if ti + 2 < NTT:
                gath(ti + 2)
            gs = mset(b, i)
            dst = None
            if last and not dbg:
                r0 = rows(b, i)
                dst = out[r0:r0 + 128, :]
            ru.apply(b, i, [(yc[s2], 0, D)], [t_yc[s2]], G2[gs], t_G2[gs], dst=dst)
        P.barrier()

    phase_ada()
    last_layer = max(layers)
    for l in layers:
        need_ctx = l < NL - 1
        kind = l % 3
        for b in range(2):
            if do_mixer:
                if kind == 0:
                    attn_layer(l, b, need_ctx)
                elif kind == 1:
                    lru_layer(l, b, need_ctx)
                else:
                    pool_layer(l, b, need_ctx)
        if do_moe:
            moe_sparse(l, not need_ctx)
    if dbg:
        for b in range(2):
            for i in range(NTILE):
                r0 = rows(b, i)
                dst = out[r0:r0 + 128, :] if i < 16 else outc[r0 - 4096:r0 - 4096 + 128, :]
                P.dma("sp", dst, res[r0:r0 + 128, :], reads=[t_res[b][i]], writes=[t_res[b][i]])
    fin_reads = [t_res[b][i] for b in range(2) for i in range(NTILE)]
    P.op("sp", lambda e: e.nop(), reads=fin_reads)
    P.barrier()
    P.emit()
    return nc, P


def _fm(v, inner=None):
    v = np.asarray(v, np.float32)
    lead = v.shape[:-1]
    K = v.shape[-1] // 128
    v = v.reshape(*lead, K, 128)
    return np.ascontiguousarray(np.moveaxis(v, -1, 0))


def _wr(w, K):
    L, E, KP, N = w.shape
    w = w.reshape(L, E, K, 128, N).transpose(0, 1, 3, 2, 4)
    return np.ascontiguousarray(w).reshape(L, E * 128 * 2, K * N // 2)


def _rope_tables():
    t = np.arange(S)
    row = (t // 64).astype(np.float32)
    col = (t % 64).astype(np.float32)
    inv = (10000.0 ** (-np.arange(16, dtype=np.float32) / 16)).astype(np.float32)
    ang = np.concatenate([row[:, None] * inv, col[:, None] * inv], axis=-1)
    cos = np.ones((TB, 32), np.float32)
    sin = np.zeros((TB, 32), np.float32)
    cos[:S] = np.cos(ang)
    sin[:S] = np.sin(ang)
    cos = np.ascontiguousarray(cos.reshape(NTILE, 128, 32).transpose(1, 0, 2))
    sin = np.ascontiguousarray(sin.reshape(NTILE, 128, 32).transpose(1, 0, 2))
    return cos, sin


def _pool_edges():
    e = np.zeros((128, 4, 2, 8), np.float32)
    for g, w in enumerate((2, 4, 8, 16)):
        for n in (S,):
            pass
        hw = w // 2
        for jx in range(8):
            t = jx
            cnt = min(t + hw, 10 ** 9) - max(t - hw, 0)
            e[:, g, 0, jx] = 1.0 / cnt
            d = 8 - jx
            cnt = min(hw, d) + hw
            e[:, g, 1, jx] = 1.0 / cnt
    return e


def prep_inputs(inputs, core, resin_override=None):
    f = lambda k: np.ascontiguousarray(np.asarray(inputs[k], np.float32))
    b0, b1 = 2 * core, 2 * core + 1
    x, ctx, c = f("x"), f("ctx"), f("c")
    if resin_override is None:
        resin = np.concatenate([x[b0], x[b1], ctx[b0], ctx[b1]], axis=0)
    else:
        resin = resin_override
    cT = np.ascontiguousarray(np.stack([c[b0], c[b1], f("c_ctx")], axis=1))
    cos, sin = _rope_tables()
    m = {
        "resin": np.ascontiguousarray(resin),
        "cT": cT,
        "w_ada": f("w_ada"),
        "badaT": np.ascontiguousarray(_fm(f("b_ada"))),
        "normgT": np.ascontiguousarray(_fm(f("norm_g"))),
        "ident": np.eye(128, dtype=np.float32),
        "attn_w_in": f("attn_w_in"),
        "attn_q_gain": f("attn_q_gain"),
        "attn_k_gain": f("attn_k_gain"),
        "attn_lam": f("attn_lam").reshape(2, 256),
        "attn_sgT": np.ascontiguousarray(f("attn_sub_gain").T),
        "attn_w_out": f("attn_w_out"),
        "rope_cos": cos,
        "rope_sin": sin,
        "lru_w_in": f("lru_w_in"),
        "lru_cwT": np.ascontiguousarray(_fm(f("lru_conv_w")[0]).transpose(0, 2, 1)),
        "lru_cbT": np.ascontiguousarray(_fm(f("lru_conv_b")[0])),
        "lru_w_a": f("lru_w_a"),
        "lru_w_x": f("lru_w_x"),
        "lru_baT": np.ascontiguousarray(_fm(f("lru_b_a")[0])),
        "lru_bxT": np.ascontiguousarray(_fm(f("lru_b_x")[0])),
        "lru_lamT": np.ascontiguousarray(_fm(f("lru_lam")[0])),
        "lru_w_out": f("lru_w_out"),
        "pool_w_in": f("pool_w_in"),
        "pool_w_grp": f("pool_w_grp"),
        "pool_scale": f("pool_scale"),
        "pool_edge": _pool_edges(),
        "moe_router": np.ascontiguousarray(np.concatenate([f("moe_router_g"), f("moe_router_e")], axis=-1)),
        "moe_w1r": _wr(f("moe_w1"), 8),
        "moe_w3r": _wr(f("moe_w3"), 8),
        "moe_w2r": _wr(f("moe_w2"), 4),
        "normg_raw": f("norm_g"),
        "utri": np.triu(np.ones((128, 128), np.float32), 1),
        "tvals": np.broadcast_to((np.arange(40, dtype=np.float32) * 512.0)[None, :], (128, 40)).copy(),
        "pidx": np.arange(128, dtype=np.float32).reshape(128, 1),
    }
    return m


_CACHE = {}


def kernel(**inputs):
    if "nc" not in _CACHE:
        _CACHE["nc"] = build()[0]
    nc = _CACHE["nc"]
    shared = prep_inputs(inputs, 0)
    x, ctx, c = (np.asarray(inputs[k], np.float32) for k in ("x", "ctx", "c"))
    cc = np.asarray(inputs["c_ctx"], np.float32)
    in_maps = []
    for core in range(8):
        m = dict(shared)
        b0, b1 = 2 * core, 2 * core + 1
        m["resin"] = np.ascontiguousarray(np.concatenate([x[b0], x[b1], ctx[b0], ctx[b1]], axis=0))
        m["cT"] = np.ascontiguousarray(np.stack([c[b0], c[b1], cc], axis=1))
        in_maps.append(m)
    res = run_bass_kernel_spmd(nc, in_maps, core_ids=list(range(8)))
    outs = [r["out"].reshape(2, S, D) for r in res.results]
    return np.concatenate(outs, axis=0).astype(np.float32)
```

```python
import contextlib
import math
import numpy as np
import concourse.bass as bass
import concourse.mybir as mybir
from concourse.ap import AP
from concourse.bass_utils import run_bass_kernel_spmd

F32 = mybir.dt.float32
BF16 = mybir.dt.bfloat16
AF = mybir.ActivationFunctionType
ALU = mybir.AluOpType
AX = mybir.AxisListType

N_DMA_SEMS = 12
SAME_ENGINE_SYNC = True


class Tok:
    __slots__ = ("name", "last_w", "readers")

    def __init__(self, name=""):
        self.name = name
        self.last_w = None
        self.readers = []


class Op:
    __slots__ = ("eng", "fn", "deps", "signal", "val", "is_dma", "slot")

    def __init__(self, eng, fn, is_dma):
        self.eng = eng
        self.fn = fn
        self.deps = []
        self.signal = False
        self.val = None
        self.is_dma = is_dma
        self.slot = None


class Prog:
    ENGS = ("pe", "act", "dve", "pool", "sp")

    def __init__(self, nc):
        self.nc = nc
        self.ops = []
        self.stack = contextlib.ExitStack()
        self.n_alloc = 0
        self.bar_idx = 0

    def sbuf(self, shape, dtype, name=None):
        self.n_alloc += 1
        return self.stack.enter_context(self.nc.sbuf_tensor(name or f"sb{self.n_alloc}", list(shape), dtype))

    def psum(self, shape, dtype, name=None):
        self.n_alloc += 1
        return self.stack.enter_context(self.nc.psum_tensor(name or f"ps{self.n_alloc}", list(shape), dtype))

    def op(self, eng, fn, reads=(), writes=(), dma=False):
        o = Op(eng, fn, dma)
        deps = []
        for t in reads:
            if t.last_w is not None:
                deps.append(t.last_w)
        for t in writes:
            if t.last_w is not None:
                deps.append(t.last_w)
            deps.extend(t.readers)
        seen = set()
        for d in deps:
            if id(d) in seen or d is o:
                continue
            seen.add(id(d))
            if (not d.is_dma) and (not dma) and d.eng == eng:
                if eng == "pe" or not SAME_ENGINE_SYNC:
                    continue
            d.signal = True
            o.deps.append(d)
        for t in writes:
            t.last_w = o
            t.readers = []
        for t in reads:
            if t in writes:
                continue
            if not dma:
                t.readers = [r for r in t.readers if r.is_dma or r.eng != eng]
            t.readers.append(o)
        self.ops.append(o)
        return o

    def dma(self, eng, out, in_, reads=(), writes=(), **kw):
        return self.op(eng, lambda e: e.dma_start(out=out, in_=in_, **kw), reads, writes, dma=True)

    def barrier(self):
        last = {}
        dmas = []
        for o in self.ops[self.bar_idx:]:
            if o.is_dma:
                dmas.append(o)
            else:
                last[o.eng] = o
        deps = list(last.values()) + dmas
        first = len(self.ops)
        for e in self.ENGS:
            o = Op(e, lambda eng: eng.nop(), False)
            o.deps = [d for d in deps if d.is_dma or d.eng != e]
            for d in o.deps:
                d.signal = True
            self.ops.append(o)
        self.bar_idx = first

    def emit(self):
        nc = self.nc
        cnt = {e: 0 for e in self.ENGS}
        dcnt = {e: 0 for e in self.ENGS}
        slotcnt = {e: [0] * N_DMA_SEMS for e in self.ENGS}
        for o in self.ops:
            if o.is_dma:
                k = dcnt[o.eng] % N_DMA_SEMS
                dcnt[o.eng] += 1
                o.slot = k
                slotcnt[o.eng][k] += 16
                o.val = slotcnt[o.eng][k]
            elif o.signal:
                cnt[o.eng] += 1
                o.val = cnt[o.eng]
        sems = {}
        for e in self.ENGS:
            if cnt[e] > 0:
                sems[e] = self.stack.enter_context(nc.semaphore(f"s_{e}"))
        dsems = {}
        for e in self.ENGS:
            if dcnt[e] > 0:
                dsems[e] = [self.stack.enter_context(nc.semaphore(f"d_{e}{k}"))
                            for k in range(min(N_DMA_SEMS, dcnt[e]))]
        by_eng = {e: [o for o in self.ops if o.eng == e] for e in self.ENGS}
        self.stats = {e: len(by_eng[e]) for e in self.ENGS}
        self.stats["maxsem"] = dict(cnt)
        nwaits = [0]

        def run(e_name, eng):
            known = {}
            for o in by_eng[e_name]:
                waits = {}
                for d in o.deps:
                    key = ("d", d.eng, d.slot) if d.is_dma else ("c", d.eng)
                    if known.get(key, 0) >= d.val:
                        continue
                    waits[key] = max(waits.get(key, 0), d.val)
                if o.is_dma and o.val > 16:
                    key = ("d", o.eng, o.slot)
                    if known.get(key, 0) < o.val - 16:
                        waits[key] = max(waits.get(key, 0), o.val - 16)
                for key, v in waits.items():
                    s = dsems[key[1]][key[2]] if key[0] == "d" else sems[key[1]]
                    eng.wait_ge(s, v)
                    known[key] = v
                    nwaits[0] += 1
                ins = o.fn(eng)
                if o.is_dma:
                    ins.then_inc(dsems[o.eng][o.slot], 16)
                elif o.signal:
                    ins.then_inc(sems[o.eng], 1)

        with nc.Block() as block:
            if by_eng["sp"]:
                @block.sync
                def _(eng):
                    run("sp", eng)
            if by_eng["pe"]:
                @block.tensor
                def _(eng):
                    run("pe", eng)
            if by_eng["act"]:
                @block.scalar
                def _(eng):
                    run("act", eng)
            if by_eng["dve"]:
                @block.vector
                def _(eng):
                    run("dve", eng)
            if by_eng["pool"]:
                @block.gpsimd
                def _(eng):
                    run("pool", eng)
        self.stats["waits"] = nwaits[0]
        self.stack.close()


class Arena:
    def __init__(self, P, nbytes):
        self.t = P.sbuf([128, nbytes // 4], F32, "arena")
        self.cap = nbytes
        self.off = 0

    def alloc(self, free_shape, dtype):
        n = 1
        for s in free_shape:
            n *= s
        nb = n * (2 if dtype == BF16 else 4)
        nb = (nb + 31) // 32 * 32
        assert self.off + nb <= self.cap, f"arena overflow {self.off}+{nb}>{self.cap}"
        a = self.t[:, self.off // 4:(self.off + nb) // 4]
        self.off += nb
        if dtype != F32:
            a = a.bitcast(dtype)
        a = a[:, 0:n]
        if len(free_shape) == 2:
            a = a.rearrange("p (a b) -> p a b", b=free_shape[1])
        elif len(free_shape) == 3:
            a = a.rearrange("p (a b c) -> p a b c", b=free_shape[1], c=free_shape[2])
        return a


def bc(ap, shape):
    return ap.broadcast_to(list(shape))


def rev2d(ap2d):
    a = ap2d.ap
    n = a[1][1]
    st = a[1][0]
    return AP(ap2d.tensor, ap2d.offset + (n - 1) * st, [list(a[0]), [-st, n]])


D = 1024
KD = 8
S = 2048
CT = 256
NTILE = 18
TB = NTILE * 128
EPS = 1e-6
NL = 4
N_EXP = 16
DE = 512
LRU_W = 1280
LW = 2310
LAT0 = 2
CTX0 = 2053


def build(layers=(0, 1, 2, 3), dbg=False, do_mixer=True, do_moe=True):
    nc = bass.Bass("TRN2", target_bir_lowering=False)
    P = Prog(nc)

    def din(name, shape):
        return nc.dram_tensor(name, list(shape), F32, kind="ExternalInput").ap()

    resin = din("resin", [4608, D])
    cT_d = din("cT", [D, 3])
    w_ada = din("w_ada", [NL, D, 6 * D])
    badaT_d = din("badaT", [128, NL, 48])
    normgT_d = din("normgT", [128, NL, 2, 8])
    ident_d = din("ident", [128, 128])
    attn_w_in = din("attn_w_in", [2, D, 3 * D])
    attn_qg = din("attn_q_gain", [2, 64])
    attn_kg = din("attn_k_gain", [2, 64])
    attn_lam = din("attn_lam", [2, 256])
    attn_sgT = din("attn_sgT", [128, 2])
    attn_w_out = din("attn_w_out", [2, D, D])
    rope_cos = din("rope_cos", [128, NTILE, 32])
    rope_sin = din("rope_sin", [128, NTILE, 32])
    lru_w_in = din("lru_w_in", [1, D, 2 * LRU_W])
    lru_cwT = din("lru_cwT", [128, 10, 4])
    lru_cbT = din("lru_cbT", [128, 10])
    lru_w_a = din("lru_w_a", [1, 2, 10, 128, 128])
    lru_w_x = din("lru_w_x", [1, 2, 10, 128, 128])
    lru_baT = din("lru_baT", [128, 2, 10])
    lru_bxT = din("lru_bxT", [128, 2, 10])
    lru_lamT = din("lru_lamT", [128, 2, 10])
    lru_w_out = din("lru_w_out", [1, LRU_W, D])
    pool_w_in = din("pool_w_in", [1, D, D])
    pool_w_grp = din("pool_w_grp", [1, 4, 256, 256])
    pool_scale = din("pool_scale", [1, D])
    pool_edge = din("pool_edge", [128, 4, 2, 8])
    moe_router = din("moe_router", [NL, D, 20])
    moe_w1r = din("moe_w1r", [NL, N_EXP * 128 * 2, 2048])
    moe_w3r = din("moe_w3r", [NL, N_EXP * 128 * 2, 2048])
    moe_w2r = din("moe_w2r", [NL, N_EXP * 128 * 2, 2048])
    normg_raw = din("normg_raw", [NL, 2, D])
    utri_d = din("utri", [128, 128])
    tvals_d = din("tvals", [128, 40])
    pidx_d = din("pidx", [128, 1])
    NSLOT = 34 * 512
    xg = nc.dram_tensor("xg", [NSLOT, D], BF16, kind="Internal").ap()
    yg = nc.dram_tensor("yg", [NSLOT, D], F32, kind="Internal").ap()
    out = nc.dram_tensor("out", [4096, D], F32, kind="ExternalOutput").ap()
    if dbg:
        outc = nc.dram_tensor("outc", [512, D], F32, kind="ExternalOutput").ap()
    res = nc.dram_tensor("res", [4608, D], F32, kind="Internal").ap()
    mods_d = nc.dram_tensor("mods_d", [NL, 144, 128], F32, kind="Internal").ap()

    ident32 = P.sbuf([128, 128], F32, "sb_ident32")
    identb = P.sbuf([128, 128], BF16, "sb_identb")
    modsT = P.sbuf([128, NL, 3, 48], F32, "sb_modsT")
    normgT = P.sbuf([128, NL, 2, 8], F32, "sb_normgT")
    Amod = P.sbuf([128, 2, 3, 8], F32, "sb_Amod")
    cm05 = P.sbuf([128, 16], F32, "sb_cm05")
    t_const = Tok("const")
    t_mods = Tok("modsT")
    t_amod = Tok("amod")
    t_modsd = [Tok(f"modsd{l}") for l in range(NL)]
    psum = P.psum([128, 8, 512], F32, "ps_all")
    t_ps = [Tok(f"psb{i}") for i in range(8)]
    ar = Arena(P, 196 * 1024)
    t_res = [[Tok(f"res{b}_{i}") for i in range(NTILE)] for b in range(2)]

    dumps = {}

    def dump(name, ap, reads):
        if not dbg or name in dumps:
            return
        shp = list(ap.shape)
        d = nc.dram_tensor("dump_" + name, shp, F32, kind="ExternalOutput").ap()
        dumps[name] = d
        P.dma("pool", d, ap, reads=reads, allow_slow_non_contiguous=True)

    def bank(i):
        return psum[:, i, :]

    def bankb(i):
        return psum[:, i, :].bitcast(BF16)

    def rows(b, i):
        if i < 16:
            return b * S + i * 128
        return 4096 + b * CT + (i - 16) * 128

    def mset(b, i):
        return b if i < 16 else 2

    def vec_op(eng):
        return "dve" if eng == 0 else "pool"

    P.dma("sp", ident32[:], ident_d, writes=[t_const])
    P.op("dve", lambda e: e.tensor_copy(out=identb[:], in_=ident32[:]), reads=[t_const], writes=[t_const])
    P.op("dve", lambda e: e.memset(cm05[:], -0.5), writes=[t_const])
    P.dma("sp", normgT[:], normgT_d, writes=[t_const])
    for b in range(2):
        for i in range(NTILE):
            r0 = rows(b, i)
            P.dma("sp", res[r0:r0 + 128, :], resin[r0:r0 + 128, :], writes=[t_res[b][i]])

    def phase_ada():
        ar.off = 0
        scT = ar.alloc([8, 3], F32)
        t_sc = Tok("scT")
        P.dma("sp", scT, cT_d.rearrange("(k p) s -> p k s", p=128), writes=[t_sc])
        P.op("act", lambda e: e.activation(out=scT, in_=scT, func=AF.Silu), reads=[t_sc], writes=[t_sc])
        badaT = ar.alloc([NL, 48], F32)
        t_b = Tok("bada")
        P.dma("sp", badaT, badaT_d, writes=[t_b])
        NW = 3
        Wt = [ar.alloc([8, 512], F32) for _ in range(NW)]
        t_W = [Tok(f"W{i}") for i in range(NW)]
        tmpT = ar.alloc([128], F32)
        t_tmp = Tok("tmpT")
        wi = 0
        for l in range(NL):
            if l not in layers:
                continue
            pb = l % 2
            wsrc = w_ada[l].rearrange("(k p) n -> p k n", p=128)
            for nch in range(12):
                s = wi % NW
                wi += 1
                P.dma("sp", Wt[s], wsrc[:, :, nch * 512:(nch + 1) * 512], writes=[t_W[s]])
                for cc in range(4):
                    c = nch * 4 + cc
                    for k in range(KD):
                        P.op("pe", lambda e, s=s, k=k, cc=cc, c=c, pb=pb: e.matmul(
                            psum[:, pb, c:c + 97:48], lhsT=Wt[s][:, k, cc * 128:(cc + 1) * 128],
                            rhs=scT[:, k, :], start=(k == 0), stop=(k == KD - 1)),
                            reads=[t_W[s], t_sc], writes=[t_ps[pb]])
            P.op("dve", lambda e, l=l, pb=pb: e.tensor_tensor(
                out=modsT[:, l], in0=psum[:, pb, 0:144].rearrange("p (s c) -> p s c", c=48),
                in1=bc(badaT[:, l:l + 1, :], [128, 3, 48]), op=ALU.add),
                reads=[t_ps[pb], t_b], writes=[t_mods])
            src = modsT[:, l].rearrange("p s c -> p (s c)")
            for (r0, r1) in ((0, 128), (128, 144)):
                n = r1 - r0
                P.op("pe", lambda e, r0=r0, r1=r1, n=n, src=src: e.transpose(
                    out=psum[0:n, 2, 0:128], in_=src[:, r0:r1], identity=ident32[:]),
                    reads=[t_mods, t_const], writes=[t_ps[2]])
                P.op("act", lambda e, n=n: e.activation(out=tmpT[0:n, :], in_=psum[0:n, 2, 0:128], func=AF.Copy),
                     reads=[t_ps[2]], writes=[t_tmp])
                P.dma("sp", mods_d[l, r0:r1, :], tmpT[0:n, :], reads=[t_tmp], writes=[t_modsd[l]])
            dump("modsT", modsT[:, l], [t_mods])
        P.barrier()

    def layer_setup(l):
        for n in range(2):
            for s in range(3):
                j = 1 + 3 * n
                P.op("dve", lambda e, n=n, s=s, j=j: e.scalar_tensor_tensor(
                    out=Amod[:, n, s, :], in0=modsT[:, l, s, j * 8:(j + 1) * 8], scalar=1.0,
                    in1=normgT[:, l, n, :], op0=ALU.add, op1=ALU.mult),
                    reads=[t_mods, t_const], writes=[t_amod])

    def Bmod(l, n, s):
        j = 3 * n
        return modsT[:, l, s, j * 8:(j + 1) * 8]

    def load_gate(l, which, s, dst, tok):
        r0 = s * 48 + which * 8
        src = mods_d[l, r0:r0 + 8, :]
        a = AP(src.tensor, src.offset, [[0, 128], [1, 1024]])
        P.dma("sp", dst, a, reads=[t_modsd[l]], writes=[tok])

    class NormBufs:
        def __init__(self, want32):
            self.NX = 3
            self.xt = [ar.alloc([D], F32) for _ in range(self.NX)]
            self.t_xt = [Tok() for _ in range(self.NX)]
            self.junk = ar.alloc([D], BF16)
            self.t_junk = Tok()
            self.st = [ar.alloc([4], F32) for _ in range(self.NX)]
            self.t_st = [Tok() for _ in range(self.NX)]
            self.xh = [ar.alloc([D], F32) for _ in range(2)]
            self.t_xh = [Tok() for _ in range(2)]
            self.cnt = 0
            if want32:
                self.u32 = [ar.alloc([KD, 128], F32) for _ in range(2)]
                self.t_u32 = [Tok() for _ in range(2)]

    def norm_tile(nb, l, n, b, i, uT, t_uT, col, want32=False):
        c = nb.cnt
        nb.cnt += 1
        sx = c % nb.NX
        s2 = c % 2
        xt, st, xh = nb.xt[sx], nb.st[sx], nb.xh[s2]
        r0 = rows(b, i)
        s = mset(b, i)
        P.dma("sp", xt, res[r0:r0 + 128, :], reads=[t_res[b][i]], writes=[nb.t_xt[sx]])
        P.op("act", lambda e: e.activation(out=nb.junk, in_=xt, func=AF.Square, accum_out=st[:, 0:1]),
             reads=[nb.t_xt[sx]], writes=[nb.t_junk, nb.t_st[sx]])
        P.op("dve", lambda e: e.tensor_scalar(out=st[:, 1:2], in0=st[:, 0:1], scalar1=1.0 / D, scalar2=EPS,
                                              op0=ALU.mult, op1=ALU.add),
             reads=[nb.t_st[sx]], writes=[nb.t_st[sx]])
        P.op("pool", lambda e: e.tensor_tensor(out=st[:, 2:3], in0=st[:, 1:2], in1=cm05[:, 0:1], op=ALU.pow),
             reads=[nb.t_st[sx], t_const], writes=[nb.t_st[sx]])
        P.op("act", lambda e: e.activation(out=xh, in_=xt, func=AF.Copy, scale=st[:, 2:3]),
             reads=[nb.t_xt[sx], nb.t_st[sx]], writes=[nb.t_xh[s2]])
        pb0 = 2 * (c % 2)
        for k in range(KD):
            pb = pb0 + k // 4
            P.op("pe", lambda e, k=k, pb=pb: e.transpose(
                out=psum[:, pb, (k % 4) * 128:(k % 4 + 1) * 128], in_=xh[:, k * 128:(k + 1) * 128],
                identity=ident32[:]), reads=[nb.t_xh[s2], t_const], writes=[t_ps[pb]])
        A = Amod[:, n, s, :]
        B = Bmod(l, n, s)
        if want32:
            u32 = nb.u32[s2]
            for k in range(KD):
                pb = pb0 + k // 4
                P.op("dve", lambda e, k=k, pb=pb: e.tensor_scalar(
                    out=u32[:, k, :], in0=psum[:, pb, (k % 4) * 128:(k % 4 + 1) * 128],
                    scalar1=A[:, k:k + 1], scalar2=B[:, k:k + 1], op0=ALU.mult, op1=ALU.add),
                    reads=[t_ps[pb], t_amod, t_mods], writes=[nb.t_u32[s2]])
            P.op("act", lambda e: e.activation(out=uT[:, :, col:col + 128], in_=u32, func=AF.Copy),
                 reads=[nb.t_u32[s2]], writes=[t_uT])
            return u32, nb.t_u32[s2]
        for k in range(KD):
            pb = pb0 + k // 4
            src = psum[:, pb, (k % 4) * 128:(k % 4 + 1) * 128]
            dst = uT[:, k, col:col + 128]
            if k % 2 == 0:
                P.op("act", lambda e, k=k, src=src, dst=dst: e.activation(
                    out=dst, in_=src, func=AF.Identity, scale=A[:, k:k + 1], bias=B[:, k:k + 1]),
                    reads=[t_ps[pb], t_amod, t_mods], writes=[t_uT])
            else:
                P.op("dve", lambda e, k=k, src=src, dst=dst: e.tensor_scalar(
                    out=dst, in0=src, scalar1=A[:, k:k + 1], scalar2=B[:, k:k + 1], op0=ALU.mult, op1=ALU.add),
                    reads=[t_ps[pb], t_amod, t_mods], writes=[t_uT])
        return None, None

    class ResUpd:
        def __init__(self):
            self.N = 3
            self.xt = [ar.alloc([D], F32) for _ in range(self.N)]
            self.t_xt = [Tok() for _ in range(self.N)]
            self.tmp = [ar.alloc([D], F32) for _ in range(2)]
            self.t_tmp = [Tok() for _ in range(2)]
            self.cnt = 0

        def prefetch(self, b, i):
            sx = self.cnt % self.N
            r0 = rows(b, i)
            P.dma("sp", self.xt[sx], res[r0:r0 + 128, :], reads=[t_res[b][i]], writes=[self.t_xt[sx]])

        def apply(self, b, i, ysrc, yreads, G, t_G, dst=None):
            sx = self.cnt % self.N
            s2 = self.cnt % 2
            self.cnt += 1
            xt, tmp = self.xt[sx], self.tmp[s2]
            for (ap_in, c0, n) in ysrc:
                P.op("dve", lambda e, ap_in=ap_in, c0=c0, n=n: e.tensor_tensor(
                    out=tmp[:, c0:c0 + n], in0=ap_in, in1=G[:, c0:c0 + n], op=ALU.mult),
                    reads=list(yreads) + [t_G], writes=[self.t_tmp[s2]])
            P.op("dve", lambda e: e.tensor_tensor(out=xt, in0=xt, in1=tmp, op=ALU.add),
                 reads=[self.t_tmp[s2], self.t_xt[sx]], writes=[self.t_xt[sx]])
            r0 = rows(b, i)
            if dst is None:
                P.dma("sp", res[r0:r0 + 128, :], xt, reads=[self.t_xt[sx]], writes=[t_res[b][i]])
            else:
                P.dma("sp", dst, xt, reads=[self.t_xt[sx]], writes=[t_res[b][i]])

    def attn_layer(l, b, need_ctx):
        j = l // 3
        lam_init = 0.8 - 0.6 * math.exp(-0.3 * l)
        ar.off = 0
        qT = ar.alloc([KD, TB], BF16)
        kT = ar.alloc([KD, TB], BF16)
        vx = ar.alloc([NTILE, 8, 130], BF16)
        t_qT, t_kT, t_vx = Tok("qT"), Tok("kT"), Tok("vx")
        small = ar.alloc([16], F32)
        t_small = Tok("small")
        mark = ar.off
        uT = ar.alloc([KD, TB], BF16)
        t_uT = Tok("uT")
        mark1 = ar.off
        nb = NormBufs(False)
        layer_setup(l)
        for i in range(NTILE):
            norm_tile(nb, l, 0, b, i, uT, t_uT, i * 128)
        dump("uT", uT, [t_uT])
        P.barrier()
        ar.off = mark1
        P.op("pool", lambda e: e.memset(vx[:, :, :, 128:130], 1.0), writes=[t_vx])
        cs_t = ar.alloc([2, NTILE, 32], F32)
        t_cs = Tok("cs")
        P.dma("sp", cs_t[:, 0], rope_cos, writes=[t_cs])
        P.dma("sp", cs_t[:, 1], rope_sin, writes=[t_cs])
        gt = ar.alloc([2, 512], F32)
        t_gt = Tok("gt")
        for qk, src in enumerate((attn_qg, attn_kg)):
            a = src[j:j + 1, :]
            P.dma("sp", gt[:, qk, :].rearrange("p (a b) -> p a b", b=64),
                  AP(a.tensor, a.offset, [[0, 128], [0, 8], [1, 64]]), writes=[t_gt])
        lq = ar.alloc([256], F32)
        t_lq = Tok("lq")
        a = attn_lam[j:j + 1, :]
        P.dma("sp", lq, AP(a.tensor, a.offset, [[0, 128], [1, 256]]), writes=[t_lq])
        lqj = ar.alloc([128], F32)
        P.op("dve", lambda e: e.tensor_tensor(out=lqj[:, 0:64], in0=lq[:, 0:64], in1=lq[:, 64:128], op=ALU.mult),
             reads=[t_lq], writes=[t_lq])
        P.op("dve", lambda e: e.tensor_tensor(out=lqj[:, 64:128], in0=lq[:, 128:192], in1=lq[:, 192:256], op=ALU.mult),
             reads=[t_lq], writes=[t_lq])
        P.op("dve", lambda e: e.tensor_reduce(out=small[:, 0:2], in_=lqj.rearrange("p (a b) -> p a b", b=64),
                                              axis=AX.X, op=ALU.add), reads=[t_lq], writes=[t_small])
        P.op("act", lambda e: e.activation(out=small[:, 2:4], in_=small[:, 0:2], func=AF.Exp),
             reads=[t_small], writes=[t_small])
        P.op("dve", lambda e: e.tensor_tensor(out=small[:, 4:5], in0=small[:, 3:4], in1=small[:, 2:3], op=ALU.subtract),
             reads=[t_small], writes=[t_small])
        P.op("dve", lambda e: e.tensor_scalar(out=small[:, 5:6], in0=small[:, 4:5], scalar1=-lam_init, scalar2=None,
                                              op0=ALU.add), reads=[t_small], writes=[t_small])
        neglam = small[:, 5:6]
        P.dma("sp", small[:, 8:10], attn_sgT, writes=[t_small])
        P.op("dve", lambda e: e.tensor_scalar(out=small[:, 6:7], in0=small[:, 8 + j:9 + j], scalar1=1.0 - lam_init,
                                              scalar2=None, op0=ALU.mult), reads=[t_small], writes=[t_small])
        sgs = small[:, 6:7]

        NWB = 2
        wch = [ar.alloc([KD, 512], BF16) for _ in range(NWB)]
        t_wch = [Tok() for _ in range(NWB)]
        RQ = 3
        qs = [ar.alloc([512], F32) for _ in range(RQ)]
        ta = [ar.alloc([512], F32) for _ in range(RQ)]
        qr = [ar.alloc([512], BF16) for _ in range(RQ)]
        stt = [ar.alloc([32], F32) for _ in range(RQ)]
        t_q = [Tok() for _ in range(RQ)]
        wsrc = attn_w_in[j].rearrange("(k p) n -> p k n", p=128)
        steps = [(nch, i) for nch in range(6) for i in range(NTILE)]

        def stage1(s):
            nch, i = steps[s]
            ws = nch % NWB
            if i == 0:
                P.dma("pool", wch[ws], wsrc[:, :, nch * 512:(nch + 1) * 512], writes=[t_wch[ws]])
            pb = 4 + (s % 2)
            r = s % RQ
            for k in range(KD):
                P.op("pe", lambda e, k=k: e.matmul(
                    bank(pb), lhsT=uT[:, k, i * 128:(i + 1) * 128], rhs=wch[ws][:, k, :],
                    start=(k == 0), stop=(k == KD - 1)), reads=[t_uT, t_wch[ws]], writes=[t_ps[pb]])
            if nch >= 4:
                h0 = (nch - 4) * 4
                P.op("act", lambda e: e.activation(
                    out=vx[:, i, h0:h0 + 4, 0:128], in_=bank(pb).rearrange("p (h d) -> p h d", d=128),
                    func=AF.Copy), reads=[t_ps[pb]], writes=[t_vx])
                return
            P.op("act", lambda e: e.activation(out=qs[r], in_=bank(pb), func=AF.Copy), reads=[t_ps[pb]], writes=[t_q[r]])
            P.op("act", lambda e: e.activation(out=ta[r], in_=bank(pb), func=AF.Square), reads=[t_ps[pb]], writes=[t_q[r]])

        def stage2a(s):
            nch, i = steps[s]
            if nch >= 4:
                return
            r = s % RQ
            tq = t_q[r]
            P.op("dve", lambda e: e.tensor_reduce(
                out=stt[r][:, 0:8], in_=ta[r].rearrange("p (a b) -> p a b", b=64), axis=AX.X, op=ALU.add),
                reads=[tq], writes=[tq])
            P.op("dve", lambda e: e.tensor_scalar(
                out=stt[r][:, 8:16], in0=stt[r][:, 0:8], scalar1=1.0 / 64, scalar2=EPS, op0=ALU.mult, op1=ALU.add),
                reads=[tq], writes=[tq])
            P.op("pool", lambda e: e.tensor_tensor(
                out=stt[r][:, 16:24], in0=stt[r][:, 8:16], in1=cm05[:, 0:8], op=ALU.pow),
                reads=[tq, t_const], writes=[tq])

        def stage2b(s):
            nch, i = steps[s]
            if nch >= 4:
                return
            r = s % RQ
            tq = t_q[r]
            qk = nch // 2
            h0 = (nch % 2) * 4
            q3 = qs[r].rearrange("p (a b) -> p a b", b=64)
            P.op("dve", lambda e: e.tensor_tensor(
                out=q3, in0=q3, in1=bc(stt[r][:, 16:24].unsqueeze(2), [128, 8, 64]), op=ALU.mult),
                reads=[tq], writes=[tq])
            P.op("dve", lambda e: e.tensor_tensor(out=qs[r], in0=qs[r], in1=gt[:, qk, :], op=ALU.mult),
                 reads=[tq, t_gt], writes=[tq])
            x4 = qs[r].rearrange("p (a m d) -> p a m d", m=2, d=32)
            t4 = ta[r].rearrange("p (a m d) -> p a m d", m=2, d=32)
            r4 = qr[r].rearrange("p (a m d) -> p a m d", m=2, d=32)

            def tb(ti):
                return bc(cs_t[:, (0, 1, 1, 0)[ti], i, :].unsqueeze(1), [128, 8, 32])
            P.op("dve", lambda e: e.tensor_tensor(out=t4[:, :, 0, :], in0=x4[:, :, 0, :], in1=tb(0), op=ALU.mult),
                 reads=[tq, t_cs], writes=[tq])
            P.op("dve", lambda e: e.tensor_tensor(out=t4[:, :, 1, :], in0=x4[:, :, 1, :], in1=tb(1), op=ALU.mult),
                 reads=[tq, t_cs], writes=[tq])
            P.op("dve", lambda e: e.tensor_tensor(out=r4[:, :, 0, :], in0=t4[:, :, 0, :], in1=t4[:, :, 1, :], op=ALU.subtract),
                 reads=[tq], writes=[tq])
            P.op("dve", lambda e: e.tensor_tensor(out=t4[:, :, 0, :], in0=x4[:, :, 0, :], in1=tb(2), op=ALU.mult),
                 reads=[tq, t_cs], writes=[tq])
            P.op("dve", lambda e: e.tensor_tensor(out=t4[:, :, 1, :], in0=x4[:, :, 1, :], in1=tb(3), op=ALU.mult),
                 reads=[tq, t_cs], writes=[tq])
            P.op("dve", lambda e: e.tensor_tensor(out=r4[:, :, 1, :], in0=t4[:, :, 0, :], in1=t4[:, :, 1, :], op=ALU.add),
                 reads=[tq], writes=[tq])
            pbt = 6 + (s % 2)
            for hh in range(4):
                P.op("pe", lambda e, hh=hh: e.transpose(
                    out=bankb(pbt)[:, hh * 128:(hh + 1) * 128], in_=qr[r][:, hh * 128:(hh + 1) * 128],
                    identity=identb[:]), reads=[tq, t_const], writes=[t_ps[pbt]])
            dstT = (qT if qk == 0 else kT)
            P.op("act", lambda e: e.activation(
                out=dstT[:, h0:h0 + 4, i * 128:(i + 1) * 128],
                in_=bankb(pbt)[:, 0:512].rearrange("p (h t) -> p h t", t=128), func=AF.Copy),
                reads=[t_ps[pbt]], writes=[t_qT if qk == 0 else t_kT])

        stage1(0)
        stage2a(0)
        for s_ in range(len(steps)):
            if s_ + 1 < len(steps):
                stage1(s_ + 1)
            stage2b(s_)
            if s_ + 1 < len(steps):
                stage2a(s_ + 1)
        dump("qT", qT, [t_qT])
        dump("kT", kT, [t_kT])
        dump("vx", vx, [t_vx])
        P.barrier()
        ar.off = mark
        wout = ar.alloc([KD, D], BF16)
        t_wout = Tok("wout")
        P.dma("pool", wout, attn_w_out[j].rearrange("(k p) n -> p k n", p=128), writes=[t_wout])
        G1 = [ar.alloc([D], F32) for _ in range(2)]
        t_G1 = [Tok(), Tok()]
        load_gate(l, 2, b, G1[0], t_G1[0])
        load_gate(l, 2, 2, G1[1], t_G1[1])
        ru = ResUpd()
        osb = ar.alloc([4, D], F32)
        t_osb = Tok("osb")
        es = [[ar.alloc([512], BF16) for _ in range(2)] for _ in range(2)]
        t_es = [[Tok(), Tok()], [Tok(), Tok()]]
        fin = ar.alloc([8], F32)
        t_fin = Tok("fin")
        tsb = ar.alloc([128], F32)
        sqt = ar.alloc([D], F32)
        onb = ar.alloc([D], BF16)
        oT = ar.alloc([KD, 128], BF16)
        t_on = Tok("on")
        t_oT = Tok("oT")
        s8 = ar.alloc([32], F32)

        qranges = [(q0, 512, list(range(NTILE))) for q0 in range(0, S, 512)]
        if need_ctx:
            qranges.append((S, CT, [16, 17]))
        def head(h, q0, nq, ktiles, nqi):
            if True:
                def s_mm(ki):
                    kt = ktiles[ki]
                    for m in range(2):
                        pb = m * 2 + (ki % 2)
                        P.op("pe", lambda e, m=m, pb=pb, kt=kt: e.matmul(
                            psum[:, pb, 0:nq], lhsT=kT[m * 64:(m + 1) * 64, h, kt * 128:(kt + 1) * 128],
                            rhs=qT[m * 64:(m + 1) * 64, h, q0:q0 + nq], start=True, stop=True),
                            reads=[t_kT, t_qT], writes=[t_ps[pb]])
                        P.op("act", lambda e, m=m, pb=pb, ki=ki: e.activation(
                            out=es[m][ki % 2][:, 0:nq], in_=psum[:, pb, 0:nq], func=AF.Exp, scale=0.125),
                            reads=[t_ps[pb]], writes=[t_es[m][ki % 2]])
                s_mm(0)
                for ki in range(len(ktiles)):
                    if ki + 1 < len(ktiles):
                        s_mm(ki + 1)
                    kt = ktiles[ki]
                    for m in range(2):
                        for qi in range(nqi):
                            gi = m * 4 + qi
                            pb = 4 + gi // 3
                            c0 = (gi % 3) * 129
                            P.op("pe", lambda e, m=m, qi=qi, pb=pb, c0=c0, kt=kt, ki=ki, gi=gi: e.matmul(
                                psum[:, pb, c0:c0 + 129], lhsT=es[m][ki % 2][:, qi * 128:(qi + 1) * 128],
                                rhs=vx[:, kt, h, 0:129], start=(ki == 0 and (gi % 3 == 0 or (nqi < 4 and qi == 0))),
                                stop=(ki == len(ktiles) - 1), skip_group_check=True),
                                reads=[t_es[m][ki % 2], t_vx], writes=[t_ps[pb]])
                for qi in range(nqi):
                    g0, g1 = qi, 4 + qi
                    pb0, c00 = 4 + g0 // 3, (g0 % 3) * 129
                    pb1, c01 = 4 + g1 // 3, (g1 % 3) * 129
                    P.op("dve", lambda e, pb0=pb0, c00=c00: e.reciprocal(out=fin[:, 0:1], in_=psum[:, pb0, c00 + 128:c00 + 129]),
                         reads=[t_ps[pb0]], writes=[t_fin])
                    P.op("dve", lambda e, pb1=pb1, c01=c01: e.reciprocal(out=fin[:, 1:2], in_=psum[:, pb1, c01 + 128:c01 + 129]),
                         reads=[t_ps[pb1]], writes=[t_fin])
                    P.op("dve", lambda e: e.tensor_tensor(out=fin[:, 2:3], in0=fin[:, 1:2], in1=neglam, op=ALU.mult),
                         reads=[t_fin, t_small], writes=[t_fin])
                    P.op("dve", lambda e, pb1=pb1, c01=c01: e.tensor_scalar(
                        out=tsb, in0=psum[:, pb1, c01:c01 + 128], scalar1=fin[:, 2:3], scalar2=None, op0=ALU.mult),
                        reads=[t_ps[pb1], t_fin], writes=[t_fin])
                    P.op("dve", lambda e, pb0=pb0, c00=c00, qi=qi: e.scalar_tensor_tensor(
                        out=osb[:, qi, h * 128:(h + 1) * 128], in0=psum[:, pb0, c00:c00 + 128], scalar=fin[:, 0:1],
                        in1=tsb, op0=ALU.mult, op1=ALU.add), reads=[t_ps[pb0], t_fin], writes=[t_osb])
        for (q0, nq, ktiles) in qranges:
            nqi = nq // 128
            for h in range(8):
                head(h, q0, nq, ktiles, nqi)
            dump("osb", osb, [t_osb])
            for qi in range(nqi):
                i = q0 // 128 + qi
                ru.prefetch(b, i)
                P.op("dve", lambda e, qi=qi: e.tensor_tensor(out=sqt, in0=osb[:, qi, :], in1=osb[:, qi, :], op=ALU.mult),
                     reads=[t_osb], writes=[t_on])
                P.op("dve", lambda e: e.tensor_reduce(out=s8[:, 0:8], in_=sqt.rearrange("p (a b) -> p a b", b=128),
                                                      axis=AX.X, op=ALU.add), reads=[t_on], writes=[t_on])
                P.op("dve", lambda e: e.tensor_scalar(out=s8[:, 8:16], in0=s8[:, 0:8], scalar1=1.0 / 128, scalar2=EPS,
                                                      op0=ALU.mult, op1=ALU.add), reads=[t_on], writes=[t_on])
                P.op("pool", lambda e: e.tensor_tensor(out=s8[:, 16:24], in0=s8[:, 8:16], in1=cm05[:, 0:8], op=ALU.pow),
                     reads=[t_on, t_const], writes=[t_on])
                P.op("dve", lambda e, qi=qi: e.tensor_tensor(
                    out=onb.rearrange("p (a b) -> p a b", b=128), in0=osb[:, qi, :].rearrange("p (a b) -> p a b", b=128),
                    in1=bc(s8[:, 16:24].unsqueeze(2), [128, 8, 128]), op=ALU.mult), reads=[t_on, t_osb], writes=[t_on])
                for hh in range(8):
                    P.op("pe", lambda e, hh=hh: e.transpose(
                        out=bankb(7)[:, hh * 128:(hh + 1) * 128], in_=onb[:, hh * 128:(hh + 1) * 128],
                        identity=identb[:]), reads=[t_on, t_const], writes=[t_ps[7]])
                P.op("act", lambda e: e.activation(out=oT, in_=bankb(7).rearrange("p (h t) -> p h t", t=128),
                                                   func=AF.Copy, scale=sgs), reads=[t_ps[7], t_small], writes=[t_oT])
                for nh in range(2):
                    for hh in range(8):
                        P.op("pe", lambda e, nh=nh, hh=hh: e.matmul(
                            bank(nh), lhsT=oT[:, hh, :], rhs=wout[:, hh, nh * 512:(nh + 1) * 512],
                            start=(hh == 0), stop=(hh == 7)), reads=[t_oT, t_wout], writes=[t_ps[nh]])
                gs = 0 if i < 16 else 1
                ru.apply(b, i, [(bank(0), 0, 512), (bank(1), 512, 512)], [t_ps[0], t_ps[1]], G1[gs], t_G1[gs])
        P.barrier()

    def lru_layer(l, b, need_ctx):
        ar.off = 0
        layer_setup(l)
        uT = ar.alloc([KD, TB], BF16)
        t_uT = Tok("uT")
        mark0 = ar.off
        nb = NormBufs(False)
        for i in range(NTILE):
            norm_tile(nb, l, 0, b, i, uT, t_uT, i * 128)
        P.barrier()
        ar.off = mark0
        mT = ar.alloc([10, TB], BF16)
        t_mT = Tok("mT")
        cw = ar.alloc([10, 4], F32)
        cb = ar.alloc([10], F32)
        bax = ar.alloc([2, 2, 10], F32)
        lam = ar.alloc([2, 10], F32)
        nsp = ar.alloc([2, 2, 10], F32)
        t_c = Tok("lruc")
        P.dma("sp", cw, lru_cwT, writes=[t_c])
        P.dma("sp", cb, lru_cbT, writes=[t_c])
        P.dma("sp", bax[:, 0], lru_baT, writes=[t_c])
        P.dma("sp", bax[:, 1], lru_bxT, writes=[t_c])
        P.dma("sp", lam, lru_lamT, writes=[t_c])
        P.op("act", lambda e: e.activation(out=lam, in_=lam, func=AF.Exp, scale=-1.0), reads=[t_c], writes=[t_c])
        P.op("act", lambda e: e.activation(out=lam, in_=lam, func=AF.Ln, bias=1.0), reads=[t_c], writes=[t_c])
        P.op("dve", lambda e: e.tensor_scalar(out=nsp[:, 0], in0=lam, scalar1=-8.0, scalar2=None, op0=ALU.mult),
             reads=[t_c], writes=[t_c])
        P.op("dve", lambda e: e.tensor_scalar(out=nsp[:, 1], in0=lam, scalar1=-16.0, scalar2=None, op0=ALU.mult),
             reads=[t_c], writes=[t_c])
        NWB = 2
        win = [ar.alloc([KD, 256], BF16) for _ in range(NWB)]
        t_win = [Tok() for _ in range(NWB)]
        wbd = [ar.alloc([4, 128], BF16) for _ in range(NWB)]
        t_wbd = [Tok() for _ in range(NWB)]
        xr = ar.alloc([LW], F32)
        xc = ar.alloc([LW], F32)
        xcb = ar.alloc([LW], BF16)
        Abuf = ar.alloc([LW], F32)
        Bbuf = ar.alloc([LW], F32)
        Tbuf = ar.alloc([LW], F32)
        H0 = ar.alloc([LW], F32)
        gate = ar.alloc([TB], BF16)
        t_x = Tok("lrux")
        t_A, t_B, t_T, t_H0, t_g = Tok(), Tok(), Tok(), Tok(), Tok()
        P.op("pool", lambda e: e.memset(xr, 0.0), writes=[t_x])
        wsrc = lru_w_in[0].rearrange("(k p) n -> p k n", p=128)
        segs = [(LAT0 + q, q, 512) for q in range(0, S, 512)] + [(CTX0, S, CT)]
        cnt = 0
        for g in range(10):
            ws = g % NWB
            P.dma("pool", win[ws][:, :, 0:128], wsrc[:, :, g * 128:(g + 1) * 128], writes=[t_win[ws]])
            P.dma("pool", win[ws][:, :, 128:256], wsrc[:, :, LRU_W + g * 128:LRU_W + (g + 1) * 128], writes=[t_win[ws]])
            for d in range(2):
                P.dma("pool", wbd[ws][:, 2 * d, :], lru_w_a[0, d, g], writes=[t_wbd[ws]])
                P.dma("pool", wbd[ws][:, 2 * d + 1, :], lru_w_x[0, d, g], writes=[t_wbd[ws]])
            for (pc, tc, n) in segs:
                for half in range(2):
                    pb = cnt % 4
                    cnt += 1
                    for k in range(KD):
                        P.op("pe", lambda e, k=k, pb=pb, half=half, tc=tc, n=n, ws=ws: e.matmul(
                            psum[:, pb, 0:n], lhsT=win[ws][:, k, half * 128:(half + 1) * 128],
                            rhs=uT[:, k, tc:tc + n], start=(k == 0), stop=(k == KD - 1)),
                            reads=[t_uT, t_win[ws]], writes=[t_ps[pb]])
                    if half == 0:
                        P.op("act", lambda e, pb=pb, tc=tc, n=n: e.activation(
                            out=gate[:, tc:tc + n], in_=psum[:, pb, 0:n], func=AF.Gelu_apprx_tanh),
                            reads=[t_ps[pb]], writes=[t_g])
                    else:
                        P.op("act", lambda e, pb=pb, pc=pc, n=n: e.activation(
                            out=xr[:, pc:pc + n], in_=psum[:, pb, 0:n], func=AF.Copy),
                            reads=[t_ps[pb]], writes=[t_x])
            W0, W1 = 2, 2309
            nW = W1 - W0
            P.op("dve", lambda e, g=g: e.tensor_scalar(
                out=xc[:, W0:W1], in0=xr[:, W0 - 2:W1 - 2], scalar1=cw[:, g, 0:1], scalar2=cb[:, g:g + 1],
                op0=ALU.mult, op1=ALU.add), reads=[t_x, t_c], writes=[t_x])
            for k in range(1, 4):
                P.op("dve", lambda e, g=g, k=k: e.scalar_tensor_tensor(
                    out=xc[:, W0:W1], in0=xr[:, W0 - 2 + k:W1 - 2 + k], scalar=cw[:, g, k:k + 1], in1=xc[:, W0:W1],
                    op0=ALU.mult, op1=ALU.add), reads=[t_x, t_c], writes=[t_x])
            P.op("pool", lambda e: e.tensor_copy(out=xcb[:, W0:W1], in_=xc[:, W0:W1]), reads=[t_x], writes=[t_x])
            for d in range(2):
                for (pc, tc, n) in segs:
                    for ax in range(2):
                        pb = 4 + cnt % 4
                        cnt += 1
                        P.op("pe", lambda e, pb=pb, pc=pc, n=n, ws=ws, d=d, ax=ax: e.matmul(
                            psum[:, pb, 0:n], lhsT=wbd[ws][:, 2 * d + ax, :], rhs=xcb[:, pc:pc + n],
                            start=True, stop=True), reads=[t_x, t_wbd[ws]], writes=[t_ps[pb]])
                        dstb = Abuf if ax == 0 else Bbuf
                        P.op("act", lambda e, pb=pb, pc=pc, n=n, dstb=dstb, ax=ax, d=d, g=g: e.activation(
                            out=dstb[:, pc:pc + n], in_=psum[:, pb, 0:n], func=AF.Sigmoid,
                            bias=bax[:, ax, d, g:g + 1]), reads=[t_ps[pb], t_c], writes=[t_A if ax == 0 else t_B])
                P.op("pool", lambda e: e.tensor_tensor(out=Bbuf[:, W0:W1], in0=Bbuf[:, W0:W1], in1=xc[:, W0:W1], op=ALU.mult),
                     reads=[t_B, t_x], writes=[t_B])
                P.op("act", lambda e, d=d, g=g: e.activation(out=Tbuf[:, W0:W1], in_=Abuf[:, W0:W1], func=AF.Exp,
                                                             scale=nsp[:, 1, d, g:g + 1]), reads=[t_A, t_c], writes=[t_T])
                P.op("act", lambda e: e.activation(out=Tbuf[:, W0:W1], in_=Tbuf[:, W0:W1], func=AF.Sqrt, scale=-1.0, bias=1.0),
                     reads=[t_T], writes=[t_T])
                rc = CTX0 if d == 0 else CTX0 + CT - 1
                P.op("dve", lambda e, rc=rc: e.memset(Tbuf[:, rc:rc + 1], 1.0), writes=[t_T])
                P.op("dve", lambda e: e.tensor_tensor(out=Bbuf[:, W0:W1], in0=Bbuf[:, W0:W1], in1=Tbuf[:, W0:W1], op=ALU.mult),
                     reads=[t_B, t_T], writes=[t_B])
                P.op("act", lambda e, d=d, g=g: e.activation(out=Abuf[:, W0:W1], in_=Abuf[:, W0:W1], func=AF.Exp,
                                                             scale=nsp[:, 0, d, g:g + 1]), reads=[t_A, t_c], writes=[t_A])
                Hd = H0 if d == 0 else Tbuf
                t_Hd = t_H0 if d == 0 else t_T
                csl = slice(CTX0, CTX0 + CT)
                lsl = slice(LAT0, LAT0 + S)
                if d == 0:
                    P.op("dve", lambda e, Hd=Hd: e.tensor_tensor_scan(
                        out=Hd[:, csl], data0=Abuf[:, csl], data1=Bbuf[:, csl], initial=0.0, op0=ALU.mult, op1=ALU.add),
                        reads=[t_A, t_B], writes=[t_Hd])
                    P.op("dve", lambda e, Hd=Hd: e.tensor_tensor_scan(
                        out=Hd[:, lsl], data0=Abuf[:, lsl], data1=Bbuf[:, lsl], initial=Hd[:, CTX0 + CT - 1:CTX0 + CT],
                        op0=ALU.mult, op1=ALU.add), reads=[t_A, t_B, t_Hd], writes=[t_Hd])
                else:
                    P.op("dve", lambda e, Hd=Hd: e.tensor_tensor_scan(
                        out=rev2d(Hd[:, csl]), data0=rev2d(Abuf[:, csl]), data1=rev2d(Bbuf[:, csl]), initial=0.0,
                        op0=ALU.mult, op1=ALU.add), reads=[t_A, t_B], writes=[t_Hd])
                    P.op("dve", lambda e, Hd=Hd: e.tensor_tensor_scan(
                        out=rev2d(Hd[:, lsl]), data0=rev2d(Abuf[:, lsl]), data1=rev2d(Bbuf[:, lsl]),
                        initial=Hd[:, CTX0:CTX0 + 1], op0=ALU.mult, op1=ALU.add), reads=[t_A, t_B, t_Hd], writes=[t_Hd])
            P.op("pool", lambda e: e.tensor_tensor(out=H0[:, W0:W1], in0=H0[:, W0:W1], in1=Tbuf[:, W0:W1], op=ALU.add),
                 reads=[t_H0, t_T], writes=[t_H0])
            P.op("dve", lambda e, g=g: e.tensor_tensor(out=mT[:, g, 0:S], in0=H0[:, LAT0:LAT0 + S], in1=gate[:, 0:S], op=ALU.mult),
                 reads=[t_H0, t_g], writes=[t_mT])
            P.op("dve", lambda e, g=g: e.tensor_tensor(out=mT[:, g, S:TB], in0=H0[:, CTX0:CTX0 + CT], in1=gate[:, S:TB], op=ALU.mult),
                 reads=[t_H0, t_g], writes=[t_mT])
        P.barrier()
        ar.off = mark0 + 10 * TB * 2
        wout = ar.alloc([10, D], BF16)
        t_wout = Tok("wout")
        P.dma("pool", wout, lru_w_out[0].rearrange("(k p) n -> p k n", p=128), writes=[t_wout])
        G1 = [ar.alloc([D], F32) for _ in range(2)]
        t_G1 = [Tok(), Tok()]
        load_gate(l, 2, b, G1[0], t_G1[0])
        load_gate(l, 2, 2, G1[1], t_G1[1])
        ru = ResUpd()
        ntl = NTILE if need_ctx else 16
        for i in range(ntl):
            ru.prefetch(b, i)
            pbs = (2 * (i % 2), 2 * (i % 2) + 1)
            for nh in range(2):
                for g in range(10):
                    P.op("pe", lambda e, nh=nh, g=g, i=i, pbs=pbs: e.matmul(
                        bank(pbs[nh]), lhsT=mT[:, g, i * 128:(i + 1) * 128], rhs=wout[:, g, nh * 512:(nh + 1) * 512],
                        start=(g == 0), stop=(g == 9)), reads=[t_mT, t_wout], writes=[t_ps[pbs[nh]]])
            gs = 0 if i < 16 else 1
            ru.apply(b, i, [(bank(pbs[0]), 0, 512), (bank(pbs[1]), 512, 512)], [t_ps[pbs[0]], t_ps[pbs[1]]],
                     G1[gs], t_G1[gs])
        P.barrier()

    def pool_layer(l, b, need_ctx):
        ar.off = 0
        layer_setup(l)
        uT = ar.alloc([KD, TB], BF16)
        t_uT = Tok("uT")
        mark0 = ar.off
        nb = NormBufs(False)
        for i in range(NTILE):
            norm_tile(nb, l, 0, b, i, uT, t_uT, i * 128)
        P.barrier()
        ar.off = mark0
        pT = ar.alloc([KD, TB], BF16)
        t_pT = Tok("pT")
        edge = ar.alloc([4, 2, 8], F32)
        t_e = Tok("edge")
        P.dma("sp", edge, pool_edge, writes=[t_e])
        NWB = 2
        win = [ar.alloc([KD, 128], BF16) for _ in range(NWB)]
        t_win = [Tok() for _ in range(NWB)]
        PADW = 8
        XL = [ar.alloc([S + 16], F32) for _ in range(3)]
        XC = [ar.alloc([CT + 16], F32) for _ in range(3)]
        t_X = Tok("poolx")
        for t in XL + XC:
            P.op("pool", lambda e, t=t: e.memset(t, 0.0), writes=[t_X])
        wsrc = pool_w_in[0].rearrange("(k p) n -> p k n", p=128)
        cnt = 0
        for c in range(8):
            gidx = c // 2
            w = (2, 4, 8, 16)[gidx]
            ws = c % NWB
            P.dma("pool", win[ws], wsrc[:, :, c * 128:(c + 1) * 128], writes=[t_win[ws]])
            for (tc, n, X, off) in [(q, 512, XL, q) for q in range(0, S, 512)] + [(S, CT, XC, 0)]:
                pb = cnt % 4
                cnt += 1
                for k in range(KD):
                    P.op("pe", lambda e, k=k, pb=pb, tc=tc, n=n, ws=ws: e.matmul(
                        psum[:, pb, 0:n], lhsT=win[ws][:, k, :], rhs=uT[:, k, tc:tc + n],
                        start=(k == 0), stop=(k == KD - 1)), reads=[t_uT, t_win[ws]], writes=[t_ps[pb]])
                P.op("act", lambda e, pb=pb, n=n, X=X, off=off: e.activation(
                    out=X[0][:, PADW + off:PADW + off + n], in_=psum[:, pb, 0:n], func=AF.Copy),
                    reads=[t_ps[pb]], writes=[t_X])
            for (X, n, tc) in ((XL, S, 0), (XC, CT, S)):
                x0 = X[0]
                cur, other = X[1], X[2]
                lo, hi = 1, PADW + n + 8
                P.op("dve", lambda e, x0=x0, cur=cur, lo=lo, hi=hi: e.tensor_tensor(
                    out=cur[:, lo:hi], in0=x0[:, lo - 1:hi - 1], in1=x0[:, lo:hi], op=ALU.add), reads=[t_X], writes=[t_X])
                step = 1
                ww = 2
                while ww < w:
                    sp_ = ww // 2
                    lo, hi = lo + sp_, hi - sp_
                    P.op("dve", lambda e, cur=cur, other=other, lo=lo, hi=hi, sp_=sp_: e.tensor_tensor(
                        out=other[:, lo:hi], in0=cur[:, lo - sp_:hi - sp_], in1=cur[:, lo + sp_:hi + sp_], op=ALU.add),
                        reads=[t_X], writes=[t_X])
                    cur, other = other, cur
                    ww *= 2
                P.op("dve", lambda e, cur=cur, x0=x0, n=n, tc=tc, c=c, w=w: e.scalar_tensor_tensor(
                    out=pT[:, c, tc:tc + n], in0=cur[:, PADW:PADW + n], scalar=1.0 / w, in1=x0[:, PADW:PADW + n],
                    op0=ALU.mult, op1=ALU.subtract), reads=[t_X], writes=[t_pT])
                hw = w // 2
                for side in range(2):
                    a0 = PADW if side == 0 else PADW + n - 8
                    o0 = tc if side == 0 else tc + n - 8
                    P.op("dve", lambda e, cur=cur, a0=a0, side=side, gidx=gidx, other=other: e.tensor_tensor(
                        out=other[:, 0:8], in0=cur[:, a0:a0 + 8], in1=edge[:, gidx, side, :], op=ALU.mult),
                        reads=[t_X, t_e], writes=[t_X])
                    P.op("dve", lambda e, x0=x0, a0=a0, o0=o0, c=c, other=other: e.tensor_tensor(
                        out=pT[:, c, o0:o0 + 8], in0=other[:, 0:8], in1=x0[:, a0:a0 + 8], op=ALU.subtract),
                        reads=[t_X], writes=[t_pT])
        P.barrier()
        ar.off = mark0 + KD * TB * 2
        wg = ar.alloc([4, 2, 256], BF16)
        t_wg = Tok("wg")
        for g in range(4):
            P.dma("pool", wg[:, g], pool_w_grp[0, g].rearrange("(k p) n -> p k n", p=128), writes=[t_wg])
        G1 = [ar.alloc([D], F32) for _ in range(2)]
        t_G1 = [Tok(), Tok()]
        load_gate(l, 2, b, G1[0], t_G1[0])
        load_gate(l, 2, 2, G1[1], t_G1[1])
        psc = ar.alloc([D], F32)
        t_psc = Tok("psc")
        a = pool_scale[0:1, :]
        P.dma("sp", psc, AP(a.tensor, a.offset, [[0, 128], [1, D]]), writes=[t_psc])
        for s in range(2):
            P.op("dve", lambda e, s=s: e.tensor_tensor(out=G1[s], in0=G1[s], in1=psc, op=ALU.mult),
                 reads=[t_psc, t_G1[s]], writes=[t_G1[s]])
        ru = ResUpd()
        ntl = NTILE if need_ctx else 16
        for i in range(ntl):
            ru.prefetch(b, i)
            pbs = (2 * (i % 2), 2 * (i % 2) + 1)
            for g in range(4):
                pb = pbs[g // 2]
                c0 = (g % 2) * 256
                for kc in range(2):
                    P.op("pe", lambda e, g=g, kc=kc, pb=pb, c0=c0, i=i: e.matmul(
                        psum[:, pb, c0:c0 + 256], lhsT=pT[:, 2 * g + kc, i * 128:(i + 1) * 128], rhs=wg[:, g, kc, :],
                        start=(kc == 0 and g % 2 == 0), stop=(kc == 1), skip_group_check=True),
                        reads=[t_pT, t_wg], writes=[t_ps[pb]])
            gs = 0 if i < 16 else 1
            ru.apply(b, i, [(bank(pbs[0]), 0, 512), (bank(pbs[1]), 512, 512)], [t_ps[pbs[0]], t_ps[pbs[1]]],
                     G1[gs], t_G1[gs])
        P.barrier()

    I32 = mybir.dt.int32

    def moe_sparse(l, last):
        tl = [(b, i) for b in range(2) for i in range(NTILE if not last else 16)]
        NTT = len(tl)
        NTE = NTT * 2 * 128 // 512 + 15
        ar.off = 0
        layer_setup(l)
        S1a = ar.alloc([NTT, 16], F32)
        S2a = ar.alloc([NTT, 16], F32)
        wts = ar.alloc([NTT, 2], F32)
        posa = ar.alloc([NTT, 16], F32)
        base = ar.alloc([16], F32)
        sidx = ar.alloc([NTT, 2], I32)
        widx = ar.alloc([NTE, 2], I32)
        t_S, t_base, t_idx = Tok("S"), Tok("base"), Tok("idx")
        G2 = [ar.alloc([D], F32) for _ in range(3)]
        t_G2 = [Tok() for _ in range(3)]
        for s_ in range(3):
            load_gate(l, 5, s_, G2[s_], t_G2[s_])
        mark_keep = ar.off
        ub = ar.alloc([NTT, D], BF16)
        t_ub = [Tok() for _ in range(NTT)]
        At = [ar.alloc([D], F32) for _ in range(3)]
        Bt = [ar.alloc([D], F32) for _ in range(3)]
        t_AB = Tok("ABtok")
        gn = ar.alloc([D], F32)
        a_ = normg_raw[l, 1:2, :]
        P.dma("sp", gn, AP(a_.tensor, a_.offset, [[0, 128], [1, D]]), writes=[t_AB])
        for s_ in range(3):
            load_gate(l, 4, s_, At[s_], t_AB)
            load_gate(l, 3, s_, Bt[s_], t_AB)
            P.op("dve", lambda e, s_=s_: e.scalar_tensor_tensor(out=At[s_], in0=At[s_], scalar=1.0, in1=gn,
                                                                op0=ALU.add, op1=ALU.mult), reads=[t_AB], writes=[t_AB])
        wr = ar.alloc([KD, 20], F32)
        t_wr = Tok("wr")
        P.dma("sp", wr, moe_router[l].rearrange("(k p) n -> p k n", p=128), writes=[t_wr])
        utri = ar.alloc([128], BF16)
        onesb = ar.alloc([128], BF16)
        cst = ar.alloc([128 + 40 + 1], F32)
        t_cst = Tok("cst")
        P.dma("sp", cst[:, 0:128], utri_d, writes=[t_cst])
        P.dma("sp", cst[:, 128:168], tvals_d, writes=[t_cst])
        P.dma("sp", cst[:, 168:169], pidx_d, writes=[t_cst])
        P.op("dve", lambda e: e.tensor_copy(out=utri, in_=cst[:, 0:128]), reads=[t_cst], writes=[t_cst])
        P.op("dve", lambda e: e.memset(onesb, 1.0), writes=[t_cst])
        P.op("dve", lambda e: e.memset(base, 0.0), writes=[t_base])
        NX, NH, NU = 4, 3, 3
        xts = [ar.alloc([D], F32) for _ in range(NX)]
        t_xts = [Tok() for _ in range(NX)]
        sts = [ar.alloc([4], F32) for _ in range(NX)]
        t_sts = [Tok() for _ in range(NX)]
        junk = ar.alloc([D], BF16)
        t_junk = Tok()
        xhs = [ar.alloc([D], F32) for _ in range(NH)]
        t_xhs = [Tok() for _ in range(NH)]
        u32s = [ar.alloc([KD, 128], F32) for _ in range(NU)]
        t_u32s = [Tok() for _ in range(NU)]
        rt = [ar.alloc([96], F32) for _ in range(2)]
        t_rt = [Tok(), Tok()]
        mb = [ar.alloc([16], BF16) for _ in range(2)]
        utmp = [ar.alloc([D], F32) for _ in range(2)]
        t_ut = [Tok(), Tok()]

        def stA1(ti):
            b, i = tl[ti]
            xt, st, xh = xts[ti % NX], sts[ti % NX], xhs[ti % NH]
            t_xt, t_st, t_xh = t_xts[ti % NX], t_sts[ti % NX], t_xhs[ti % NH]
            r0 = rows(b, i)
            P.dma("sp", xt, res[r0:r0 + 128, :], reads=[t_res[b][i]], writes=[t_xt])
            P.op("act", lambda e: e.activation(out=junk, in_=xt, func=AF.Square, accum_out=st[:, 0:1]),
                 reads=[t_xt], writes=[t_junk, t_st])
            P.op("dve", lambda e: e.tensor_scalar(out=st[:, 1:2], in0=st[:, 0:1], scalar1=1.0 / D, scalar2=EPS,
                                                  op0=ALU.mult, op1=ALU.add), reads=[t_st], writes=[t_st])
            P.op("pool", lambda e: e.tensor_tensor(out=st[:, 2:3], in0=st[:, 1:2], in1=cm05[:, 0:1], op=ALU.pow),
                 reads=[t_st, t_const], writes=[t_st])
            P.op("act", lambda e: e.activation(out=xh, in_=xt, func=AF.Copy, scale=st[:, 2:3]),
                 reads=[t_xt, t_st], writes=[t_xh])
            pb0 = 2 * (ti % 2)
            for k in range(KD):
                pb = pb0 + k // 4
                P.op("pe", lambda e, k=k, pb=pb: e.transpose(
                    out=psum[:, pb, (k % 4) * 128:(k % 4 + 1) * 128], in_=xh[:, k * 128:(k + 1) * 128],
                    identity=ident32[:]), reads=[t_xh, t_const], writes=[t_ps[pb]])

        def stA2(ti):
            b, i = tl[ti]
            xh, u32 = xhs[ti % NH], u32s[ti % NU]
            t_xh, t_u32 = t_xhs[ti % NH], t_u32s[ti % NU]
            s_ = mset(b, i)
            s2 = ti % 2
            pb0 = 2 * (ti % 2)
            A = Amod[:, 1, s_, :]
            B = Bmod(l, 1, s_)
            for k in range(KD):
                pb = pb0 + k // 4
                P.op("dve", lambda e, k=k, pb=pb: e.tensor_scalar(
                    out=u32[:, k, :], in0=psum[:, pb, (k % 4) * 128:(k % 4 + 1) * 128],
                    scalar1=A[:, k:k + 1], scalar2=B[:, k:k + 1], op0=ALU.mult, op1=ALU.add),
                    reads=[t_ps[pb], t_amod, t_mods], writes=[t_u32])
            P.op("pool", lambda e: e.tensor_tensor(out=utmp[s2], in0=xh, in1=At[s_], op=ALU.mult),
                 reads=[t_xh, t_AB], writes=[t_ut[s2]])
            P.op("pool", lambda e: e.tensor_tensor(out=ub[:, ti, :], in0=utmp[s2], in1=Bt[s_], op=ALU.add),
                 reads=[t_ut[s2], t_AB], writes=[t_ub[ti]])

        def stB(ti):
            u32, t_u32 = u32s[ti % NU], t_u32s[ti % NU]
            s2 = ti % 2
            R = rt[s2]
            tr = t_rt[s2]
            pb = 4 + s2
            for k in range(KD):
                P.op("pe", lambda e, k=k: e.matmul(
                    psum[:, pb, 0:20], lhsT=u32[:, k, :], rhs=wr[:, k, :], start=(k == 0), stop=(k == KD - 1)),
                    reads=[t_u32, t_wr], writes=[t_ps[pb]])
            P.op("dve", lambda e: e.tensor_copy(out=R[:, 0:20], in_=psum[:, pb, 0:20]), reads=[t_ps[pb]], writes=[tr])
            P.op("dve", lambda e: e.tensor_reduce(out=R[:, 20:21], in_=R[:, 0:4], axis=AX.X, op=ALU.max), reads=[tr], writes=[tr])
            P.op("dve", lambda e: e.tensor_scalar(out=R[:, 21:22], in0=R[:, 20:21], scalar1=-1.0, scalar2=None, op0=ALU.mult), reads=[tr], writes=[tr])
            P.op("dve", lambda e: e.tensor_scalar(out=R[:, 24:28], in0=R[:, 0:4], scalar1=R[:, 20:21], scalar2=None, op0=ALU.is_equal), reads=[tr], writes=[tr])
            P.op("act", lambda e: e.activation(out=R[:, 28:32], in_=R[:, 0:4], func=AF.Exp, bias=R[:, 21:22], accum_out=R[:, 32:33]), reads=[tr], writes=[tr])
            P.op("dve", lambda e: e.tensor_tensor(
                out=R[:, 36:52].rearrange("p (g j) -> p g j", j=4), in0=R[:, 4:20].rearrange("p (g j) -> p g j", j=4),
                in1=bc(R[:, 24:28].unsqueeze(2), [128, 4, 4]), op=ALU.mult), reads=[tr], writes=[tr])
            P.op("dve", lambda e: e.tensor_reduce(out=R[:, 52:56], in_=R[:, 36:52].rearrange("p (g j) -> p j g", j=4),
                                                  axis=AX.X, op=ALU.add), reads=[tr], writes=[tr])
            P.op("dve", lambda e: e.tensor_reduce(out=R[:, 56:57], in_=R[:, 52:56], axis=AX.X, op=ALU.max), reads=[tr], writes=[tr])
            P.op("dve", lambda e: e.tensor_scalar(out=R[:, 57:58], in0=R[:, 56:57], scalar1=-1.0, scalar2=None, op0=ALU.mult), reads=[tr], writes=[tr])
            P.op("dve", lambda e: e.tensor_scalar(out=R[:, 60:64], in0=R[:, 52:56], scalar1=R[:, 56:57], scalar2=None, op0=ALU.is_equal), reads=[tr], writes=[tr])
            P.op("dve", lambda e: e.scalar_tensor_tensor(out=R[:, 64:68], in0=R[:, 60:64], scalar=-1e30, in1=R[:, 52:56],
                                                         op0=ALU.mult, op1=ALU.add), reads=[tr], writes=[tr])
            P.op("dve", lambda e: e.tensor_reduce(out=R[:, 68:69], in_=R[:, 64:68], axis=AX.X, op=ALU.max), reads=[tr], writes=[tr])
            P.op("dve", lambda e: e.tensor_scalar(out=R[:, 72:76], in0=R[:, 64:68], scalar1=R[:, 68:69], scalar2=None, op0=ALU.is_equal), reads=[tr], writes=[tr])
            P.op("act", lambda e: e.activation(out=R[:, 76:77], in_=R[:, 68:69], func=AF.Exp, bias=R[:, 57:58]), reads=[tr], writes=[tr])
            P.op("dve", lambda e: e.tensor_tensor(
                out=S1a[:, ti, :].rearrange("p (g j) -> p g j", j=4), in0=bc(R[:, 24:28].unsqueeze(2), [128, 4, 4]),
                in1=bc(R[:, 60:64].unsqueeze(1), [128, 4, 4]), op=ALU.mult), reads=[tr], writes=[t_S])
            P.op("dve", lambda e: e.tensor_tensor(
                out=S2a[:, ti, :].rearrange("p (g j) -> p g j", j=4), in0=bc(R[:, 24:28].unsqueeze(2), [128, 4, 4]),
                in1=bc(R[:, 72:76].unsqueeze(1), [128, 4, 4]), op=ALU.mult), reads=[tr], writes=[t_S])
            P.op("dve", lambda e: e.tensor_tensor(out=mb[s2], in0=S1a[:, ti, :], in1=S2a[:, ti, :], op=ALU.add),
                 reads=[t_S], writes=[tr])
            pbp = 6 + s2
            P.op("pe", lambda e: e.matmul(psum[:, pbp, 0:16], lhsT=utri, rhs=mb[s2], start=True, stop=True),
                 reads=[tr, t_cst], writes=[t_ps[pbp]])
            P.op("pe", lambda e: e.matmul(psum[:, pbp, 16:32], lhsT=onesb, rhs=mb[s2], start=False, stop=True, skip_group_check=True),
                 reads=[tr, t_cst], writes=[t_ps[pbp]])
            P.op("dve", lambda e: e.reciprocal(out=R[:, 33:34], in_=R[:, 32:33]), reads=[tr], writes=[tr])
            P.op("dve", lambda e: e.tensor_scalar(out=R[:, 77:78], in0=R[:, 76:77], scalar1=1.0, scalar2=None, op0=ALU.add), reads=[tr], writes=[tr])
            P.op("dve", lambda e: e.reciprocal(out=R[:, 78:79], in_=R[:, 77:78]), reads=[tr], writes=[tr])
            P.op("dve", lambda e: e.tensor_tensor(out=wts[:, ti, 0:1], in0=R[:, 78:79], in1=R[:, 33:34], op=ALU.mult), reads=[tr], writes=[tr, t_S])
            P.op("dve", lambda e: e.tensor_tensor(out=wts[:, ti, 1:2], in0=wts[:, ti, 0:1], in1=R[:, 76:77], op=ALU.mult), reads=[tr, t_S], writes=[t_S])

        def stC(ti):
            pbp = 6 + ti % 2
            P.op("dve", lambda e: e.tensor_tensor(out=posa[:, ti, :], in0=psum[:, pbp, 0:16], in1=base, op=ALU.add),
                 reads=[t_ps[pbp], t_base], writes=[t_S])
            P.op("dve", lambda e: e.tensor_tensor(out=base, in0=psum[:, pbp, 16:32], in1=base, op=ALU.add),
                 reads=[t_ps[pbp], t_base], writes=[t_base])

        stA1(0)
        if NTT > 1:
            stA1(1)
        stA2(0)
        for ti in range(NTT):
            if ti + 2 < NTT:
                stA1(ti + 2)
            if ti + 1 < NTT:
                stA2(ti + 1)
            if ti >= 1:
                stC(ti - 1)
            stB(ti)
        stC(NTT - 1)
        sg = ar.alloc([96], F32)
        sgi = ar.alloc([16], I32)
        t_sg = Tok("sg")
        P.op("dve", lambda e: e.tensor_scalar(out=sg[:, 0:16], in0=base, scalar1=511.0, scalar2=None, op0=ALU.add),
             reads=[t_base], writes=[t_sg])
        P.op("dve", lambda e: e.tensor_copy(out=sgi, in_=sg[:, 0:16]), reads=[t_sg], writes=[t_sg])
        P.op("dve", lambda e: e.tensor_scalar(out=sgi, in0=sgi, scalar1=9, scalar2=9, op0=ALU.arith_shift_right,
                                              op1=ALU.logical_shift_left), reads=[t_sg], writes=[t_sg])
        P.op("dve", lambda e: e.tensor_copy(out=sg[:, 0:16], in_=sgi), reads=[t_sg], writes=[t_sg])
        P.op("dve", lambda e: e.memset(sg[:, 48:64], 1.0), writes=[t_sg])
        P.op("dve", lambda e: e.tensor_tensor_scan(out=sg[:, 16:32], data0=sg[:, 48:64], data1=sg[:, 0:16], initial=0.0,
                                                   op0=ALU.mult, op1=ALU.add), reads=[t_sg], writes=[t_sg])
        P.op("dve", lambda e: e.tensor_tensor(out=sg[:, 32:48], in0=sg[:, 16:32], in1=sg[:, 0:16], op=ALU.subtract),
             reads=[t_sg], writes=[t_sg])
        big = ar.alloc([NTT, 16], F32)
        slf = ar.alloc([NTT, 2], F32)
        P.op("dve", lambda e: e.tensor_tensor(out=posa, in0=posa, in1=bc(sg[:, 32:48].unsqueeze(1), [128, NTT, 16]), op=ALU.add),
             reads=[t_S, t_sg], writes=[t_S])
        for k_, Sk in enumerate((S1a, S2a)):
            P.op("dve", lambda e, Sk=Sk: e.tensor_tensor(out=big, in0=posa, in1=Sk, op=ALU.mult), reads=[t_S], writes=[t_sg])
            P.op("dve", lambda e, k_=k_: e.tensor_reduce(out=slf[:, :, k_], in_=big, axis=AX.X, op=ALU.add), reads=[t_sg], writes=[t_sg])
        P.op("dve", lambda e: e.tensor_copy(out=sidx, in_=slf), reads=[t_sg], writes=[t_idx])
        cmp = ar.alloc([NTE, 16], F32)
        etf = ar.alloc([NTE, 4], F32)
        P.op("dve", lambda e: e.tensor_tensor(out=cmp, in0=bc(sg[:, 16:32].unsqueeze(1), [128, NTE, 16]),
                                              in1=bc(cst[:, 128:128 + NTE].unsqueeze(2), [128, NTE, 16]), op=ALU.is_le),
             reads=[t_sg, t_cst], writes=[t_sg])
        P.op("dve", lambda e: e.tensor_reduce(out=etf[:, :, 0], in_=cmp, axis=AX.X, op=ALU.add), reads=[t_sg], writes=[t_sg])
        P.op("dve", lambda e: e.tensor_scalar(out=etf[:, :, 1], in0=etf[:, :, 0], scalar1=15.0, scalar2=256.0, op0=ALU.min, op1=ALU.mult),
             reads=[t_sg], writes=[t_sg])
        P.op("dve", lambda e: e.tensor_scalar(out=etf[:, :, 1], in0=etf[:, :, 1], scalar1=float(l * N_EXP * 256), scalar2=None, op0=ALU.add),
             reads=[t_sg], writes=[t_sg])
        P.op("dve", lambda e: e.scalar_tensor_tensor(out=etf[:, :, 2], in0=bc(cst[:, 168:169], [128, NTE]), scalar=2.0, in1=etf[:, :, 1],
                                                     op0=ALU.mult, op1=ALU.add), reads=[t_sg, t_cst], writes=[t_sg])
        P.op("dve", lambda e: e.tensor_scalar(out=etf[:, :, 3], in0=etf[:, :, 2], scalar1=1.0, scalar2=None, op0=ALU.add),
             reads=[t_sg], writes=[t_sg])
        P.op("dve", lambda e: e.tensor_copy(out=widx, in_=etf[:, :, 2:4]), reads=[t_sg], writes=[t_idx])
        t_xg = Tok("xg")
        for ti in range(NTT):
            for k_ in range(2):
                P.op("pool", lambda e, ti=ti, k_=k_: e.indirect_dma_start(
                    out=xg, out_offset=bass.IndirectOffsetOnAxis(ap=sidx[:, ti, k_:k_ + 1], axis=0),
                    in_=ub[:, ti, :], in_offset=None), reads=[t_ub[ti], t_idx], writes=[Tok()], dma=True)
        dump("sidx", slf, [t_sg])
        dump("etf", etf[:, :, 0], [t_sg])
        dump("wts", wts, [t_S])
        P.barrier()
        ar.off = mark_keep
        NWB = 2
        w1 = [ar.alloc([KD, DE], BF16) for _ in range(NWB)]
        w3 = [ar.alloc([KD, DE], BF16) for _ in range(NWB)]
        w2 = [ar.alloc([4, D], BF16) for _ in range(NWB)]
        t_w1 = [Tok() for _ in range(NWB)]
        t_w3 = [Tok() for _ in range(NWB)]
        t_w2 = [Tok() for _ in range(NWB)]
        NSTG = 8
        stage = [ar.alloc([2048], F32) for _ in range(NSTG)]
        t_stage = [Tok() for _ in range(NSTG)]
        scnt = 0
        NXT = 8
        xtok = [ar.alloc([D], BF16) for _ in range(NXT)]
        t_xtok = [Tok() for _ in range(NXT)]
        xT = [ar.alloc([KD, 512], BF16) for _ in range(2)]
        t_xT = [Tok(), Tok()]
        hT = [ar.alloc([4, 512], BF16) for _ in range(2)]
        t_hT = [Tok(), Tok()]
        sl = [ar.alloc([512], F32) for _ in range(2)]
        t_sl = [Tok(), Tok()]
        ysb = [ar.alloc([D], F32) for _ in range(3)]
        t_ysb = [Tok() for _ in range(3)]
        t_yg = Tok("yg")
        w1r = moe_w1r.rearrange("l r n -> (l r) n")
        w3r = moe_w3r.rearrange("l r n -> (l r) n")
        w2r = moe_w2r.rearrange("l r n -> (l r) n")
        cntb = [0, 0]

        def issue_loads(t):
            ws = t % NWB
            for wi_, wsrc in enumerate((w1r, w3r, w2r)):
                for hf in range(2):
                    si = (6 * t + 2 * wi_ + hf) % NSTG
                    P.op("pool", lambda e, si=si, wsrc=wsrc, t=t, hf=hf: e.indirect_dma_start(
                        out=stage[si], out_offset=None, in_=wsrc,
                        in_offset=bass.IndirectOffsetOnAxis(ap=widx[:, t, hf:hf + 1], axis=0)),
                        reads=[t_idx], writes=[t_stage[si]], dma=True)
            for i4 in range(4):
                xi = (4 * t + i4) % NXT
                r0 = t * 512 + i4 * 128
                P.dma("sp", xtok[xi], xg[r0:r0 + 128, :], reads=[t_xg], writes=[t_xtok[xi]])

        def casts(t):
            ws = t % NWB
            for wi_, (wt, tw, cengs) in enumerate(((w1, t_w1, ("dve", "dve")), (w3, t_w3, ("act", "act")),
                                                   (w2, t_w2, ("act", "dve")))):
                for hf in range(2):
                    si = (6 * t + 2 * wi_ + hf) % NSTG
                    dst = wt[ws].rearrange("p a b -> p (a b)")[:, hf * 2048:(hf + 1) * 2048]
                    if cengs[hf] == "act":
                        P.op("act", lambda e, si=si, dst=dst: e.activation(out=dst, in_=stage[si], func=AF.Copy),
                             reads=[t_stage[si]], writes=[tw[ws]])
                    else:
                        P.op("dve", lambda e, si=si, dst=dst: e.tensor_copy(out=dst, in_=stage[si]),
                             reads=[t_stage[si]], writes=[tw[ws]])

        def trans(t):
            xs_ = t % 2
            for i4 in range(4):
                xi = (4 * t + i4) % NXT
                pbt = i4 % 2
                for k in range(KD):
                    P.op("pe", lambda e, k=k, pbt=pbt, xi=xi: e.transpose(
                        out=bankb(pbt)[:, k * 128:(k + 1) * 128], in_=xtok[xi][:, k * 128:(k + 1) * 128],
                        identity=identb[:]), reads=[t_xtok[xi], t_const], writes=[t_ps[pbt]])
                src_ = bankb(pbt).rearrange("p (k t) -> p k t", t=128)
                dst_ = xT[xs_][:, :, i4 * 128:(i4 + 1) * 128]
                if i4 % 2 == 0:
                    P.op("act", lambda e, src_=src_, dst_=dst_: e.activation(out=dst_, in_=src_, func=AF.Copy),
                         reads=[t_ps[pbt]], writes=[t_xT[xs_]])
                else:
                    P.op("dve", lambda e, src_=src_, dst_=dst_: e.tensor_copy(out=dst_, in_=src_),
                         reads=[t_ps[pbt]], writes=[t_xT[xs_]])

        def mm1(t):
            ws, xs_, hs = t % NWB, t % 2, t % 2
            for hc in range(4):
                pb1 = 2 + (cntb[0] % 2) * 2
                pb3 = pb1 + 1
                s2 = cntb[0] % 2
                cntb[0] += 1
                for (wt, pb, twt) in ((w1, pb1, t_w1), (w3, pb3, t_w3)):
                    for k in range(KD):
                        P.op("pe", lambda e, wt=wt, pb=pb, k=k, hc=hc, ws=ws, xs_=xs_: e.matmul(
                            bank(pb), lhsT=wt[ws][:, k, hc * 128:(hc + 1) * 128], rhs=xT[xs_][:, k, :],
                            start=(k == 0), stop=(k == KD - 1)), reads=[t_xT[xs_], twt[ws]], writes=[t_ps[pb]])
                P.op("act", lambda e, pb1=pb1, s2=s2: e.activation(out=sl[s2], in_=bank(pb1), func=AF.Silu),
                     reads=[t_ps[pb1]], writes=[t_sl[s2]])
                P.op("dve", lambda e, pb3=pb3, s2=s2, hs=hs, hc=hc: e.tensor_tensor(
                    out=hT[hs][:, hc, :], in0=bank(pb3), in1=sl[s2], op=ALU.mult),
                    reads=[t_ps[pb3], t_sl[s2]], writes=[t_hT[hs]])

        def mm2(t):
            ws, hs = t % NWB, t % 2
            for tq in range(4):
                yi = cntb[1] % 3
                cntb[1] += 1
                for nh in range(2):
                    pb = 6 + nh
                    for hc in range(4):
                        P.op("pe", lambda e, pb=pb, hc=hc, tq=tq, nh=nh, hs=hs, ws=ws: e.matmul(
                            bank(pb), lhsT=hT[hs][:, hc, tq * 128:(tq + 1) * 128], rhs=w2[ws][:, hc, nh * 512:(nh + 1) * 512],
                            start=(hc == 0), stop=(hc == 3)), reads=[t_hT[hs], t_w2[ws]], writes=[t_ps[pb]])
                    if nh == 0:
                        P.op("act", lambda e, pb=pb, yi=yi: e.activation(out=ysb[yi][:, 0:512], in_=bank(pb), func=AF.Copy),
                             reads=[t_ps[pb]], writes=[t_ysb[yi]])
                    else:
                        P.op("dve", lambda e, pb=pb, yi=yi: e.tensor_copy(out=ysb[yi][:, 512:1024], in_=bank(pb)),
                             reads=[t_ps[pb]], writes=[t_ysb[yi]])
                r0 = t * 512 + tq * 128
                P.dma("sp", yg[r0:r0 + 128, :], ysb[yi], reads=[t_ysb[yi]], writes=[Tok()])

        issue_loads(0)
        casts(0)
        trans(0)
        for t in range(NTE):
            if t + 1 < NTE:
                issue_loads(t + 1)
            mm1(t)
            if t + 1 < NTE:
                trans(t + 1)
                casts(t + 1)
            mm2(t)
        P.barrier()
        ar.off = mark_keep
        ru = ResUpd()
        NY = 3
        yk = [[ar.alloc([D], F32) for _ in range(2)] for _ in range(NY)]
        t_yk = [[Tok(), Tok()] for _ in range(NY)]
        yc = [ar.alloc([D], F32) for _ in range(2)]
        t_yc = [Tok(), Tok()]

        def gath(ti):
            s3 = ti % NY
            for k_ in range(2):
                P.op("pool", lambda e, ti=ti, k_=k_, s3=s3: e.indirect_dma_start(
                    out=yk[s3][k_], out_offset=None, in_=yg,
                    in_offset=bass.IndirectOffsetOnAxis(ap=sidx[:, ti, k_:k_ + 1], axis=0)),
                    reads=[t_idx, t_yg], writes=[t_yk[s3][k_]], dma=True)

        gath(0)
        gath(1)
        for ti, (b, i) in enumerate(tl):
            s2 = ti % 2
            s3 = ti % NY
            ru.prefetch(b, i)
            P.op("dve", lambda e, ti=ti, s2=s2, s3=s3: e.tensor_scalar(out=yc[s2], in0=yk[s3][0], scalar1=wts[:, ti, 0:1], scalar2=None,
                                                                         op0=ALU.mult), reads=[t_yk[s3][0], t_S], writes=[t_yc[s2]])
            P.op("dve", lambda e, ti=ti, s2=s2, s3=s3: e.scalar_tensor_tensor(out=yc[s2], in0=yk[s3][1], scalar=wts[:, ti, 1:2], in1=yc[s2],
                                                                                op0=ALU.mult, op1=ALU.add),
                 reads=[t_yk[s3][1], t_S, t_yc[s2]], writes=[t_yc[s2]])
            if ti + 2 < NTT:
                gath(ti + 2)
            gs = mset(b, i)
            dst = None
            if last and not dbg:
                r0 = rows(b, i)
                dst = out[r0:r0 + 128, :]
            ru.apply(b, i, [(yc[s2], 0, D)], [t_yc[s2]], G2[gs], t_G2[gs], dst=dst)
        P.barrier()

    phase_ada()
    last_layer = max(layers)
    for l in layers:
        need_ctx = l < NL - 1
        kind = l % 3
        for b in range(2):
            if do_mixer:
                if kind == 0:
                    attn_layer(l, b, need_ctx)
                elif kind == 1:
                    lru_layer(l, b, need_ctx)
                else:
                    pool_layer(l, b, need_ctx)
        if do_moe:
            moe_sparse(l, not need_ctx)
    if dbg:
        for b in range(2):
            for i in range(NTILE):
                r0 = rows(b, i)
                dst = out[r0:r0 + 128, :] if i < 16 else outc[r0 - 4096:r0 - 4096 + 128, :]
                P.dma("sp", dst, res[r0:r0 + 128, :], reads=[t_res[b][i]], writes=[t_res[b][i]])
    fin_reads = [t_res[b][i] for b in range(2) for i in range(NTILE)]
    P.op("sp", lambda e: e.nop(), reads=fin_reads)
    P.barrier()
    P.emit()
    return nc, P


def _fm(v, inner=None):
    v = np.asarray(v, np.float32)
    lead = v.shape[:-1]
    K = v.shape[-1] // 128
    v = v.reshape(*lead, K, 128)
    return np.ascontiguousarray(np.moveaxis(v, -1, 0))


def _wr(w, K):
    L, E, KP, N = w.shape
    w = w.reshape(L, E, K, 128, N).transpose(0, 1, 3, 2, 4)
    return np.ascontiguousarray(w).reshape(L, E * 128 * 2, K * N // 2)


def _rope_tables():
    t = np.arange(S)
    row = (t // 64).astype(np.float32)
    col = (t % 64).astype(np.float32)
    inv = (10000.0 ** (-np.arange(16, dtype=np.float32) / 16)).astype(np.float32)
    ang = np.concatenate([row[:, None] * inv, col[:, None] * inv], axis=-1)
    cos = np.ones((TB, 32), np.float32)
    sin = np.zeros((TB, 32), np.float32)
    cos[:S] = np.cos(ang)
    sin[:S] = np.sin(ang)
    cos = np.ascontiguousarray(cos.reshape(NTILE, 128, 32).transpose(1, 0, 2))
    sin = np.ascontiguousarray(sin.reshape(NTILE, 128, 32).transpose(1, 0, 2))
    return cos, sin


def _pool_edges():
    e = np.zeros((128, 4, 2, 8), np.float32)
    for g, w in enumerate((2, 4, 8, 16)):
        for n in (S,):
            pass
        hw = w // 2
        for jx in range(8):
            t = jx
            cnt = min(t + hw, 10 ** 9) - max(t - hw, 0)
            e[:, g, 0, jx] = 1.0 / cnt
            d = 8 - jx
            cnt = min(hw, d) + hw
            e[:, g, 1, jx] = 1.0 / cnt
    return e


def prep_inputs(inputs, core, resin_override=None):
    f = lambda k: np.ascontiguousarray(np.asarray(inputs[k], np.float32))
    b0, b1 = 2 * core, 2 * core + 1
    x, ctx, c = f("x"), f("ctx"), f("c")
    if resin_override is None:
        resin = np.concatenate([x[b0], x[b1], ctx[b0], ctx[b1]], axis=0)
    else:
        resin = resin_override
    cT = np.ascontiguousarray(np.stack([c[b0], c[b1], f("c_ctx")], axis=1))
    cos, sin = _rope_tables()
    m = {
        "resin": np.ascontiguousarray(resin),
        "cT": cT,
        "w_ada": f("w_ada"),
        "badaT": np.ascontiguousarray(_fm(f("b_ada"))),
        "normgT": np.ascontiguousarray(_fm(f("norm_g"))),
        "ident": np.eye(128, dtype=np.float32),
        "attn_w_in": f("attn_w_in"),
        "attn_q_gain": f("attn_q_gain"),
        "attn_k_gain": f("attn_k_gain"),
        "attn_lam": f("attn_lam").reshape(2, 256),
        "attn_sgT": np.ascontiguousarray(f("attn_sub_gain").T),
        "attn_w_out": f("attn_w_out"),
        "rope_cos": cos,
        "rope_sin": sin,
        "lru_w_in": f("lru_w_in"),
        "lru_cwT": np.ascontiguousarray(_fm(f("lru_conv_w")[0]).transpose(0, 2, 1)),
        "lru_cbT": np.ascontiguousarray(_fm(f("lru_conv_b")[0])),
        "lru_w_a": f("lru_w_a"),
        "lru_w_x": f("lru_w_x"),
        "lru_baT": np.ascontiguousarray(_fm(f("lru_b_a")[0])),
        "lru_bxT": np.ascontiguousarray(_fm(f("lru_b_x")[0])),
        "lru_lamT": np.ascontiguousarray(_fm(f("lru_lam")[0])),
        "lru_w_out": f("lru_w_out"),
        "pool_w_in": f("pool_w_in"),
        "pool_w_grp": f("pool_w_grp"),
        "pool_scale": f("pool_scale"),
        "pool_edge": _pool_edges(),
        "moe_router": np.ascontiguousarray(np.concatenate([f("moe_router_g"), f("moe_router_e")], axis=-1)),
        "moe_w1r": _wr(f("moe_w1"), 8),
        "moe_w3r": _wr(f("moe_w3"), 8),
        "moe_w2r": _wr(f("moe_w2"), 4),
        "normg_raw": f("norm_g"),
        "utri": np.triu(np.ones((128, 128), np.float32), 1),
        "tvals": np.broadcast_to((np.arange(40, dtype=np.float32) * 512.0)[None, :], (128, 40)).copy(),
        "pidx": np.arange(128, dtype=np.float32).reshape(128, 1),
    }
    return m


_CACHE = {}


def kernel(**inputs):
    if "nc" not in _CACHE:
        _CACHE["nc"] = build()[0]
    nc = _CACHE["nc"]
    shared = prep_inputs(inputs, 0)
    x, ctx, c = (np.asarray(inputs[k], np.float32) for k in ("x", "ctx", "c"))
    cc = np.asarray(inputs["c_ctx"], np.float32)
    in_maps = []
    for core in range(8):
        m = dict(shared)
        b0, b1 = 2 * core, 2 * core + 1
        m["resin"] = np.ascontiguousarray(np.concatenate([x[b0], x[b1], ctx[b0], ctx[b1]], axis=0))
        m["cT"] = np.ascontiguousarray(np.stack([c[b0], c[b1], cc], axis=1))
        in_maps.append(m)
    res = run_bass_kernel_spmd(nc, in_maps, core_ids=list(range(8)))
    outs = [r["out"].reshape(2, S, D) for r in res.results]
    return np.concatenate(outs, axis=0).astype(np.float32)
```

```python
import contextlib
import math
import numpy as np
import concourse.bass as bass
import concourse.mybir as mybir
from concourse.ap import AP
from concourse.bass_utils import run_bass_kernel_spmd

F32 = mybir.dt.float32
BF16 = mybir.dt.bfloat16
AF = mybir.ActivationFunctionType
ALU = mybir.AluOpType
AX = mybir.AxisListType

N_DMA_SEMS = 12
SAME_ENGINE_SYNC = True


class Tok:
    __slots__ = ("name", "last_w", "readers")

    def __init__(self, name=""):
        self.name = name
        self.last_w = None
        self.readers = []


class Op:
    __slots__ = ("eng", "fn", "deps", "signal", "val", "is_dma", "slot")

    def __init__(self, eng, fn, is_dma):
        self.eng = eng
        self.fn = fn
        self.deps = []
        self.signal = False
        self.val = None
        self.is_dma = is_dma
        self.slot = None


class Prog:
    ENGS = ("pe", "act", "dve", "pool", "sp")

    def __init__(self, nc):
        self.nc = nc
        self.ops = []
        self.stack = contextlib.ExitStack()
        self.n_alloc = 0
        self.bar_idx = 0

    def sbuf(self, shape, dtype, name=None):
        self.n_alloc += 1
        return self.stack.enter_context(self.nc.sbuf_tensor(name or f"sb{self.n_alloc}", list(shape), dtype))

    def psum(self, shape, dtype, name=None):
        self.n_alloc += 1
        return self.stack.enter_context(self.nc.psum_tensor(name or f"ps{self.n_alloc}", list(shape), dtype))

    def op(self, eng, fn, reads=(), writes=(), dma=False):
        o = Op(eng, fn, dma)
        deps = []
        for t in reads:
            if t.last_w is not None:
                deps.append(t.last_w)
        for t in writes:
            if t.last_w is not None:
                deps.append(t.last_w)
            deps.extend(t.readers)
        seen = set()
        for d in deps:
            if id(d) in seen or d is o:
                continue
            seen.add(id(d))
            if (not d.is_dma) and (not dma) and d.eng == eng:
                if eng == "pe" or not SAME_ENGINE_SYNC:
                    continue
            d.signal = True
            o.deps.append(d)
        for t in writes:
            t.last_w = o
            t.readers = []
        for t in reads:
            if t in writes:
                continue
            if not dma:
                t.readers = [r for r in t.readers if r.is_dma or r.eng != eng]
            t.readers.append(o)
        self.ops.append(o)
        return o

    def dma(self, eng, out, in_, reads=(), writes=(), **kw):
        return self.op(eng, lambda e: e.dma_start(out=out, in_=in_, **kw), reads, writes, dma=True)

    def barrier(self):
        last = {}
        dmas = []
        for o in self.ops[self.bar_idx:]:
            if o.is_dma:
                dmas.append(o)
            else:
                last[o.eng] = o
        deps = list(last.values()) + dmas
        first = len(self.ops)
        for e in self.ENGS:
            o = Op(e, lambda eng: eng.nop(), False)
            o.deps = [d for d in deps if d.is_dma or d.eng != e]
            for d in o.deps:
                d.signal = True
            self.ops.append(o)
        self.bar_idx = first

    def emit(self):
        nc = self.nc
        cnt = {e: 0 for e in self.ENGS}
        dcnt = {e: 0 for e in self.ENGS}
        slotcnt = {e: [0] * N_DMA_SEMS for e in self.ENGS}
        for o in self.ops:
            if o.is_dma:
                k = dcnt[o.eng] % N_DMA_SEMS
                dcnt[o.eng] += 1
                o.slot = k
                slotcnt[o.eng][k] += 16
                o.val = slotcnt[o.eng][k]
            elif o.signal:
                cnt[o.eng] += 1
                o.val = cnt[o.eng]
        sems = {}
        for e in self.ENGS:
            if cnt[e] > 0:
                sems[e] = self.stack.enter_context(nc.semaphore(f"s_{e}"))
        dsems = {}
        for e in self.ENGS:
            if dcnt[e] > 0:
                dsems[e] = [self.stack.enter_context(nc.semaphore(f"d_{e}{k}"))
                            for k in range(min(N_DMA_SEMS, dcnt[e]))]
        by_eng = {e: [o for o in self.ops if o.eng == e] for e in self.ENGS}
        self.stats = {e: len(by_eng[e]) for e in self.ENGS}
        self.stats["maxsem"] = dict(cnt)
        nwaits = [0]

        def run(e_name, eng):
            known = {}
            for o in by_eng[e_name]:
                waits = {}
                for d in o.deps:
                    key = ("d", d.eng, d.slot) if d.is_dma else ("c", d.eng)
                    if known.get(key, 0) >= d.val:
                        continue
                    waits[key] = max(waits.get(key, 0), d.val)
                if o.is_dma and o.val > 16:
                    key = ("d", o.eng, o.slot)
                    if known.get(key, 0) < o.val - 16:
                        waits[key] = max(waits.get(key, 0), o.val - 16)
                for key, v in waits.items():
                    s = dsems[key[1]][key[2]] if key[0] == "d" else sems[key[1]]
                    eng.wait_ge(s, v)
                    known[key] = v
                    nwaits[0] += 1
                ins = o.fn(eng)
                if o.is_dma:
                    ins.then_inc(dsems[o.eng][o.slot], 16)
                elif o.signal:
                    ins.then_inc(sems[o.eng], 1)

        with nc.Block() as block:
            if by_eng["sp"]:
                @block.sync
                def _(eng):
                    run("sp", eng)
            if by_eng["pe"]:
                @block.tensor
                def _(eng):
                    run("pe", eng)
            if by_eng["act"]:
                @block.scalar
                def _(eng):
                    run("act", eng)
            if by_eng["dve"]:
                @block.vector
                def _(eng):
                    run("dve", eng)
            if by_eng["pool"]:
                @block.gpsimd
                def _(eng):
                    run("pool", eng)
        self.stats["waits"] = nwaits[0]
        self.stack.close()


class Arena:
    def __init__(self, P, nbytes):
        self.t = P.sbuf([128, nbytes // 4], F32, "arena")
        self.cap = nbytes
        self.off = 0

    def alloc(self, free_shape, dtype):
        n = 1
        for s in free_shape:
            n *= s
        nb = n * (2 if dtype == BF16 else 4)
        nb = (nb + 31) // 32 * 32
        assert self.off + nb <= self.cap, f"arena overflow {self.off}+{nb}>{self.cap}"
        a = self.t[:, self.off // 4:(self.off + nb) // 4]
        self.off += nb
        if dtype != F32:
            a = a.bitcast(dtype)
        a = a[:, 0:n]
        if len(free_shape) == 2:
            a = a.rearrange("p (a b) -> p a b", b=free_shape[1])
        elif len(free_shape) == 3:
            a = a.rearrange("p (a b c) -> p a b c", b=free_shape[1], c=free_shape[2])
        return a


def bc(ap, shape):
    return ap.broadcast_to(list(shape))


def rev2d(ap2d):
    a = ap2d.ap
    n = a[1][1]
    st = a[1][0]
    return AP(ap2d.tensor, ap2d.offset + (n - 1) * st, [list(a[0]), [-st, n]])


D = 1024
KD = 8
S = 2048
CT = 256
NTILE = 18
TB = NTILE * 128
EPS = 1e-6
NL = 4
N_EXP = 16
DE = 512
LRU_W = 1280
LW = 2310
LAT0 = 2
CTX0 = 2053


def build(layers=(0, 1, 2, 3), dbg=False, do_mixer=True, do_moe=True):
    nc = bass.Bass("TRN2", target_bir_lowering=False)
    P = Prog(nc)

    def din(name, shape):
        return nc.dram_tensor(name, list(shape), F32, kind="ExternalInput").ap()

    resin = din("resin", [4608, D])
    cT_d = din("cT", [D, 3])
    w_ada = din("w_ada", [NL, D, 6 * D])
    badaT_d = din("badaT", [128, NL, 48])
    normgT_d = din("normgT", [128, NL, 2, 8])
    ident_d = din("ident", [128, 128])
    attn_w_in = din("attn_w_in", [2, D, 3 * D])
    attn_qg = din("attn_q_gain", [2, 64])
    attn_kg = din("attn_k_gain", [2, 64])
    attn_lam = din("attn_lam", [2, 256])
    attn_sgT = din("attn_sgT", [128, 2])
    attn_w_out = din("attn_w_out", [2, D, D])
    rope_cos = din("rope_cos", [128, NTILE, 32])
    rope_sin = din("rope_sin", [128, NTILE, 32])
    lru_w_in = din("lru_w_in", [1, D, 2 * LRU_W])
    lru_cwT = din("lru_cwT", [128, 10, 4])
    lru_cbT = din("lru_cbT", [128, 10])
    lru_w_a = din("lru_w_a", [1, 2, 10, 128, 128])
    lru_w_x = din("lru_w_x", [1, 2, 10, 128, 128])
    lru_baT = din("lru_baT", [128, 2, 10])
    lru_bxT = din("lru_bxT", [128, 2, 10])
    lru_lamT = din("lru_lamT", [128, 2, 10])
    lru_w_out = din("lru_w_out", [1, LRU_W, D])
    pool_w_in = din("pool_w_in", [1, D, D])
    pool_w_grp = din("pool_w_grp", [1, 4, 256, 256])
    pool_scale = din("pool_scale", [1, D])
    pool_edge = din("pool_edge", [128, 4, 2, 8])
    moe_router = din("moe_router", [NL, D, 20])
    moe_w1r = din("moe_w1r", [NL, N_EXP * 128 * 2, 2048])
    moe_w3r = din("moe_w3r", [NL, N_EXP * 128 * 2, 2048])
    moe_w2r = din("moe_w2r", [NL, N_EXP * 128 * 2, 2048])
    normg_raw = din("normg_raw", [NL, 2, D])
    utri_d = din("utri", [128, 128])
    tvals_d = din("tvals", [128, 40])
    pidx_d = din("pidx", [128, 1])
    NSLOT = 34 * 512
    xg = nc.dram_tensor("xg", [NSLOT, D], BF16, kind="Internal").ap()
    yg = nc.dram_tensor("yg", [NSLOT, D], F32, kind="Internal").ap()
    out = nc.dram_tensor("out", [4096, D], F32, kind="ExternalOutput").ap()
    if dbg:
        outc = nc.dram_tensor("outc", [512, D], F32, kind="ExternalOutput").ap()
    res = nc.dram_tensor("res", [4608, D], F32, kind="Internal").ap()
    mods_d = nc.dram_tensor("mods_d", [NL, 144, 128], F32, kind="Internal").ap()

    ident32 = P.sbuf([128, 128], F32, "sb_ident32")
    identb = P.sbuf([128, 128], BF16, "sb_identb")
    modsT = P.sbuf([128, NL, 3, 48], F32, "sb_modsT")
    normgT = P.sbuf([128, NL, 2, 8], F32, "sb_normgT")
    Amod = P.sbuf([128, 2, 3, 8], F32, "sb_Amod")
    cm05 = P.sbuf([128, 16], F32, "sb_cm05")
    t_const = Tok("const")
    t_mods = Tok("modsT")
    t_amod = Tok("amod")
    t_modsd = [Tok(f"modsd{l}") for l in range(NL)]
    psum = P.psum([128, 8, 512], F32, "ps_all")
    t_ps = [Tok(f"psb{i}") for i in range(8)]
    ar = Arena(P, 196 * 1024)
    t_res = [[Tok(f"res{b}_{i}") for i in range(NTILE)] for b in range(2)]

    dumps = {}

    def dump(name, ap, reads):
        if not dbg or name in dumps:
            return
        shp = list(ap.shape)
        d = nc.dram_tensor("dump_" + name, shp, F32, kind="ExternalOutput").ap()
        dumps[name] = d
        P.dma("pool", d, ap, reads=reads, allow_slow_non_contiguous=True)

    def bank(i):
        return psum[:, i, :]

    def bankb(i):
        return psum[:, i, :].bitcast(BF16)

    def rows(b, i):
        if i < 16:
            return b * S + i * 128
        return 4096 + b * CT + (i - 16) * 128

    def mset(b, i):
        return b if i < 16 else 2

    def vec_op(eng):
        return "dve" if eng == 0 else "pool"

    P.dma("sp", ident32[:], ident_d, writes=[t_const])
    P.op("dve", lambda e: e.tensor_copy(out=identb[:], in_=ident32[:]), reads=[t_const], writes=[t_const])
    P.op("dve", lambda e: e.memset(cm05[:], -0.5), writes=[t_const])
    P.dma("sp", normgT[:], normgT_d, writes=[t_const])
    for b in range(2):
        for i in range(NTILE):
            r0 = rows(b, i)
            P.dma("sp", res[r0:r0 + 128, :], resin[r0:r0 + 128, :], writes=[t_res[b][i]])

    def phase_ada():
        ar.off = 0
        scT = ar.alloc([8, 3], F32)
        t_sc = Tok("scT")
        P.dma("sp", scT, cT_d.rearrange("(k p) s -> p k s", p=128), writes=[t_sc])
        P.op("act", lambda e: e.activation(out=scT, in_=scT, func=AF.Silu), reads=[t_sc], writes=[t_sc])
        badaT = ar.alloc([NL, 48], F32)
        t_b = Tok("bada")
        P.dma("sp", badaT, badaT_d, writes=[t_b])
        NW = 3
        Wt = [ar.alloc([8, 512], F32) for _ in range(NW)]
        t_W = [Tok(f"W{i}") for i in range(NW)]
        tmpT = ar.alloc([128], F32)
        t_tmp = Tok("tmpT")
        wi = 0
        for l in range(NL):
            if l not in layers:
                continue
            pb = l % 2
            wsrc = w_ada[l].rearrange("(k p) n -> p k n", p=128)
            for nch in range(12):
                s = wi % NW
                wi += 1
                P.dma("sp", Wt[s], wsrc[:, :, nch * 512:(nch + 1) * 512], writes=[t_W[s]])
                for cc in range(4):
                    c = nch * 4 + cc
                    for k in range(KD):
                        P.op("pe", lambda e, s=s, k=k, cc=cc, c=c, pb=pb: e.matmul(
                            psum[:, pb, c:c + 97:48], lhsT=Wt[s][:, k, cc * 128:(cc + 1) * 128],
                            rhs=scT[:, k, :], start=(k == 0), stop=(k == KD - 1)),
                            reads=[t_W[s], t_sc], writes=[t_ps[pb]])
            P.op("dve", lambda e, l=l, pb=pb: e.tensor_tensor(
                out=modsT[:, l], in0=psum[:, pb, 0:144].rearrange("p (s c) -> p s c", c=48),
                in1=bc(badaT[:, l:l + 1, :], [128, 3, 48]), op=ALU.add),
                reads=[t_ps[pb], t_b], writes=[t_mods])
            src = modsT[:, l].rearrange("p s c -> p (s c)")
            for (r0, r1) in ((0, 128), (128, 144)):
                n = r1 - r0
                P.op("pe", lambda e, r0=r0, r1=r1, n=n, src=src: e.transpose(
                    out=psum[0:n, 2, 0:128], in_=src[:, r0:r1], identity=ident32[:]),
                    reads=[t_mods, t_const], writes=[t_ps[2]])
                P.op("act", lambda e, n=n: e.activation(out=tmpT[0:n, :], in_=psum[0:n, 2, 0:128], func=AF.Copy),
                     reads=[t_ps[2]], writes=[t_tmp])
                P.dma("sp", mods_d[l, r0:r1, :], tmpT[0:n, :], reads=[t_tmp], writes=[t_modsd[l]])
            dump("modsT", modsT[:, l], [t_mods])
        P.barrier()

    def layer_setup(l):
        for n in range(2):
            for s in range(3):
                j = 1 + 3 * n
                P.op("dve", lambda e, n=n, s=s, j=j: e.scalar_tensor_tensor(
                    out=Amod[:, n, s, :], in0=modsT[:, l, s, j * 8:(j + 1) * 8], scalar=1.0,
                    in1=normgT[:, l, n, :], op0=ALU.add, op1=ALU.mult),
                    reads=[t_mods, t_const], writes=[t_amod])

    def Bmod(l, n, s):
        j = 3 * n
        return modsT[:, l, s, j * 8:(j + 1) * 8]

    def load_gate(l, which, s, dst, tok):
        r0 = s * 48 + which * 8
        src = mods_d[l, r0:r0 + 8, :]
        a = AP(src.tensor, src.offset, [[0, 128], [1, 1024]])
        P.dma("sp", dst, a, reads=[t_modsd[l]], writes=[tok])

    class NormBufs:
        def __init__(self, want32):
            self.NX = 4
            self.NH = 3
            self.xt = [ar.alloc([D], F32) for _ in range(self.NX)]
            self.t_xt = [Tok() for _ in range(self.NX)]
            self.junk = ar.alloc([D], BF16)
            self.t_junk = Tok()
            self.st = [ar.alloc([4], F32) for _ in range(self.NX)]
            self.t_st = [Tok() for _ in range(self.NX)]
            self.xh = [ar.alloc([D], F32) for _ in range(self.NH)]
            self.t_xh = [Tok() for _ in range(self.NH)]
            self.cnt = 0
            if want32:
                self.u32 = [ar.alloc([KD, 128], F32) for _ in range(2)]
                self.t_u32 = [Tok() for _ in range(2)]

    def norm_all(nb, l, n, b, tiles, uT, t_uT):
        c0 = nb.cnt
        nb.cnt += len(tiles)
        nt_ = len(tiles)
        for j_ in range(min(2, nt_)):
            norm_tile(nb, l, n, b, tiles[j_], uT, t_uT, tiles[j_] * 128, stage=1, c=c0 + j_)
        for j_ in range(nt_):
            norm_tile(nb, l, n, b, tiles[j_], uT, t_uT, tiles[j_] * 128, stage=2, c=c0 + j_)
            if j_ + 2 < nt_:
                norm_tile(nb, l, n, b, tiles[j_ + 2], uT, t_uT, tiles[j_ + 2] * 128, stage=1, c=c0 + j_ + 2)

    def norm_tile(nb, l, n, b, i, uT, t_uT, col, want32=False, stage=0, c=None):
        if c is None:
            c = nb.cnt
            nb.cnt += 1
        sx = c % nb.NX
        s2 = c % nb.NH
        xt, st, xh = nb.xt[sx], nb.st[sx], nb.xh[s2]
        r0 = rows(b, i)
        s = mset(b, i)
        pb0 = 2 * (c % 2)
        if stage in (0, 1):
            P.dma("sp", xt, res[r0:r0 + 128, :], reads=[t_res[b][i]], writes=[nb.t_xt[sx]])
            P.op("act", lambda e: e.activation(out=nb.junk, in_=xt, func=AF.Square, accum_out=st[:, 0:1]),
                 reads=[nb.t_xt[sx]], writes=[nb.t_junk, nb.t_st[sx]])
            P.op("dve", lambda e: e.tensor_scalar(out=st[:, 1:2], in0=st[:, 0:1], scalar1=1.0 / D, scalar2=EPS,
                                                  op0=ALU.mult, op1=ALU.add),
                 reads=[nb.t_st[sx]], writes=[nb.t_st[sx]])
            P.op("pool", lambda e: e.tensor_tensor(out=st[:, 2:3], in0=st[:, 1:2], in1=cm05[:, 0:1], op=ALU.pow),
                 reads=[nb.t_st[sx], t_const], writes=[nb.t_st[sx]])
            P.op("act", lambda e: e.activation(out=xh, in_=xt, func=AF.Copy, scale=st[:, 2:3]),
                 reads=[nb.t_xt[sx], nb.t_st[sx]], writes=[nb.t_xh[s2]])
            for k in range(KD):
                pb = pb0 + k // 4
                P.op("pe", lambda e, k=k, pb=pb: e.transpose(
                    out=psum[:, pb, (k % 4) * 128:(k % 4 + 1) * 128], in_=xh[:, k * 128:(k + 1) * 128],
                    identity=ident32[:]), reads=[nb.t_xh[s2], t_const], writes=[t_ps[pb]])
            if stage == 1:
                return None, None
        A = Amod[:, n, s, :]
        B = Bmod(l, n, s)
        if want32:
            u32 = nb.u32[s2]
            for k in range(KD):
                pb = pb0 + k // 4
                P.op("dve", lambda e, k=k, pb=pb: e.tensor_scalar(
                    out=u32[:, k, :], in0=psum[:, pb, (k % 4) * 128:(k % 4 + 1) * 128],
                    scalar1=A[:, k:k + 1], scalar2=B[:, k:k + 1], op0=ALU.mult, op1=ALU.add),
                    reads=[t_ps[pb], t_amod, t_mods], writes=[nb.t_u32[s2]])
            P.op("act", lambda e: e.activation(out=uT[:, :, col:col + 128], in_=u32, func=AF.Copy),
                 reads=[nb.t_u32[s2]], writes=[t_uT])
            return u32, nb.t_u32[s2]
        for k in range(KD):
            pb = pb0 + k // 4
            src = psum[:, pb, (k % 4) * 128:(k % 4 + 1) * 128]
            dst = uT[:, k, col:col + 128]
            if k % 2 == 0:
                P.op("act", lambda e, k=k, src=src, dst=dst: e.activation(
                    out=dst, in_=src, func=AF.Identity, scale=A[:, k:k + 1], bias=B[:, k:k + 1]),
                    reads=[t_ps[pb], t_amod, t_mods], writes=[t_uT])
            else:
                P.op("dve", lambda e, k=k, src=src, dst=dst: e.tensor_scalar(
                    out=dst, in0=src, scalar1=A[:, k:k + 1], scalar2=B[:, k:k + 1], op0=ALU.mult, op1=ALU.add),
                    reads=[t_ps[pb], t_amod, t_mods], writes=[t_uT])
        return None, None

    class ResUpd:
        def __init__(self):
            self.N = 3
            self.xt = [ar.alloc([D], F32) for _ in range(self.N)]
            self.t_xt = [Tok() for _ in range(self.N)]
            self.tmp = [ar.alloc([D], F32) for _ in range(2)]
            self.t_tmp = [Tok() for _ in range(2)]
            self.cnt = 0

        def prefetch(self, b, i):
            sx = self.cnt % self.N
            r0 = rows(b, i)
            P.dma("sp", self.xt[sx], res[r0:r0 + 128, :], reads=[t_res[b][i]], writes=[self.t_xt[sx]])

        def apply(self, b, i, ysrc, yreads, G, t_G, dst=None):
            sx = self.cnt % self.N
            s2 = self.cnt % 2
            self.cnt += 1
            xt, tmp = self.xt[sx], self.tmp[s2]
            for (ap_in, c0, n) in ysrc:
                P.op("dve", lambda e, ap_in=ap_in, c0=c0, n=n: e.tensor_tensor(
                    out=tmp[:, c0:c0 + n], in0=ap_in, in1=G[:, c0:c0 + n], op=ALU.mult),
                    reads=list(yreads) + [t_G], writes=[self.t_tmp[s2]])
            P.op("dve", lambda e: e.tensor_tensor(out=xt, in0=xt, in1=tmp, op=ALU.add),
                 reads=[self.t_tmp[s2], self.t_xt[sx]], writes=[self.t_xt[sx]])
            r0 = rows(b, i)
            if dst is None:
                P.dma("sp", res[r0:r0 + 128, :], xt, reads=[self.t_xt[sx]], writes=[t_res[b][i]])
            else:
                P.dma("sp", dst, xt, reads=[self.t_xt[sx]], writes=[t_res[b][i]])

    def attn_layer(l, b, need_ctx):
        j = l // 3
        lam_init = 0.8 - 0.6 * math.exp(-0.3 * l)
        ar.off = 0
        qT = ar.alloc([KD, TB], BF16)
        kT = ar.alloc([KD, TB], BF16)
        vx = ar.alloc([NTILE, 8, 130], BF16)
        t_qT, t_kT, t_vx = Tok("qT"), Tok("kT"), Tok("vx")
        small = ar.alloc([16], F32)
        t_small = Tok("small")
        mark = ar.off
        uT = ar.alloc([KD, TB], BF16)
        t_uT = Tok("uT")
        mark1 = ar.off
        nb = NormBufs(False)
        layer_setup(l)
        norm_all(nb, l, 0, b, list(range(NTILE)), uT, t_uT)
        dump("uT", uT, [t_uT])
        P.barrier()
        ar.off = mark1
        P.op("pool", lambda e: e.memset(vx[:, :, :, 128:130], 1.0), writes=[t_vx])
        cs_t = ar.alloc([2, NTILE, 32], F32)
        t_cs = Tok("cs")
        P.dma("sp", cs_t[:, 0], rope_cos, writes=[t_cs])
        P.dma("sp", cs_t[:, 1], rope_sin, writes=[t_cs])
        gt = ar.alloc([2, 512], F32)
        t_gt = Tok("gt")
        for qk, src in enumerate((attn_qg, attn_kg)):
            a = src[j:j + 1, :]
            P.dma("sp", gt[:, qk, :].rearrange("p (a b) -> p a b", b=64),
                  AP(a.tensor, a.offset, [[0, 128], [0, 8], [1, 64]]), writes=[t_gt])
        lq = ar.alloc([256], F32)
        t_lq = Tok("lq")
        a = attn_lam[j:j + 1, :]
        P.dma("sp", lq, AP(a.tensor, a.offset, [[0, 128], [1, 256]]), writes=[t_lq])
        lqj = ar.alloc([128], F32)
        P.op("dve", lambda e: e.tensor_tensor(out=lqj[:, 0:64], in0=lq[:, 0:64], in1=lq[:, 64:128], op=ALU.mult),
             reads=[t_lq], writes=[t_lq])
        P.op("dve", lambda e: e.tensor_tensor(out=lqj[:, 64:128], in0=lq[:, 128:192], in1=lq[:, 192:256], op=ALU.mult),
             reads=[t_lq], writes=[t_lq])
        P.op("dve", lambda e: e.tensor_reduce(out=small[:, 0:2], in_=lqj.rearrange("p (a b) -> p a b", b=64),
                                              axis=AX.X, op=ALU.add), reads=[t_lq], writes=[t_small])
        P.op("act", lambda e: e.activation(out=small[:, 2:4], in_=small[:, 0:2], func=AF.Exp),
             reads=[t_small], writes=[t_small])
        P.op("dve", lambda e: e.tensor_tensor(out=small[:, 4:5], in0=small[:, 3:4], in1=small[:, 2:3], op=ALU.subtract),
             reads=[t_small], writes=[t_small])
        P.op("dve", lambda e: e.tensor_scalar(out=small[:, 5:6], in0=small[:, 4:5], scalar1=-lam_init, scalar2=None,
                                              op0=ALU.add), reads=[t_small], writes=[t_small])
        neglam = small[:, 5:6]
        P.dma("sp", small[:, 8:10], attn_sgT, writes=[t_small])
        P.op("dve", lambda e: e.tensor_scalar(out=small[:, 6:7], in0=small[:, 8 + j:9 + j], scalar1=1.0 - lam_init,
                                              scalar2=None, op0=ALU.mult), reads=[t_small], writes=[t_small])
        sgs = small[:, 6:7]

        NWB = 2
        wch = [ar.alloc([KD, 512], BF16) for _ in range(NWB)]
        t_wch = [Tok() for _ in range(NWB)]
        RQ = 3
        qs = [ar.alloc([512], F32) for _ in range(RQ)]
        ta = [ar.alloc([512], F32) for _ in range(RQ)]
        qr = [ar.alloc([512], BF16) for _ in range(RQ)]
        stt = [ar.alloc([32], F32) for _ in range(RQ)]
        t_q = [Tok() for _ in range(RQ)]
        wsrc = attn_w_in[j].rearrange("(k p) n -> p k n", p=128)
        steps = [(nch, i) for nch in range(6) for i in range(NTILE)]

        def stage1(s):
            nch, i = steps[s]
            ws = nch % NWB
            if i == 0:
                P.dma("pool", wch[ws], wsrc[:, :, nch * 512:(nch + 1) * 512], writes=[t_wch[ws]])
            pb = 4 + (s % 2)
            r = s % RQ
            for k in range(KD):
                P.op("pe", lambda e, k=k: e.matmul(
                    bank(pb), lhsT=uT[:, k, i * 128:(i + 1) * 128], rhs=wch[ws][:, k, :],
                    start=(k == 0), stop=(k == KD - 1)), reads=[t_uT, t_wch[ws]], writes=[t_ps[pb]])
            if nch >= 4:
                h0 = (nch - 4) * 4
                P.op("act", lambda e: e.activation(
                    out=vx[:, i, h0:h0 + 4, 0:128], in_=bank(pb).rearrange("p (h d) -> p h d", d=128),
                    func=AF.Copy), reads=[t_ps[pb]], writes=[t_vx])
                return
            P.op("act", lambda e: e.activation(out=qs[r], in_=bank(pb), func=AF.Copy), reads=[t_ps[pb]], writes=[t_q[r]])
            P.op("act", lambda e: e.activation(out=ta[r], in_=bank(pb), func=AF.Square), reads=[t_ps[pb]], writes=[t_q[r]])

        def stage2a(s):
            nch, i = steps[s]
            if nch >= 4:
                return
            r = s % RQ
            tq = t_q[r]
            P.op("dve", lambda e: e.tensor_reduce(
                out=stt[r][:, 0:8], in_=ta[r].rearrange("p (a b) -> p a b", b=64), axis=AX.X, op=ALU.add),
                reads=[tq], writes=[tq])
            P.op("dve", lambda e: e.tensor_scalar(
                out=stt[r][:, 8:16], in0=stt[r][:, 0:8], scalar1=1.0 / 64, scalar2=EPS, op0=ALU.mult, op1=ALU.add),
                reads=[tq], writes=[tq])
            P.op("pool", lambda e: e.tensor_tensor(
                out=stt[r][:, 16:24], in0=stt[r][:, 8:16], in1=cm05[:, 0:8], op=ALU.pow),
                reads=[tq, t_const], writes=[tq])

        def stage2b(s):
            nch, i = steps[s]
            if nch >= 4:
                return
            r = s % RQ
            tq = t_q[r]
            qk = nch // 2
            h0 = (nch % 2) * 4
            q3 = qs[r].rearrange("p (a b) -> p a b", b=64)
            P.op("dve", lambda e: e.tensor_tensor(
                out=q3, in0=q3, in1=bc(stt[r][:, 16:24].unsqueeze(2), [128, 8, 64]), op=ALU.mult),
                reads=[tq], writes=[tq])
            P.op("dve", lambda e: e.tensor_tensor(out=qs[r], in0=qs[r], in1=gt[:, qk, :], op=ALU.mult),
                 reads=[tq, t_gt], writes=[tq])
            x4 = qs[r].rearrange("p (a m d) -> p a m d", m=2, d=32)
            t4 = ta[r].rearrange("p (a m d) -> p a m d", m=2, d=32)
            r4 = qr[r].rearrange("p (a m d) -> p a m d", m=2, d=32)

            def tb(ti):
                return bc(cs_t[:, (0, 1, 1, 0)[ti], i, :].unsqueeze(1), [128, 8, 32])
            P.op("dve", lambda e: e.tensor_tensor(out=t4[:, :, 0, :], in0=x4[:, :, 0, :], in1=tb(0), op=ALU.mult),
                 reads=[tq, t_cs], writes=[tq])
            P.op("dve", lambda e: e.tensor_tensor(out=t4[:, :, 1, :], in0=x4[:, :, 1, :], in1=tb(1), op=ALU.mult),
                 reads=[tq, t_cs], writes=[tq])
            P.op("dve", lambda e: e.tensor_tensor(out=r4[:, :, 0, :], in0=t4[:, :, 0, :], in1=t4[:, :, 1, :], op=ALU.subtract),
                 reads=[tq], writes=[tq])
            P.op("dve", lambda e: e.tensor_tensor(out=t4[:, :, 0, :], in0=x4[:, :, 0, :], in1=tb(2), op=ALU.mult),
                 reads=[tq, t_cs], writes=[tq])
            P.op("dve", lambda e: e.tensor_tensor(out=t4[:, :, 1, :], in0=x4[:, :, 1, :], in1=tb(3), op=ALU.mult),
                 reads=[tq, t_cs], writes=[tq])
            P.op("dve", lambda e: e.tensor_tensor(out=r4[:, :, 1, :], in0=t4[:, :, 0, :], in1=t4[:, :, 1, :], op=ALU.add),
                 reads=[tq], writes=[tq])
            pbt = 6 + (s % 2)
            for hh in range(4):
                P.op("pe", lambda e, hh=hh: e.transpose(
                    out=bankb(pbt)[:, hh * 128:(hh + 1) * 128], in_=qr[r][:, hh * 128:(hh + 1) * 128],
                    identity=identb[:]), reads=[tq, t_const], writes=[t_ps[pbt]])
            dstT = (qT if qk == 0 else kT)
            P.op("act", lambda e: e.activation(
                out=dstT[:, h0:h0 + 4, i * 128:(i + 1) * 128],
                in_=bankb(pbt)[:, 0:512].rearrange("p (h t) -> p h t", t=128), func=AF.Copy),
                reads=[t_ps[pbt]], writes=[t_qT if qk == 0 else t_kT])

        stage1(0)
        stage2a(0)
        for s_ in range(len(steps)):
            if s_ + 1 < len(steps):
                stage1(s_ + 1)
            stage2b(s_)
            if s_ + 1 < len(steps):
                stage2a(s_ + 1)
        dump("qT", qT, [t_qT])
        dump("kT", kT, [t_kT])
        dump("vx", vx, [t_vx])
        P.barrier()
        ar.off = mark
        wout = ar.alloc([KD, D], BF16)
        t_wout = Tok("wout")
        P.dma("pool", wout, attn_w_out[j].rearrange("(k p) n -> p k n", p=128), writes=[t_wout])
        G1 = [ar.alloc([D], F32) for _ in range(2)]
        t_G1 = [Tok(), Tok()]
        load_gate(l, 2, b, G1[0], t_G1[0])
        load_gate(l, 2, 2, G1[1], t_G1[1])
        ru = ResUpd()
        osb = ar.alloc([4, D], F32)
        t_osb = Tok("osb")
        es = [ar.alloc([2, 512], BF16) for _ in range(2)]
        t_es = [Tok(), Tok()]
        fin = ar.alloc([16], F32)
        ost = ar.alloc([3, 388], F32)
        t_ost = Tok("ost")
        t_fin = Tok("fin")
        t_tsb = Tok("tsb")
        tsb = ar.alloc([128], F32)
        sqt = ar.alloc([D], F32)
        onb = ar.alloc([D], BF16)
        oT = ar.alloc([KD, 128], BF16)
        t_on = Tok("on")
        t_oT = Tok("oT")
        s8 = ar.alloc([32], F32)

        qranges = [(q0, 512, list(range(NTILE))) for q0 in range(0, S, 512)]
        if need_ctx:
            qranges.append((S, CT, [16, 17]))
        def head(h, q0, nq, ktiles, nqi):
            if True:
                def s_mm(ki):
                    kt = ktiles[ki]
                    j2 = ki % 2
                    for m in range(2):
                        pb = j2 * 2 + m
                        P.op("pe", lambda e, m=m, pb=pb, kt=kt: e.matmul(
                            psum[:, pb, 0:nq], lhsT=kT[m * 64:(m + 1) * 64, h, kt * 128:(kt + 1) * 128],
                            rhs=qT[m * 64:(m + 1) * 64, h, q0:q0 + nq], start=True, stop=True),
                            reads=[t_kT, t_qT], writes=[t_ps[pb]])
                    P.op("act", lambda e, j2=j2: e.activation(
                        out=es[j2][:, :, 0:nq], in_=psum[:, j2 * 2:j2 * 2 + 2, 0:nq], func=AF.Exp, scale=0.125),
                        reads=[t_ps[j2 * 2], t_ps[j2 * 2 + 1]], writes=[t_es[j2]])
                s_mm(0)
                for ki in range(len(ktiles)):
                    if ki + 1 < len(ktiles):
                        s_mm(ki + 1)
                    kt = ktiles[ki]
                    for m in range(2):
                        for qi in range(nqi):
                            gi = m * 4 + qi
                            pb = 4 + gi // 3
                            c0 = (gi % 3) * 129
                            P.op("pe", lambda e, m=m, qi=qi, pb=pb, c0=c0, kt=kt, ki=ki, gi=gi: e.matmul(
                                psum[:, pb, c0:c0 + 129], lhsT=es[ki % 2][:, m, qi * 128:(qi + 1) * 128],
                                rhs=vx[:, kt, h, 0:129], start=(ki == 0 and (gi % 3 == 0 or (nqi < 4 and qi == 0))),
                                stop=(ki == len(ktiles) - 1), skip_group_check=True),
                                reads=[t_es[ki % 2], t_vx], writes=[t_ps[pb]])
                nbk = 3 if nqi == 4 else 2
                for bk in range(nbk):
                    P.op("act", lambda e, bk=bk: e.activation(out=ost[:, bk, 0:387], in_=psum[:, 4 + bk, 0:387], func=AF.Copy),
                         reads=[t_ps[4 + bk]], writes=[t_ost])
                for bk in range(nbk):
                    P.op("dve", lambda e, bk=bk: e.reciprocal(out=fin[:, 3 * bk:3 * bk + 3], in_=ost[:, bk, 128:387:129]),
                         reads=[t_ost], writes=[t_fin])
                P.op("dve", lambda e: e.tensor_scalar(out=fin[:, 8:12], in0=fin[:, 4:8], scalar1=neglam, scalar2=None, op0=ALU.mult),
                     reads=[t_fin, t_small], writes=[t_fin])
                for qi in range(nqi):
                    g0, g1 = qi, 4 + qi
                    pb0, c00 = 4 + g0 // 3, (g0 % 3) * 129
                    pb1, c01 = 4 + g1 // 3, (g1 % 3) * 129
                    P.op("dve", lambda e, pb1=pb1, c01=c01, qi=qi: e.tensor_scalar(
                        out=tsb, in0=ost[:, pb1 - 4, c01:c01 + 128], scalar1=fin[:, 8 + qi:9 + qi], scalar2=None, op0=ALU.mult),
                        reads=[t_ost, t_fin], writes=[t_tsb])
                    P.op("dve", lambda e, pb0=pb0, c00=c00, qi=qi: e.scalar_tensor_tensor(
                        out=osb[:, qi, h * 128:(h + 1) * 128], in0=ost[:, pb0 - 4, c00:c00 + 128], scalar=fin[:, qi:qi + 1],
                        in1=tsb, op0=ALU.mult, op1=ALU.add), reads=[t_ost, t_fin, t_tsb], writes=[t_osb])
        onb4 = ar.alloc([4, D], BF16)

        def stageX(q0, nqi):
            for qi in range(nqi):
                P.op("dve", lambda e, qi=qi: e.tensor_tensor(out=sqt, in0=osb[:, qi, :], in1=osb[:, qi, :], op=ALU.mult),
                     reads=[t_osb], writes=[t_sq])
                P.op("dve", lambda e: e.tensor_reduce(out=s8[:, 0:8], in_=sqt.rearrange("p (a b) -> p a b", b=128),
                                                      axis=AX.X, op=ALU.add), reads=[t_sq], writes=[t_sq])
                P.op("dve", lambda e: e.tensor_scalar(out=s8[:, 8:16], in0=s8[:, 0:8], scalar1=1.0 / 128, scalar2=EPS,
                                                      op0=ALU.mult, op1=ALU.add), reads=[t_sq], writes=[t_sq])
                P.op("pool", lambda e: e.tensor_tensor(out=s8[:, 16:24], in0=s8[:, 8:16], in1=cm05[:, 0:8], op=ALU.pow),
                     reads=[t_sq, t_const], writes=[t_sq])
                P.op("dve", lambda e, qi=qi: e.tensor_tensor(
                    out=onb4[:, qi, :].rearrange("p (a b) -> p a b", b=128), in0=osb[:, qi, :].rearrange("p (a b) -> p a b", b=128),
                    in1=bc(s8[:, 16:24].unsqueeze(2), [128, 8, 128]), op=ALU.mult), reads=[t_sq, t_osb], writes=[t_on])

        def stageY(q0, nqi):
            for qi in range(nqi):
                i = q0 // 128 + qi
                ru.prefetch(b, i)
                for hh in range(8):
                    P.op("pe", lambda e, hh=hh, qi=qi: e.transpose(
                        out=bankb(7)[:, hh * 128:(hh + 1) * 128], in_=onb4[:, qi, hh * 128:(hh + 1) * 128],
                        identity=identb[:]), reads=[t_on, t_const], writes=[t_ps[7]])
                P.op("act", lambda e: e.activation(out=oT, in_=bankb(7).rearrange("p (h t) -> p h t", t=128),
                                                   func=AF.Copy, scale=sgs), reads=[t_ps[7], t_small], writes=[t_oT])
                for nh in range(2):
                    for hh in range(8):
                        P.op("pe", lambda e, nh=nh, hh=hh: e.matmul(
                            bank(nh), lhsT=oT[:, hh, :], rhs=wout[:, hh, nh * 512:(nh + 1) * 512],
                            start=(hh == 0), stop=(hh == 7)), reads=[t_oT, t_wout], writes=[t_ps[nh]])
                gs = 0 if i < 16 else 1
                ru.apply(b, i, [(bank(0), 0, 512), (bank(1), 512, 512)], [t_ps[0], t_ps[1]], G1[gs], t_G1[gs])

        t_sq = Tok("sq")
        pending = None
        for (q0, nq, ktiles) in qranges:
            nqi = nq // 128
            for h in range(8):
                head(h, q0, nq, ktiles, nqi)
            dump("osb", osb, [t_osb])
            if pending is not None:
                stageY(*pending)
            stageX(q0, nqi)
            pending = (q0, nqi)
        stageY(*pending)
        P.barrier()

    def lru_layer(l, b, need_ctx):
        ar.off = 0
        layer_setup(l)
        uT = ar.alloc([KD, TB], BF16)
        t_uT = Tok("uT")
        mark0 = ar.off
        nb = NormBufs(False)
        norm_all(nb, l, 0, b, list(range(NTILE)), uT, t_uT)
        P.barrier()
        ar.off = mark0
        mT = ar.alloc([10, TB], BF16)
        t_mT = Tok("mT")
        cw = ar.alloc([10, 4], F32)
        cb = ar.alloc([10], F32)
        bax = ar.alloc([2, 2, 10], F32)
        lam = ar.alloc([2, 10], F32)
        nsp = ar.alloc([2, 2, 10], F32)
        t_c = Tok("lruc")
        P.dma("sp", cw, lru_cwT, writes=[t_c])
        P.dma("sp", cb, lru_cbT, writes=[t_c])
        P.dma("sp", bax[:, 0], lru_baT, writes=[t_c])
        P.dma("sp", bax[:, 1], lru_bxT, writes=[t_c])
        P.dma("sp", lam, lru_lamT, writes=[t_c])
        P.op("act", lambda e: e.activation(out=lam, in_=lam, func=AF.Exp, scale=-1.0), reads=[t_c], writes=[t_c])
        P.op("act", lambda e: e.activation(out=lam, in_=lam, func=AF.Ln, bias=1.0), reads=[t_c], writes=[t_c])
        P.op("dve", lambda e: e.tensor_scalar(out=nsp[:, 0], in0=lam, scalar1=-8.0, scalar2=None, op0=ALU.mult),
             reads=[t_c], writes=[t_c])
        P.op("dve", lambda e: e.tensor_scalar(out=nsp[:, 1], in0=lam, scalar1=-16.0, scalar2=None, op0=ALU.mult),
             reads=[t_c], writes=[t_c])
        NWB = 2
        win = [ar.alloc([KD, 256], BF16) for _ in range(NWB)]
        t_win = [Tok() for _ in range(NWB)]
        wbd = [ar.alloc([4, 128], BF16) for _ in range(NWB)]
        t_wbd = [Tok() for _ in range(NWB)]
        xr = ar.alloc([LW], F32)
        xc = ar.alloc([LW], F32)
        xcb = ar.alloc([LW], BF16)
        Abuf = ar.alloc([LW], F32)
        Bbuf = ar.alloc([LW], F32)
        Tbuf = ar.alloc([LW], F32)
        H0 = ar.alloc([LW], F32)
        gate = ar.alloc([TB], BF16)
        t_x = Tok("lrux")
        t_A, t_B, t_T, t_H0, t_g = Tok(), Tok(), Tok(), Tok(), Tok()
        P.op("pool", lambda e: e.memset(xr, 0.0), writes=[t_x])
        wsrc = lru_w_in[0].rearrange("(k p) n -> p k n", p=128)
        segs = [(LAT0 + q, q, 512) for q in range(0, S, 512)] + [(CTX0, S, CT)]
        cnt = 0
        for g in range(10):
            ws = g % NWB
            P.dma("pool", win[ws][:, :, 0:128], wsrc[:, :, g * 128:(g + 1) * 128], writes=[t_win[ws]])
            P.dma("pool", win[ws][:, :, 128:256], wsrc[:, :, LRU_W + g * 128:LRU_W + (g + 1) * 128], writes=[t_win[ws]])
            for d in range(2):
                P.dma("pool", wbd[ws][:, 2 * d, :], lru_w_a[0, d, g], writes=[t_wbd[ws]])
                P.dma("pool", wbd[ws][:, 2 * d + 1, :], lru_w_x[0, d, g], writes=[t_wbd[ws]])
            for (pc, tc, n) in segs:
                for half in range(2):
                    pb = cnt % 4
                    cnt += 1
                    for k in range(KD):
                        P.op("pe", lambda e, k=k, pb=pb, half=half, tc=tc, n=n, ws=ws: e.matmul(
                            psum[:, pb, 0:n], lhsT=win[ws][:, k, half * 128:(half + 1) * 128],
                            rhs=uT[:, k, tc:tc + n], start=(k == 0), stop=(k == KD - 1)),
                            reads=[t_uT, t_win[ws]], writes=[t_ps[pb]])
                    if half == 0:
                        P.op("act", lambda e, pb=pb, tc=tc, n=n: e.activation(
                            out=gate[:, tc:tc + n], in_=psum[:, pb, 0:n], func=AF.Gelu_apprx_tanh),
                            reads=[t_ps[pb]], writes=[t_g])
                    else:
                        P.op("act", lambda e, pb=pb, pc=pc, n=n: e.activation(
                            out=xr[:, pc:pc + n], in_=psum[:, pb, 0:n], func=AF.Copy),
                            reads=[t_ps[pb]], writes=[t_x])
            W0, W1 = 2, 2309
            nW = W1 - W0
            P.op("dve", lambda e, g=g: e.tensor_scalar(
                out=xc[:, W0:W1], in0=xr[:, W0 - 2:W1 - 2], scalar1=cw[:, g, 0:1], scalar2=cb[:, g:g + 1],
                op0=ALU.mult, op1=ALU.add), reads=[t_x, t_c], writes=[t_x])
            for k in range(1, 4):
                P.op("dve", lambda e, g=g, k=k: e.scalar_tensor_tensor(
                    out=xc[:, W0:W1], in0=xr[:, W0 - 2 + k:W1 - 2 + k], scalar=cw[:, g, k:k + 1], in1=xc[:, W0:W1],
                    op0=ALU.mult, op1=ALU.add), reads=[t_x, t_c], writes=[t_x])
            P.op("pool", lambda e: e.tensor_copy(out=xcb[:, W0:W1], in_=xc[:, W0:W1]), reads=[t_x], writes=[t_x])
            for d in range(2):
                for (pc, tc, n) in segs:
                    for ax in range(2):
                        pb = 4 + cnt % 4
                        cnt += 1
                        P.op("pe", lambda e, pb=pb, pc=pc, n=n, ws=ws, d=d, ax=ax: e.matmul(
                            psum[:, pb, 0:n], lhsT=wbd[ws][:, 2 * d + ax, :], rhs=xcb[:, pc:pc + n],
                            start=True, stop=True), reads=[t_x, t_wbd[ws]], writes=[t_ps[pb]])
                        dstb = Abuf if ax == 0 else Bbuf
                        P.op("act", lambda e, pb=pb, pc=pc, n=n, dstb=dstb, ax=ax, d=d, g=g: e.activation(
                            out=dstb[:, pc:pc + n], in_=psum[:, pb, 0:n], func=AF.Sigmoid,
                            bias=bax[:, ax, d, g:g + 1]), reads=[t_ps[pb], t_c], writes=[t_A if ax == 0 else t_B])
                P.op("pool", lambda e: e.tensor_tensor(out=Bbuf[:, W0:W1], in0=Bbuf[:, W0:W1], in1=xc[:, W0:W1], op=ALU.mult),
                     reads=[t_B, t_x], writes=[t_B])
                P.op("act", lambda e, d=d, g=g: e.activation(out=Tbuf[:, W0:W1], in_=Abuf[:, W0:W1], func=AF.Exp,
                                                             scale=nsp[:, 1, d, g:g + 1]), reads=[t_A, t_c], writes=[t_T])
                P.op("act", lambda e: e.activation(out=Tbuf[:, W0:W1], in_=Tbuf[:, W0:W1], func=AF.Sqrt, scale=-1.0, bias=1.0),
                     reads=[t_T], writes=[t_T])
                rc = CTX0 if d == 0 else CTX0 + CT - 1
                P.op("dve", lambda e, rc=rc: e.memset(Tbuf[:, rc:rc + 1], 1.0), writes=[t_T])
                P.op("dve", lambda e: e.tensor_tensor(out=Bbuf[:, W0:W1], in0=Bbuf[:, W0:W1], in1=Tbuf[:, W0:W1], op=ALU.mult),
                     reads=[t_B, t_T], writes=[t_B])
                P.op("act", lambda e, d=d, g=g: e.activation(out=Abuf[:, W0:W1], in_=Abuf[:, W0:W1], func=AF.Exp,
                                                             scale=nsp[:, 0, d, g:g + 1]), reads=[t_A, t_c], writes=[t_A])
                Hd = H0 if d == 0 else Tbuf
                t_Hd = t_H0 if d == 0 else t_T
                csl = slice(CTX0, CTX0 + CT)
                lsl = slice(LAT0, LAT0 + S)
                if d == 0:
                    P.op("dve", lambda e, Hd=Hd: e.tensor_tensor_scan(
                        out=Hd[:, csl], data0=Abuf[:, csl], data1=Bbuf[:, csl], initial=0.0, op0=ALU.mult, op1=ALU.add),
                        reads=[t_A, t_B], writes=[t_Hd])
                    P.op("dve", lambda e, Hd=Hd: e.tensor_tensor_scan(
                        out=Hd[:, lsl], data0=Abuf[:, lsl], data1=Bbuf[:, lsl], initial=Hd[:, CTX0 + CT - 1:CTX0 + CT],
                        op0=ALU.mult, op1=ALU.add), reads=[t_A, t_B, t_Hd], writes=[t_Hd])
                else:
                    P.op("dve", lambda e, Hd=Hd: e.tensor_tensor_scan(
                        out=rev2d(Hd[:, csl]), data0=rev2d(Abuf[:, csl]), data1=rev2d(Bbuf[:, csl]), initial=0.0,
                        op0=ALU.mult, op1=ALU.add), reads=[t_A, t_B], writes=[t_Hd])
                    P.op("dve", lambda e, Hd=Hd: e.tensor_tensor_scan(
                        out=rev2d(Hd[:, lsl]), data0=rev2d(Abuf[:, lsl]), data1=rev2d(Bbuf[:, lsl]),
                        initial=Hd[:, CTX0:CTX0 + 1], op0=ALU.mult, op1=ALU.add), reads=[t_A, t_B, t_Hd], writes=[t_Hd])
            P.op("pool", lambda e: e.tensor_tensor(out=H0[:, W0:W1], in0=H0[:, W0:W1], in1=Tbuf[:, W0:W1], op=ALU.add),
                 reads=[t_H0, t_T], writes=[t_H0])
            P.op("dve", lambda e, g=g: e.tensor_tensor(out=mT[:, g, 0:S], in0=H0[:, LAT0:LAT0 + S], in1=gate[:, 0:S], op=ALU.mult),
                 reads=[t_H0, t_g], writes=[t_mT])
            P.op("dve", lambda e, g=g: e.tensor_tensor(out=mT[:, g, S:TB], in0=H0[:, CTX0:CTX0 + CT], in1=gate[:, S:TB], op=ALU.mult),
                 reads=[t_H0, t_g], writes=[t_mT])
        P.barrier()
        ar.off = mark0 + 10 * TB * 2
        wout = ar.alloc([10, D], BF16)
        t_wout = Tok("wout")
        P.dma("pool", wout, lru_w_out[0].rearrange("(k p) n -> p k n", p=128), writes=[t_wout])
        G1 = [ar.alloc([D], F32) for _ in range(2)]
        t_G1 = [Tok(), Tok()]
        load_gate(l, 2, b, G1[0], t_G1[0])
        load_gate(l, 2, 2, G1[1], t_G1[1])
        ru = ResUpd()
        ntl = NTILE if need_ctx else 16
        for i in range(ntl):
            ru.prefetch(b, i)
            pbs = (2 * (i % 2), 2 * (i % 2) + 1)
            for nh in range(2):
                for g in range(10):
                    P.op("pe", lambda e, nh=nh, g=g, i=i, pbs=pbs: e.matmul(
                        bank(pbs[nh]), lhsT=mT[:, g, i * 128:(i + 1) * 128], rhs=wout[:, g, nh * 512:(nh + 1) * 512],
                        start=(g == 0), stop=(g == 9)), reads=[t_mT, t_wout], writes=[t_ps[pbs[nh]]])
            gs = 0 if i < 16 else 1
            ru.apply(b, i, [(bank(pbs[0]), 0, 512), (bank(pbs[1]), 512, 512)], [t_ps[pbs[0]], t_ps[pbs[1]]],
                     G1[gs], t_G1[gs])
        P.barrier()

    def pool_layer(l, b, need_ctx):
        ar.off = 0
        layer_setup(l)
        uT = ar.alloc([KD, TB], BF16)
        t_uT = Tok("uT")
        mark0 = ar.off
        nb = NormBufs(False)
        norm_all(nb, l, 0, b, list(range(NTILE)), uT, t_uT)
        P.barrier()
        ar.off = mark0
        pT = ar.alloc([KD, TB], BF16)
        t_pT = Tok("pT")
        edge = ar.alloc([4, 2, 8], F32)
        t_e = Tok("edge")
        P.dma("sp", edge, pool_edge, writes=[t_e])
        NWB = 2
        win = [ar.alloc([KD, 128], BF16) for _ in range(NWB)]
        t_win = [Tok() for _ in range(NWB)]
        PADW = 8
        XL = [ar.alloc([S + 16], F32) for _ in range(3)]
        XC = [ar.alloc([CT + 16], F32) for _ in range(3)]
        t_X = Tok("poolx")
        for t in XL + XC:
            P.op("pool", lambda e, t=t: e.memset(t, 0.0), writes=[t_X])
        wsrc = pool_w_in[0].rearrange("(k p) n -> p k n", p=128)
        cnt = 0
        for c in range(8):
            gidx = c // 2
            w = (2, 4, 8, 16)[gidx]
            ws = c % NWB
            P.dma("pool", win[ws], wsrc[:, :, c * 128:(c + 1) * 128], writes=[t_win[ws]])
            for (tc, n, X, off) in [(q, 512, XL, q) for q in range(0, S, 512)] + [(S, CT, XC, 0)]:
                pb = cnt % 4
                cnt += 1
                for k in range(KD):
                    P.op("pe", lambda e, k=k, pb=pb, tc=tc, n=n, ws=ws: e.matmul(
                        psum[:, pb, 0:n], lhsT=win[ws][:, k, :], rhs=uT[:, k, tc:tc + n],
                        start=(k == 0), stop=(k == KD - 1)), reads=[t_uT, t_win[ws]], writes=[t_ps[pb]])
                P.op("act", lambda e, pb=pb, n=n, X=X, off=off: e.activation(
                    out=X[0][:, PADW + off:PADW + off + n], in_=psum[:, pb, 0:n], func=AF.Copy),
                    reads=[t_ps[pb]], writes=[t_X])
            for (X, n, tc) in ((XL, S, 0), (XC, CT, S)):
                x0 = X[0]
                cur, other = X[1], X[2]
                lo, hi = 1, PADW + n + 8
                P.op("dve", lambda e, x0=x0, cur=cur, lo=lo, hi=hi: e.tensor_tensor(
                    out=cur[:, lo:hi], in0=x0[:, lo - 1:hi - 1], in1=x0[:, lo:hi], op=ALU.add), reads=[t_X], writes=[t_X])
                step = 1
                ww = 2
                while ww < w:
                    sp_ = ww // 2
                    lo, hi = lo + sp_, hi - sp_
                    P.op("dve", lambda e, cur=cur, other=other, lo=lo, hi=hi, sp_=sp_: e.tensor_tensor(
                        out=other[:, lo:hi], in0=cur[:, lo - sp_:hi - sp_], in1=cur[:, lo + sp_:hi + sp_], op=ALU.add),
                        reads=[t_X], writes=[t_X])
                    cur, other = other, cur
                    ww *= 2
                P.op("dve", lambda e, cur=cur, x0=x0, n=n, tc=tc, c=c, w=w: e.scalar_tensor_tensor(
                    out=pT[:, c, tc:tc + n], in0=cur[:, PADW:PADW + n], scalar=1.0 / w, in1=x0[:, PADW:PADW + n],
                    op0=ALU.mult, op1=ALU.subtract), reads=[t_X], writes=[t_pT])
                hw = w // 2
                for side in range(2):
                    a0 = PADW if side == 0 else PADW + n - 8
                    o0 = tc if side == 0 else tc + n - 8
                    P.op("dve", lambda e, cur=cur, a0=a0, side=side, gidx=gidx, other=other: e.tensor_tensor(
                        out=other[:, 0:8], in0=cur[:, a0:a0 + 8], in1=edge[:, gidx, side, :], op=ALU.mult),
                        reads=[t_X, t_e], writes=[t_X])
                    P.op("dve", lambda e, x0=x0, a0=a0, o0=o0, c=c, other=other: e.tensor_tensor(
                        out=pT[:, c, o0:o0 + 8], in0=other[:, 0:8], in1=x0[:, a0:a0 + 8], op=ALU.subtract),
                        reads=[t_X], writes=[t_pT])
        P.barrier()
        ar.off = mark0 + KD * TB * 2
        wg = ar.alloc([4, 2, 256], BF16)
        t_wg = Tok("wg")
        for g in range(4):
            P.dma("pool", wg[:, g], pool_w_grp[0, g].rearrange("(k p) n -> p k n", p=128), writes=[t_wg])
        G1 = [ar.alloc([D], F32) for _ in range(2)]
        t_G1 = [Tok(), Tok()]
        load_gate(l, 2, b, G1[0], t_G1[0])
        load_gate(l, 2, 2, G1[1], t_G1[1])
        psc = ar.alloc([D], F32)
        t_psc = Tok("psc")
        a = pool_scale[0:1, :]
        P.dma("sp", psc, AP(a.tensor, a.offset, [[0, 128], [1, D]]), writes=[t_psc])
        for s in range(2):
            P.op("dve", lambda e, s=s: e.tensor_tensor(out=G1[s], in0=G1[s], in1=psc, op=ALU.mult),
                 reads=[t_psc, t_G1[s]], writes=[t_G1[s]])
        ru = ResUpd()
        ntl = NTILE if need_ctx else 16
        for i in range(ntl):
            ru.prefetch(b, i)
            pbs = (2 * (i % 2), 2 * (i % 2) + 1)
            for g in range(4):
                pb = pbs[g // 2]
                c0 = (g % 2) * 256
                for kc in range(2):
                    P.op("pe", lambda e, g=g, kc=kc, pb=pb, c0=c0, i=i: e.matmul(
                        psum[:, pb, c0:c0 + 256], lhsT=pT[:, 2 * g + kc, i * 128:(i + 1) * 128], rhs=wg[:, g, kc, :],
                        start=(kc == 0 and g % 2 == 0), stop=(kc == 1), skip_group_check=True),
                        reads=[t_pT, t_wg], writes=[t_ps[pb]])
            gs = 0 if i < 16 else 1
            ru.apply(b, i, [(bank(pbs[0]), 0, 512), (bank(pbs[1]), 512, 512)], [t_ps[pbs[0]], t_ps[pbs[1]]],
                     G1[gs], t_G1[gs])
        P.barrier()

    I32 = mybir.dt.int32

    def moe_sparse(l, last):
        tl = [(b, i) for b in range(2) for i in range(NTILE if not last else 16)]
        NTT = len(tl)
        NTE = NTT * 2 * 128 // 512 + 15
        ar.off = 0
        layer_setup(l)
        S1a = ar.alloc([NTT, 16], F32)
        S2a = ar.alloc([NTT, 16], F32)
        wts = ar.alloc([NTT, 2], F32)
        posa = ar.alloc([NTT, 16], F32)
        base = ar.alloc([16], F32)
        sidx = ar.alloc([NTT, 2], I32)
        widx = ar.alloc([NTE, 2], I32)
        t_S, t_base, t_idx = Tok("S"), Tok("base"), Tok("idx")
        G2 = [ar.alloc([D], F32) for _ in range(3)]
        t_G2 = [Tok() for _ in range(3)]
        for s_ in range(3):
            load_gate(l, 5, s_, G2[s_], t_G2[s_])
        mark_keep = ar.off
        ub = ar.alloc([NTT, D], BF16)
        t_ub = [Tok() for _ in range(NTT)]
        At = [ar.alloc([D], F32) for _ in range(3)]
        Bt = [ar.alloc([D], F32) for _ in range(3)]
        t_AB = Tok("ABtok")
        gn = ar.alloc([D], F32)
        a_ = normg_raw[l, 1:2, :]
        P.dma("sp", gn, AP(a_.tensor, a_.offset, [[0, 128], [1, D]]), writes=[t_AB])
        for s_ in range(3):
            load_gate(l, 4, s_, At[s_], t_AB)
            load_gate(l, 3, s_, Bt[s_], t_AB)
            P.op("dve", lambda e, s_=s_: e.scalar_tensor_tensor(out=At[s_], in0=At[s_], scalar=1.0, in1=gn,
                                                                op0=ALU.add, op1=ALU.mult), reads=[t_AB], writes=[t_AB])
        wr = ar.alloc([KD, 20], F32)
        t_wr = Tok("wr")
        P.dma("sp", wr, moe_router[l].rearrange("(k p) n -> p k n", p=128), writes=[t_wr])
        utri = ar.alloc([128], BF16)
        onesb = ar.alloc([128], BF16)
        cst = ar.alloc([128 + 40 + 1], F32)
        t_cst = Tok("cst")
        P.dma("sp", cst[:, 0:128], utri_d, writes=[t_cst])
        P.dma("sp", cst[:, 128:168], tvals_d, writes=[t_cst])
        P.dma("sp", cst[:, 168:169], pidx_d, writes=[t_cst])
        P.op("dve", lambda e: e.tensor_copy(out=utri, in_=cst[:, 0:128]), reads=[t_cst], writes=[t_cst])
        P.op("dve", lambda e: e.memset(onesb, 1.0), writes=[t_cst])
        P.op("dve", lambda e: e.memset(base, 0.0), writes=[t_base])
        NX, NH, NU = 4, 3, 3
        xts = [ar.alloc([D], F32) for _ in range(NX)]
        t_xts = [Tok() for _ in range(NX)]
        sts = [ar.alloc([4], F32) for _ in range(NX)]
        t_sts = [Tok() for _ in range(NX)]
        junk = ar.alloc([D], BF16)
        t_junk = Tok()
        xhs = [ar.alloc([D], F32) for _ in range(NH)]
        t_xhs = [Tok() for _ in range(NH)]
        u32s = [ar.alloc([KD, 128], F32) for _ in range(NU)]
        t_u32s = [Tok() for _ in range(NU)]
        rt = [ar.alloc([96], F32) for _ in range(2)]
        t_rt = [Tok(), Tok()]
        mb = [ar.alloc([16], BF16) for _ in range(2)]
        utmp = [ar.alloc([D], F32) for _ in range(2)]
        t_ut = [Tok(), Tok()]

        def stA1(ti):
            b, i = tl[ti]
            xt, st, xh = xts[ti % NX], sts[ti % NX], xhs[ti % NH]
            t_xt, t_st, t_xh = t_xts[ti % NX], t_sts[ti % NX], t_xhs[ti % NH]
            r0 = rows(b, i)
            P.dma("sp", xt, res[r0:r0 + 128, :], reads=[t_res[b][i]], writes=[t_xt])
            P.op("act", lambda e: e.activation(out=junk, in_=xt, func=AF.Square, accum_out=st[:, 0:1]),
                 reads=[t_xt], writes=[t_junk, t_st])
            P.op("dve", lambda e: e.tensor_scalar(out=st[:, 1:2], in0=st[:, 0:1], scalar1=1.0 / D, scalar2=EPS,
                                                  op0=ALU.mult, op1=ALU.add), reads=[t_st], writes=[t_st])
            P.op("pool", lambda e: e.tensor_tensor(out=st[:, 2:3], in0=st[:, 1:2], in1=cm05[:, 0:1], op=ALU.pow),
                 reads=[t_st, t_const], writes=[t_st])
            P.op("act", lambda e: e.activation(out=xh, in_=xt, func=AF.Copy, scale=st[:, 2:3]),
                 reads=[t_xt, t_st], writes=[t_xh])
            pb0 = 2 * (ti % 2)
            for k in range(KD):
                pb = pb0 + k // 4
                P.op("pe", lambda e, k=k, pb=pb: e.transpose(
                    out=psum[:, pb, (k % 4) * 128:(k % 4 + 1) * 128], in_=xh[:, k * 128:(k + 1) * 128],
                    identity=ident32[:]), reads=[t_xh, t_const], writes=[t_ps[pb]])

        def stA2(ti):
            b, i = tl[ti]
            xh, u32 = xhs[ti % NH], u32s[ti % NU]
            t_xh, t_u32 = t_xhs[ti % NH], t_u32s[ti % NU]
            s_ = mset(b, i)
            s2 = ti % 2
            pb0 = 2 * (ti % 2)
            A = Amod[:, 1, s_, :]
            B = Bmod(l, 1, s_)
            for k in range(KD):
                pb = pb0 + k // 4
                P.op("dve", lambda e, k=k, pb=pb: e.tensor_scalar(
                    out=u32[:, k, :], in0=psum[:, pb, (k % 4) * 128:(k % 4 + 1) * 128],
                    scalar1=A[:, k:k + 1], scalar2=B[:, k:k + 1], op0=ALU.mult, op1=ALU.add),
                    reads=[t_ps[pb], t_amod, t_mods], writes=[t_u32])
            P.op("pool", lambda e: e.tensor_tensor(out=utmp[s2], in0=xh, in1=At[s_], op=ALU.mult),
                 reads=[t_xh, t_AB], writes=[t_ut[s2]])
            P.op("pool", lambda e: e.tensor_tensor(out=ub[:, ti, :], in0=utmp[s2], in1=Bt[s_], op=ALU.add),
                 reads=[t_ut[s2], t_AB], writes=[t_ub[ti]])

        def stB(ti):
            u32, t_u32 = u32s[ti % NU], t_u32s[ti % NU]
            s2 = ti % 2
            R = rt[s2]
            tr = t_rt[s2]
            pb = 4 + s2
            for k in range(KD):
                P.op("pe", lambda e, k=k: e.matmul(
                    psum[:, pb, 0:20], lhsT=u32[:, k, :], rhs=wr[:, k, :], start=(k == 0), stop=(k == KD - 1)),
                    reads=[t_u32, t_wr], writes=[t_ps[pb]])
            P.op("dve", lambda e: e.tensor_copy(out=R[:, 0:20], in_=psum[:, pb, 0:20]), reads=[t_ps[pb]], writes=[tr])
            P.op("dve", lambda e: e.tensor_reduce(out=R[:, 20:21], in_=R[:, 0:4], axis=AX.X, op=ALU.max), reads=[tr], writes=[tr])
            P.op("dve", lambda e: e.tensor_scalar(out=R[:, 21:22], in0=R[:, 20:21], scalar1=-1.0, scalar2=None, op0=ALU.mult), reads=[tr], writes=[tr])
            P.op("dve", lambda e: e.tensor_scalar(out=R[:, 24:28], in0=R[:, 0:4], scalar1=R[:, 20:21], scalar2=None, op0=ALU.is_equal), reads=[tr], writes=[tr])
            P.op("act", lambda e: e.activation(out=R[:, 28:32], in_=R[:, 0:4], func=AF.Exp, bias=R[:, 21:22], accum_out=R[:, 32:33]), reads=[tr], writes=[tr])
            P.op("dve", lambda e: e.tensor_tensor(
                out=R[:, 36:52].rearrange("p (g j) -> p g j", j=4), in0=R[:, 4:20].rearrange("p (g j) -> p g j", j=4),
                in1=bc(R[:, 24:28].unsqueeze(2), [128, 4, 4]), op=ALU.mult), reads=[tr], writes=[tr])
            P.op("dve", lambda e: e.tensor_reduce(out=R[:, 52:56], in_=R[:, 36:52].rearrange("p (g j) -> p j g", j=4),
                                                  axis=AX.X, op=ALU.add), reads=[tr], writes=[tr])
            P.op("dve", lambda e: e.tensor_reduce(out=R[:, 56:57], in_=R[:, 52:56], axis=AX.X, op=ALU.max), reads=[tr], writes=[tr])
            P.op("dve", lambda e: e.tensor_scalar(out=R[:, 57:58], in0=R[:, 56:57], scalar1=-1.0, scalar2=None, op0=ALU.mult), reads=[tr], writes=[tr])
            P.op("dve", lambda e: e.tensor_scalar(out=R[:, 60:64], in0=R[:, 52:56], scalar1=R[:, 56:57], scalar2=None, op0=ALU.is_equal), reads=[tr], writes=[tr])
            P.op("dve", lambda e: e.scalar_tensor_tensor(out=R[:, 64:68], in0=R[:, 60:64], scalar=-1e30, in1=R[:, 52:56],
                                                         op0=ALU.mult, op1=ALU.add), reads=[tr], writes=[tr])
            P.op("dve", lambda e: e.tensor_reduce(out=R[:, 68:69], in_=R[:, 64:68], axis=AX.X, op=ALU.max), reads=[tr], writes=[tr])
            P.op("dve", lambda e: e.tensor_scalar(out=R[:, 72:76], in0=R[:, 64:68], scalar1=R[:, 68:69], scalar2=None, op0=ALU.is_equal), reads=[tr], writes=[tr])
            P.op("act", lambda e: e.activation(out=R[:, 76:77], in_=R[:, 68:69], func=AF.Exp, bias=R[:, 57:58]), reads=[tr], writes=[tr])
            P.op("dve", lambda e: e.tensor_tensor(
                out=S1a[:, ti, :].rearrange("p (g j) -> p g j", j=4), in0=bc(R[:, 24:28].unsqueeze(2), [128, 4, 4]),
                in1=bc(R[:, 60:64].unsqueeze(1), [128, 4, 4]), op=ALU.mult), reads=[tr], writes=[t_S])
            P.op("dve", lambda e: e.tensor_tensor(
                out=S2a[:, ti, :].rearrange("p (g j) -> p g j", j=4), in0=bc(R[:, 24:28].unsqueeze(2), [128, 4, 4]),
                in1=bc(R[:, 72:76].unsqueeze(1), [128, 4, 4]), op=ALU.mult), reads=[tr], writes=[t_S])
            P.op("dve", lambda e: e.tensor_tensor(out=mb[s2], in0=S1a[:, ti, :], in1=S2a[:, ti, :], op=ALU.add),
                 reads=[t_S], writes=[tr])
            pbp = 6 + s2
            P.op("pe", lambda e: e.matmul(psum[:, pbp, 0:16], lhsT=utri, rhs=mb[s2], start=True, stop=True),
                 reads=[tr, t_cst], writes=[t_ps[pbp]])
            P.op("pe", lambda e: e.matmul(psum[:, pbp, 16:32], lhsT=onesb, rhs=mb[s2], start=False, stop=True, skip_group_check=True),
                 reads=[tr, t_cst], writes=[t_ps[pbp]])
            P.op("dve", lambda e: e.reciprocal(out=R[:, 33:34], in_=R[:, 32:33]), reads=[tr], writes=[tr])
            P.op("dve", lambda e: e.tensor_scalar(out=R[:, 77:78], in0=R[:, 76:77], scalar1=1.0, scalar2=None, op0=ALU.add), reads=[tr], writes=[tr])
            P.op("dve", lambda e: e.reciprocal(out=R[:, 78:79], in_=R[:, 77:78]), reads=[tr], writes=[tr])
            P.op("dve", lambda e: e.tensor_tensor(out=wts[:, ti, 0:1], in0=R[:, 78:79], in1=R[:, 33:34], op=ALU.mult), reads=[tr], writes=[tr, t_S])
            P.op("dve", lambda e: e.tensor_tensor(out=wts[:, ti, 1:2], in0=wts[:, ti, 0:1], in1=R[:, 76:77], op=ALU.mult), reads=[tr, t_S], writes=[t_S])

        def stC(ti):
            pbp = 6 + ti % 2
            P.op("dve", lambda e: e.tensor_tensor(out=posa[:, ti, :], in0=psum[:, pbp, 0:16], in1=base, op=ALU.add),
                 reads=[t_ps[pbp], t_base], writes=[t_S])
            P.op("dve", lambda e: e.tensor_tensor(out=base, in0=psum[:, pbp, 16:32], in1=base, op=ALU.add),
                 reads=[t_ps[pbp], t_base], writes=[t_base])

        stA1(0)
        if NTT > 1:
            stA1(1)
        stA2(0)
        for ti in range(NTT):
            if ti + 2 < NTT:
                stA1(ti + 2)
            if ti + 1 < NTT:
                stA2(ti + 1)
            if ti >= 1:
                stC(ti - 1)
            stB(ti)
        stC(NTT - 1)
        sg = ar.alloc([96], F32)
        sgi = ar.alloc([16], I32)
        t_sg = Tok("sg")
        P.op("dve", lambda e: e.tensor_scalar(out=sg[:, 0:16], in0=base, scalar1=511.0, scalar2=None, op0=ALU.add),
             reads=[t_base], writes=[t_sg])
        P.op("dve", lambda e: e.tensor_copy(out=sgi, in_=sg[:, 0:16]), reads=[t_sg], writes=[t_sg])
        P.op("dve", lambda e: e.tensor_scalar(out=sgi, in0=sgi, scalar1=9, scalar2=9, op0=ALU.arith_shift_right,
                                              op1=ALU.logical_shift_left), reads=[t_sg], writes=[t_sg])
        P.op("dve", lambda e: e.tensor_copy(out=sg[:, 0:16], in_=sgi), reads=[t_sg], writes=[t_sg])
        P.op("dve", lambda e: e.memset(sg[:, 48:64], 1.0), writes=[t_sg])
        P.op("dve", lambda e: e.tensor_tensor_scan(out=sg[:, 16:32], data0=sg[:, 48:64], data1=sg[:, 0:16], initial=0.0,
                                                   op0=ALU.mult, op1=ALU.add), reads=[t_sg], writes=[t_sg])
        P.op("dve", lambda e: e.tensor_tensor(out=sg[:, 32:48], in0=sg[:, 16:32], in1=sg[:, 0:16], op=ALU.subtract),
             reads=[t_sg], writes=[t_sg])
        big = ar.alloc([NTT, 16], F32)
        slf = ar.alloc([NTT, 2], F32)
        P.op("dve", lambda e: e.tensor_tensor(out=posa, in0=posa, in1=bc(sg[:, 32:48].unsqueeze(1), [128, NTT, 16]), op=ALU.add),
             reads=[t_S, t_sg], writes=[t_S])
        for k_, Sk in enumerate((S1a, S2a)):
            P.op("dve", lambda e, Sk=Sk: e.tensor_tensor(out=big, in0=posa, in1=Sk, op=ALU.mult), reads=[t_S], writes=[t_sg])
            P.op("dve", lambda e, k_=k_: e.tensor_reduce(out=slf[:, :, k_], in_=big, axis=AX.X, op=ALU.add), reads=[t_sg], writes=[t_sg])
        P.op("dve", lambda e: e.tensor_copy(out=sidx, in_=slf), reads=[t_sg], writes=[t_idx])
        cmp = ar.alloc([NTE, 16], F32)
        etf = ar.alloc([NTE, 4], F32)
        P.op("dve", lambda e: e.tensor_tensor(out=cmp, in0=bc(sg[:, 16:32].unsqueeze(1), [128, NTE, 16]),
                                              in1=bc(cst[:, 128:128 + NTE].unsqueeze(2), [128, NTE, 16]), op=ALU.is_le),
             reads=[t_sg, t_cst], writes=[t_sg])
        P.op("dve", lambda e: e.tensor_reduce(out=etf[:, :, 0], in_=cmp, axis=AX.X, op=ALU.add), reads=[t_sg], writes=[t_sg])
        P.op("dve", lambda e: e.tensor_scalar(out=etf[:, :, 1], in0=etf[:, :, 0], scalar1=15.0, scalar2=256.0, op0=ALU.min, op1=ALU.mult),
             reads=[t_sg], writes=[t_sg])
        P.op("dve", lambda e: e.tensor_scalar(out=etf[:, :, 1], in0=etf[:, :, 1], scalar1=float(l * N_EXP * 256), scalar2=None, op0=ALU.add),
             reads=[t_sg], writes=[t_sg])
        P.op("dve", lambda e: e.scalar_tensor_tensor(out=etf[:, :, 2], in0=bc(cst[:, 168:169], [128, NTE]), scalar=2.0, in1=etf[:, :, 1],
                                                     op0=ALU.mult, op1=ALU.add), reads=[t_sg, t_cst], writes=[t_sg])
        P.op("dve", lambda e: e.tensor_scalar(out=etf[:, :, 3], in0=etf[:, :, 2], scalar1=1.0, scalar2=None, op0=ALU.add),
             reads=[t_sg], writes=[t_sg])
        P.op("dve", lambda e: e.tensor_copy(out=widx, in_=etf[:, :, 2:4]), reads=[t_sg], writes=[t_idx])
        t_xg = Tok("xg")
        for ti in range(NTT):
            for k_ in range(2):
                P.op("pool", lambda e, ti=ti, k_=k_: e.indirect_dma_start(
                    out=xg, out_offset=bass.IndirectOffsetOnAxis(ap=sidx[:, ti, k_:k_ + 1], axis=0),
                    in_=ub[:, ti, :], in_offset=None), reads=[t_ub[ti], t_idx], writes=[Tok()], dma=True)
        dump("sidx", slf, [t_sg])
        dump("etf", etf[:, :, 0], [t_sg])
        dump("wts", wts, [t_S])
        P.barrier()
        ar.off = mark_keep
        NWB = 2
        w1 = [ar.alloc([KD, DE], BF16) for _ in range(NWB)]
        w3 = [ar.alloc([KD, DE], BF16) for _ in range(NWB)]
        w2 = [ar.alloc([4, D], BF16) for _ in range(NWB)]
        t_w1 = [Tok() for _ in range(NWB)]
        t_w3 = [Tok() for _ in range(NWB)]
        t_w2 = [Tok() for _ in range(NWB)]
        NSTG = 8
        stage = [ar.alloc([2048], F32) for _ in range(NSTG)]
        t_stage = [Tok() for _ in range(NSTG)]
        scnt = 0
        NXT = 8
        xtok = [ar.alloc([D], BF16) for _ in range(NXT)]
        t_xtok = [Tok() for _ in range(NXT)]
        xT = [ar.alloc([KD, 512], BF16) for _ in range(2)]
        t_xT = [Tok(), Tok()]
        hT = [ar.alloc([4, 512], BF16) for _ in range(2)]
        t_hT = [Tok(), Tok()]
        sl = [ar.alloc([512], F32) for _ in range(2)]
        t_sl = [Tok(), Tok()]
        ysb = [ar.alloc([D], F32) for _ in range(3)]
        t_ysb = [Tok() for _ in range(3)]
        t_yg = Tok("yg")
        w1r = moe_w1r.rearrange("l r n -> (l r) n")
        w3r = moe_w3r.rearrange("l r n -> (l r) n")
        w2r = moe_w2r.rearrange("l r n -> (l r) n")
        cntb = [0, 0]

        def issue_loads(t):
            ws = t % NWB
            for wi_, wsrc in enumerate((w1r, w3r, w2r)):
                for hf in range(2):
                    si = (6 * t + 2 * wi_ + hf) % NSTG
                    P.op("pool", lambda e, si=si, wsrc=wsrc, t=t, hf=hf: e.indirect_dma_start(
                        out=stage[si], out_offset=None, in_=wsrc,
                        in_offset=bass.IndirectOffsetOnAxis(ap=widx[:, t, hf:hf + 1], axis=0)),
                        reads=[t_idx], writes=[t_stage[si]], dma=True)
            for i4 in range(4):
                xi = (4 * t + i4) % NXT
                r0 = t * 512 + i4 * 128
                P.dma("sp", xtok[xi], xg[r0:r0 + 128, :], reads=[t_xg], writes=[t_xtok[xi]])

        def casts(t):
            ws = t % NWB
            for wi_, (wt, tw, cengs) in enumerate(((w1, t_w1, ("dve", "dve")), (w3, t_w3, ("act", "act")),
                                                   (w2, t_w2, ("act", "dve")))):
                for hf in range(2):
                    si = (6 * t + 2 * wi_ + hf) % NSTG
                    dst = wt[ws].rearrange("p a b -> p (a b)")[:, hf * 2048:(hf + 1) * 2048]
                    if cengs[hf] == "act":
                        P.op("act", lambda e, si=si, dst=dst: e.activation(out=dst, in_=stage[si], func=AF.Copy),
                             reads=[t_stage[si]], writes=[tw[ws]])
                    else:
                        P.op("dve", lambda e, si=si, dst=dst: e.tensor_copy(out=dst, in_=stage[si]),
                             reads=[t_stage[si]], writes=[tw[ws]])

        def trans(t):
            xs_ = t % 2
            for i4 in range(4):
                xi = (4 * t + i4) % NXT
                pbt = i4 % 2
                for k in range(KD):
                    P.op("pe", lambda e, k=k, pbt=pbt, xi=xi: e.transpose(
                        out=bankb(pbt)[:, k * 128:(k + 1) * 128], in_=xtok[xi][:, k * 128:(k + 1) * 128],
                        identity=identb[:]), reads=[t_xtok[xi], t_const], writes=[t_ps[pbt]])
                src_ = bankb(pbt).rearrange("p (k t) -> p k t", t=128)
                dst_ = xT[xs_][:, :, i4 * 128:(i4 + 1) * 128]
                if i4 % 2 == 0:
                    P.op("act", lambda e, src_=src_, dst_=dst_: e.activation(out=dst_, in_=src_, func=AF.Copy),
                         reads=[t_ps[pbt]], writes=[t_xT[xs_]])
                else:
                    P.op("dve", lambda e, src_=src_, dst_=dst_: e.tensor_copy(out=dst_, in_=src_),
                         reads=[t_ps[pbt]], writes=[t_xT[xs_]])

        def mm1(t):
            ws, xs_, hs = t % NWB, t % 2, t % 2
            for hc in range(4):
                pb1 = 2 + (cntb[0] % 2) * 2
                pb3 = pb1 + 1
                s2 = cntb[0] % 2
                cntb[0] += 1
                for (wt, pb, twt) in ((w1, pb1, t_w1), (w3, pb3, t_w3)):
                    for k in range(KD):
                        P.op("pe", lambda e, wt=wt, pb=pb, k=k, hc=hc, ws=ws, xs_=xs_: e.matmul(
                            bank(pb), lhsT=wt[ws][:, k, hc * 128:(hc + 1) * 128], rhs=xT[xs_][:, k, :],
                            start=(k == 0), stop=(k == KD - 1)), reads=[t_xT[xs_], twt[ws]], writes=[t_ps[pb]])
                P.op("act", lambda e, pb1=pb1, s2=s2: e.activation(out=sl[s2], in_=bank(pb1), func=AF.Silu),
                     reads=[t_ps[pb1]], writes=[t_sl[s2]])
                P.op("dve", lambda e, pb3=pb3, s2=s2, hs=hs, hc=hc: e.tensor_tensor(
                    out=hT[hs][:, hc, :], in0=bank(pb3), in1=sl[s2], op=ALU.mult),
                    reads=[t_ps[pb3], t_sl[s2]], writes=[t_hT[hs]])

        def mm2(t):
            ws, hs = t % NWB, t % 2
            for tq in range(4):
                yi = cntb[1] % 3
                cntb[1] += 1
                for nh in range(2):
                    pb = (6 + nh) if tq % 2 == 0 else nh
                    for hc in range(4):
                        P.op("pe", lambda e, pb=pb, hc=hc, tq=tq, nh=nh, hs=hs, ws=ws: e.matmul(
                            bank(pb), lhsT=hT[hs][:, hc, tq * 128:(tq + 1) * 128], rhs=w2[ws][:, hc, nh * 512:(nh + 1) * 512],
                            start=(hc == 0), stop=(hc == 3)), reads=[t_hT[hs], t_w2[ws]], writes=[t_ps[pb]])
                    if nh == 0:
                        P.op("act", lambda e, pb=pb, yi=yi: e.activation(out=ysb[yi][:, 0:512], in_=bank(pb), func=AF.Copy),
                             reads=[t_ps[pb]], writes=[t_ysb[yi]])
                    else:
                        P.op("dve", lambda e, pb=pb, yi=yi: e.tensor_copy(out=ysb[yi][:, 512:1024], in_=bank(pb)),
                             reads=[t_ps[pb]], writes=[t_ysb[yi]])
                r0 = t * 512 + tq * 128
                P.dma("sp", yg[r0:r0 + 128, :], ysb[yi], reads=[t_ysb[yi]], writes=[Tok()])

        issue_loads(0)
        casts(0)
        trans(0)
        for t in range(NTE):
            if t + 1 < NTE:
                issue_loads(t + 1)
            mm1(t)
            if t + 1 < NTE:
                trans(t + 1)
                casts(t + 1)
            mm2(t)
        P.barrier()
        ar.off = mark_keep
        ru = ResUpd()
        NY = 3
        yk = [[ar.alloc([D], F32) for _ in range(2)] for _ in range(NY)]
        t_yk = [[Tok(), Tok()] for _ in range(NY)]
        yc = [ar.alloc([D], F32) for _ in range(2)]
        t_yc = [Tok(), Tok()]

        def gath(ti):
            s3 = ti % NY
            for k_ in range(2):
                P.op("pool", lambda e, ti=ti, k_=k_, s3=s3: e.indirect_dma_start(
                    out=yk[s3][k_], out_offset=None, in_=yg,
                    in_offset=bass.IndirectOffsetOnAxis(ap=sidx[:, ti, k_:k_ + 1], axis=0)),
                    reads=[t_idx, t_yg], writes=[t_yk[s3][k_]], dma=True)

        gath(0)
        gath(1)
        for ti, (b, i) in enumerate(tl):
            s2 = ti % 2
            s3 = ti % NY
            ru.prefetch(b, i)
            P.op("dve", lambda e, ti=ti, s2=s2, s3=s3: e.tensor_scalar(out=yc[s2], in0=yk[s3][0], scalar1=wts[:, ti, 0:1], scalar2=None,
                                                                         op0=ALU.mult), reads=[t_yk[s3][0], t_S], writes=[t_yc[s2]])
            P.op("dve", lambda e, ti=ti, s2=s2, s3=s3: e.scalar_tensor_tensor(out=yc[s2], in0=yk[s3][1], scalar=wts[:, ti, 1:2], in1=yc[s2],
                                                                                op0=ALU.mult, op1=ALU.add),
                 reads=[t_yk[s3][1], t_S, t_yc[s2]], writes=[t_yc[s2]])
            if ti + 2 < NTT:
                gath(ti + 2)
            gs = mset(b, i)
            dst = None
            if last and not dbg:
                r0 = rows(b, i)
                dst = out[r0:r0 + 128, :]
            ru.apply(b, i, [(yc[s2], 0, D)], [t_yc[s2]], G2[gs], t_G2[gs], dst=dst)
        P.barrier()

    phase_ada()
    last_layer = max(layers)
    for l in layers:
        need_ctx = l < NL - 1
        kind = l % 3
        for b in range(2):
            if do_mixer:
                if kind == 0:
                    attn_layer(l, b, need_ctx)
                elif kind == 1:
                    lru_layer(l, b, need_ctx)
                else:
                    pool_layer(l, b, need_ctx)
        if do_moe:
            moe_sparse(l, not need_ctx)
    if dbg:
        for b in range(2):
            for i in range(NTILE):
                r0 = rows(b, i)
                dst = out[r0:r0 + 128, :] if i < 16 else outc[r0 - 4096:r0 - 4096 + 128, :]
                P.dma("sp", dst, res[r0:r0 + 128, :], reads=[t_res[b][i]], writes=[t_res[b][i]])
    fin_reads = [t_res[b][i] for b in range(2) for i in range(NTILE)]
    P.op("sp", lambda e: e.nop(), reads=fin_reads)
    P.barrier()
    P.emit()
    return nc, P


def _fm(v, inner=None):
    v = np.asarray(v, np.float32)
    lead = v.shape[:-1]
    K = v.shape[-1] // 128
    v = v.reshape(*lead, K, 128)
    return np.ascontiguousarray(np.moveaxis(v, -1, 0))


def _wr(w, K):
    L, E, KP, N = w.shape
    w = w.reshape(L, E, K, 128, N).transpose(0, 1, 3, 2, 4)
    return np.ascontiguousarray(w).reshape(L, E * 128 * 2, K * N // 2)


def _rope_tables():
    t = np.arange(S)
    row = (t // 64).astype(np.float32)
    col = (t % 64).astype(np.float32)
    inv = (10000.0 ** (-np.arange(16, dtype=np.float32) / 16)).astype(np.float32)
    ang = np.concatenate([row[:, None] * inv, col[:, None] * inv], axis=-1)
    cos = np.ones((TB, 32), np.float32)
    sin = np.zeros((TB, 32), np.float32)
    cos[:S] = np.cos(ang)
    sin[:S] = np.sin(ang)
    cos = np.ascontiguousarray(cos.reshape(NTILE, 128, 32).transpose(1, 0, 2))
    sin = np.ascontiguousarray(sin.reshape(NTILE, 128, 32).transpose(1, 0, 2))
    return cos, sin


def _pool_edges():
    e = np.zeros((128, 4, 2, 8), np.float32)
    for g, w in enumerate((2, 4, 8, 16)):
        for n in (S,):
            pass
        hw = w // 2
        for jx in range(8):
            t = jx
            cnt = min(t + hw, 10 ** 9) - max(t - hw, 0)
            e[:, g, 0, jx] = 1.0 / cnt
            d = 8 - jx
            cnt = min(hw, d) + hw
            e[:, g, 1, jx] = 1.0 / cnt
    return e


def prep_inputs(inputs, core, resin_override=None):
    f = lambda k: np.ascontiguousarray(np.asarray(inputs[k], np.float32))
    b0, b1 = 2 * core, 2 * core + 1
    x, ctx, c = f("x"), f("ctx"), f("c")
    if resin_override is None:
        resin = np.concatenate([x[b0], x[b1], ctx[b0], ctx[b1]], axis=0)
    else:
        resin = resin_override
    cT = np.ascontiguousarray(np.stack([c[b0], c[b1], f("c_ctx")], axis=1))
    cos, sin = _rope_tables()
    m = {
        "resin": np.ascontiguousarray(resin),
        "cT": cT,
        "w_ada": f("w_ada"),
        "badaT": np.ascontiguousarray(_fm(f("b_ada"))),
        "normgT": np.ascontiguousarray(_fm(f("norm_g"))),
        "ident": np.eye(128, dtype=np.float32),
        "attn_w_in": f("attn_w_in"),
        "attn_q_gain": f("attn_q_gain"),
        "attn_k_gain": f("attn_k_gain"),
        "attn_lam": f("attn_lam").reshape(2, 256),
        "attn_sgT": np.ascontiguousarray(f("attn_sub_gain").T),
        "attn_w_out": f("attn_w_out"),
        "rope_cos": cos,
        "rope_sin": sin,
        "lru_w_in": f("lru_w_in"),
        "lru_cwT": np.ascontiguousarray(_fm(f("lru_conv_w")[0]).transpose(0, 2, 1)),
        "lru_cbT": np.ascontiguousarray(_fm(f("lru_conv_b")[0])),
        "lru_w_a": f("lru_w_a"),
        "lru_w_x": f("lru_w_x"),
        "lru_baT": np.ascontiguousarray(_fm(f("lru_b_a")[0])),
        "lru_bxT": np.ascontiguousarray(_fm(f("lru_b_x")[0])),
        "lru_lamT": np.ascontiguousarray(_fm(f("lru_lam")[0])),
        "lru_w_out": f("lru_w_out"),
        "pool_w_in": f("pool_w_in"),
        "pool_w_grp": f("pool_w_grp"),
        "pool_scale": f("pool_scale"),
        "pool_edge": _pool_edges(),
        "moe_router": np.ascontiguousarray(np.concatenate([f("moe_router_g"), f("moe_router_e")], axis=-1)),
        "moe_w1r": _wr(f("moe_w1"), 8),
        "moe_w3r": _wr(f("moe_w3"), 8),
        "moe_w2r": _wr(f("moe_w2"), 4),
        "normg_raw": f("norm_g"),
        "utri": np.triu(np.ones((128, 128), np.float32), 1),
        "tvals": np.broadcast_to((np.arange(40, dtype=np.float32) * 512.0)[None, :], (128, 40)).copy(),
        "pidx": np.arange(128, dtype=np.float32).reshape(128, 1),
    }
    return m


_CACHE = {}


def kernel(**inputs):
    if "nc" not in _CACHE:
        _CACHE["nc"] = build()[0]
    nc = _CACHE["nc"]
    shared = prep_inputs(inputs, 0)
    x, ctx, c = (np.asarray(inputs[k], np.float32) for k in ("x", "ctx", "c"))
    cc = np.asarray(inputs["c_ctx"], np.float32)
    in_maps = []
    for core in range(8):
        m = dict(shared)
        b0, b1 = 2 * core, 2 * core + 1
        m["resin"] = np.ascontiguousarray(np.concatenate([x[b0], x[b1], ctx[b0], ctx[b1]], axis=0))
        m["cT"] = np.ascontiguousarray(np.stack([c[b0], c[b1], cc], axis=1))
        in_maps.append(m)
    res = run_bass_kernel_spmd(nc, in_maps, core_ids=list(range(8)))
    outs = [r["out"].reshape(2, S, D) for r in res.results]
    return np.concatenate(outs, axis=0).astype(np.float32)
```
